# Optimizing a Trainium2 kernel written in Bass

```python
import math
import jax, jax.numpy as jnp
from jax import lax
import numpy as np

D_MODEL = 2048
BATCH = 4
SEQ = 4096
DEPTH = 1

CONV_WIDTH = D_MODEL // 2
CONV_K = 3
N_HEADS = 16
N_KV_HEADS = 4
HEAD_DIM = 64
ATTN_WIDTH = N_HEADS * HEAD_DIM
KV_WIDTH = N_KV_HEADS * HEAD_DIM
IDX_HEADS = 16
IDX_DIM = 64
IDX_TOPK_MAX = 256
Q_BLOCK = 128
REL_BUCKETS = 32
REL_MAX_DIST = 128
N_EXPERTS = 64
N_GROUPS = 8
TOPK_GROUPS = 4
TOP_K = 8
D_EXPERT = 512
ROUTED_SCALE = 2.5
EPS = 1e-6
NEG = -1e30

kernel_name = "hybrid_conv_dsa_moe_block"


def rms_norm(x, w):
    xf = x.astype(jnp.float32)
    y = xf * lax.rsqrt(jnp.mean(xf * xf, axis=-1, keepdims=True) + EPS)
    return (y * w.astype(jnp.float32)).astype(x.dtype)


def layer_norm(x, w, b):
    xf = x.astype(jnp.float32)
    mu = jnp.mean(xf, axis=-1, keepdims=True)
    var = jnp.mean(jnp.square(xf - mu), axis=-1, keepdims=True)
    y = (xf - mu) * lax.rsqrt(var + EPS)
    return (y * w.astype(jnp.float32) + b.astype(jnp.float32)).astype(x.dtype)


def t5_bucket(dist):
    n = jnp.maximum(dist, 0)
    max_exact = REL_BUCKETS // 2
    nf = jnp.maximum(n, 1).astype(jnp.float32)
    large = max_exact + (jnp.log(nf / max_exact) / math.log(REL_MAX_DIST / max_exact)
                         * (REL_BUCKETS - max_exact)).astype(jnp.int32)
    large = jnp.minimum(large, REL_BUCKETS - 1)
    return jnp.where(n < max_exact, n, large)


def short_conv_mixer(b_gate, c_gate, u, conv_w):
    v = c_gate * u
    y = lax.conv_general_dilated(
        v, conv_w[:, None, :].astype(v.dtype), window_strides=(1,),
        padding=[(CONV_K - 1, 0)], dimension_numbers=("NWC", "WIO", "NWC"),
        feature_group_count=CONV_WIDTH)
    return b_gate * y


def dsa_attention(q, k, v, qi, ki, wi, rel_bias):
    B, S = q.shape[0], q.shape[1]
    n_sel = min(IDX_TOPK_MAX, S // 4)
    nb = S // Q_BLOCK
    rep = N_HEADS // N_KV_HEADS
    key_pos = jnp.arange(S, dtype=jnp.int32)

    def blockify(a):
        return a.reshape((B, nb, Q_BLOCK) + a.shape[2:]).swapaxes(0, 1)

    def one_block(args):
        blk, qb, qib, wib = args
        t = blk * Q_BLOCK + jnp.arange(Q_BLOCK, dtype=jnp.int32)
        dots = jnp.einsum("bqhd,bsd->bqhs", qib, ki, preferred_element_type=jnp.float32)
        idx_score = jnp.einsum("bqh,bqhs->bqs", wib.astype(jnp.float32),
                               jax.nn.relu(dots)) * (IDX_DIM ** -0.5)
        causal = key_pos[None, :] <= t[:, None]
        idx_score = jnp.where(causal[None], idx_score, NEG)
        _, sel = lax.top_k(idx_score, n_sel)
        valid = sel <= t[None, :, None]
        k_sel = jax.vmap(lambda kk, ii: kk[ii])(k, sel)
        v_sel = jax.vmap(lambda vv, ii: vv[ii])(v, sel)
        qg = qb.reshape(B, Q_BLOCK, N_KV_HEADS, rep, HEAD_DIM)
        logits = jnp.einsum("bqgrd,bqngd->bqgrn", qg, k_sel,
                            preferred_element_type=jnp.float32) * (HEAD_DIM ** -0.5)
        bias = rel_bias[t5_bucket(t[None, :, None] - sel)]
        bias = bias.reshape(B, Q_BLOCK, n_sel, N_KV_HEADS, rep).transpose(0, 1, 3, 4, 2)
        logits = logits + bias.astype(jnp.float32)
        logits = jnp.where(valid[:, :, None, None, :], logits, NEG)
        p = jax.nn.softmax(logits, axis=-1)
        o = jnp.einsum("bqgrn,bqngd->bqgrd", p.astype(v.dtype), v_sel)
        return o.reshape(B, Q_BLOCK, ATTN_WIDTH)

    out = lax.map(one_block, (jnp.arange(nb, dtype=jnp.int32), blockify(q),
                              blockify(qi), blockify(wi)))
    return out.swapaxes(0, 1).reshape(B, S, ATTN_WIDTH)


def swiglu(x, w1, w3, w2):
    return jnp.dot(jax.nn.silu(jnp.dot(x, w1)) * jnp.dot(x, w3), w2)


def moe_ffn(h, w_router, router_bias, w1, w3, w2, ws1, ws3, ws2):
    B, S, D = h.shape
    xt = h.reshape(B * S, D)
    scores = jax.nn.sigmoid(jnp.dot(xt, w_router, preferred_element_type=jnp.float32))
    sel_scores = scores + router_bias.astype(jnp.float32)
    grp = sel_scores.reshape(B * S, N_GROUPS, N_EXPERTS // N_GROUPS)
    grp_score = lax.top_k(grp, 2)[0].sum(-1)
    _, top_g = lax.top_k(grp_score, TOPK_GROUPS)
    gmask = jnp.sum(jax.nn.one_hot(top_g, N_GROUPS, dtype=jnp.float32), axis=1) > 0
    emask = jnp.repeat(gmask, N_EXPERTS // N_GROUPS, axis=1)
    _, top_e = lax.top_k(jnp.where(emask, sel_scores, NEG), TOP_K)
    w_sel = jnp.take_along_axis(scores, top_e, axis=1)
    w_sel = w_sel / jnp.sum(w_sel, axis=-1, keepdims=True) * ROUTED_SCALE
    combine = jnp.einsum("nk,nke->ne", w_sel,
                         jax.nn.one_hot(top_e, N_EXPERTS, dtype=jnp.float32)).astype(xt.dtype)
    out = swiglu(xt, ws1, ws3, ws2)
    for e in range(N_EXPERTS):
        out = out + combine[:, e:e + 1] * swiglu(xt, w1[e], w3[e], w2[e])
    return out.reshape(B, S, D)


def _split_sizes():
    return [CONV_WIDTH, CONV_WIDTH, CONV_WIDTH, ATTN_WIDTH, KV_WIDTH, KV_WIDTH,
            IDX_HEADS * IDX_DIM, IDX_DIM, IDX_HEADS, D_MODEL, D_MODEL]


def setup_inputs(seed: int = 0) -> dict:
    key = jax.random.key(seed)
    ks = jax.random.split(key, 32)
    L, D, E, F = DEPTH, D_MODEL, N_EXPERTS, D_EXPERT
    total_in = sum(_split_sizes())

    def nrm(k, shape, scale):
        return jax.random.normal(k, shape, jnp.float32) * scale

    return {
        "x": nrm(ks[0], (BATCH, SEQ, D), 1.0),
        "c": nrm(ks[1], (BATCH, D), 1.0),
        "rel_bias": nrm(ks[2], (REL_BUCKETS, N_HEADS), 0.5),
        "norm1_w": 1.0 + nrm(ks[3], (L, D), 0.02),
        "norm2_w": 1.0 + nrm(ks[4], (L, D), 0.02),
        "w_ada": nrm(ks[5], (L, D, 6 * D), 0.3 * D ** -0.5),
        "b_ada": nrm(ks[6], (L, 6 * D), 0.02),
        "w_in": nrm(ks[7], (L, D, total_in), D ** -0.5),
        "conv_w": nrm(ks[8], (L, CONV_K, CONV_WIDTH), CONV_K ** -0.5),
        "w_conv_out": nrm(ks[9], (L, CONV_WIDTH, D), CONV_WIDTH ** -0.5),
        "q_norm_w": 1.0 + nrm(ks[10], (L, HEAD_DIM), 0.02),
        "k_norm_w": 1.0 + nrm(ks[11], (L, HEAD_DIM), 0.02),
        "idx_k_norm_w": 1.0 + nrm(ks[12], (L, IDX_DIM), 0.02),
        "idx_k_norm_b": nrm(ks[13], (L, IDX_DIM), 0.02),
        "w_attn_out": nrm(ks[14], (L, ATTN_WIDTH, D), ATTN_WIDTH ** -0.5),
        "w_o": nrm(ks[15], (L, D, D), D ** -0.5),
        "w_router": nrm(ks[16], (L, D, E), D ** -0.5),
        "router_bias": nrm(ks[17], (L, E), 0.01),
        "w1": nrm(ks[18], (L, E, D, F), D ** -0.5),
        "w3": nrm(ks[19], (L, E, D, F), D ** -0.5),
        "w2": nrm(ks[20], (L, E, F, D), F ** -0.5),
        "ws1": nrm(ks[21], (L, D, F), D ** -0.5),
        "ws3": nrm(ks[22], (L, D, F), D ** -0.5),
        "ws2": nrm(ks[23], (L, F, D), F ** -0.5),
    }


def reference(x, c, rel_bias, norm1_w, norm2_w, w_ada, b_ada, w_in, conv_w, w_conv_out,
              q_norm_w, k_norm_w, idx_k_norm_w, idx_k_norm_b, w_attn_out, w_o,
              w_router, router_bias, w1, w3, w2, ws1, ws3, ws2):
    B, S, D = x.shape
    sizes = _split_sizes()
    splits = [int(v) for v in np.cumsum(sizes)[:-1]]
    for l in range(DEPTH):
        mod = jnp.dot(jax.nn.silu(c), w_ada[l]) + b_ada[l]
        sh1, sc1, g1, sh2, sc2, g2 = [m[:, None, :] for m in jnp.split(mod, 6, axis=-1)]

        h = rms_norm(x, norm1_w[l]) * (1 + sc1) + sh1
        proj = jnp.dot(h, w_in[l])
        cb, cc, cu, q, k, v, qi, ki, wi, ga, gb = jnp.split(proj, splits, axis=-1)

        y_conv = jnp.dot(short_conv_mixer(cb, cc, cu, conv_w[l]), w_conv_out[l])

        q = rms_norm(q.reshape(B, S, N_HEADS, HEAD_DIM), q_norm_w[l])
        k = rms_norm(k.reshape(B, S, N_KV_HEADS, HEAD_DIM), k_norm_w[l])
        v = v.reshape(B, S, N_KV_HEADS, HEAD_DIM)
        qi = qi.reshape(B, S, IDX_HEADS, IDX_DIM)
        ki = layer_norm(ki, idx_k_norm_w[l], idx_k_norm_b[l])
        wi = wi * (IDX_HEADS ** -0.5)
        y_attn = jnp.dot(dsa_attention(q, k, v, qi, ki, wi, rel_bias), w_attn_out[l])

        mixed = jax.nn.sigmoid(ga) * y_conv + jax.nn.sigmoid(gb) * y_attn
        x = x + g1 * jnp.dot(mixed, w_o[l])

        h2 = rms_norm(x, norm2_w[l]) * (1 + sc2) + sh2
        x = x + g2 * moe_ffn(h2, w_router[l], router_bias[l], w1[l], w3[l], w2[l],
                             ws1[l], ws3[l], ws2[l])
    return x
```

```python
import math
import numpy as np
import concourse.bass as bass
import concourse.mybir as mybir
from concourse.bass_utils import run_bass_kernel_spmd

F32 = mybir.dt.float32
BF16 = mybir.dt.bfloat16
AF = mybir.ActivationFunctionType
ALU = mybir.AluOpType
AX = mybir.AxisListType

D = 2048
KC = 16
S = 4096
NBLK = 32
OWN = 16
NT = 4
E = 64
FE = 512
BIG = 1.0e30
EPS = 1e-6
NBIS = 18
COMPUTE = ("pe", "act", "dve", "pool")
DEBUG = {}


class Op:
    __slots__ = ("eng", "fn", "deps", "needed", "event", "is_dma", "presem")

    def __init__(self, eng, fn, is_dma=False):
        self.eng, self.fn, self.is_dma = eng, fn, is_dma
        self.deps, self.needed, self.event, self.presem = [], False, None, None


class Sched:
    def __init__(self):
        self.ops = {e: [] for e in ("pe", "act", "dve", "pool", "sp")}
        self.bufw, self.bufr = {}, {}
        self.since = []
        self.limit = None
        self.count = 0

    def op(self, eng, fn, r=(), w=(), is_dma=False):
        o = Op(eng, fn, is_dma)
        self.count += 1
        if self.limit is not None and self.count > self.limit:
            return o
        deps = set()
        for k in r:
            if k in self.bufw:
                deps.add(self.bufw[k])
        for k in w:
            if k in self.bufw:
                deps.add(self.bufw[k])
            deps.update(self.bufr.get(k, ()))
        deps.discard(o)
        o.deps = list(deps)
        for d in o.deps:
            d.needed = True
        for k in r:
            self.bufr.setdefault(k, []).append(o)
        for k in w:
            self.bufw[k] = o
            self.bufr[k] = []
        self.ops[eng].append(o)
        self.since.append(o)
        return o

    def dma(self, q, out, in_, r=(), w=(), **kw):
        return self.op(q, lambda e: e.dma_start(out=out, in_=in_, **kw), r, w, is_dma=True)

    def barrier(self):
        tails = []
        last = {}
        for o in self.since:
            if o.is_dma:
                tails.append(o)
            else:
                last[o.eng] = o
        tails += list(last.values())
        for t in tails:
            t.needed = True
        for e in self.ops:
            o = Op(e, None)
            o.deps = list(tails)
            self.ops[e].append(o)
        self.since = []
        self.bufw, self.bufr = {}, {}


def t5_bucket_np(n):
    n = np.maximum(n, 0)
    nf = np.maximum(n, 1).astype(np.float32)
    large = 16 + (np.log(nf / np.float32(16)) / np.float32(math.log(8.0)) * np.float32(16)).astype(np.int32)
    large = np.minimum(large, 31)
    return np.where(n < 16, n, large)


def build_program(dbg=()):
    nc = bass.Bass("TRN2", target_bir_lowering=False)
    sch = Sched()
    for d_ in dbg:
        if isinstance(d_, str) and d_.startswith("maxops="):
            sch.limit = int(d_.split("=")[1])

    def din(name, shape, dt=F32):
        return nc.dram_tensor(name, list(shape), dt, kind="ExternalInput").ap()

    def dscr(name, shape, dt):
        kind = "ExternalOutput" if name in dbg else "Internal"
        return nc.dram_tensor(name, list(shape), dt, kind=kind).ap()

    xa = din("xa", [S, D])
    xo = din("xo", [OWN * 128, D])
    xh = din("xh", [32, D])
    hmask_d = din("hmask", [128, 32])
    cmask_d = din("cmask", [128, 256])
    biasT_d = din("biasT", [128, 4 * 16 * 128])
    c_d = din("c", [16, 128])
    n1_d = din("norm1_w", [16, 128])
    n2_d = din("norm2_w", [16, 128])
    wada_d = din("w_ada", [D, 6 * D])
    bada_d = din("b_ada", [96, 128])
    win_d = din("w_in", [D, 9808])
    convw_d = din("conv_w", [24, 128])
    wco_d = din("w_conv_out", [1024, D])
    qnw_d = din("q_norm_w", [64, 1])
    knw_d = din("k_norm_w", [64, 1])
    ikw_d = din("idx_k_norm_w", [64, 1])
    ikb_d = din("idx_k_norm_b", [64, 1])
    wao_d = din("w_attn_out", [1024, D])
    wo_d = din("w_o", [D, D])
    wr_d = din("w_router", [D, E])
    rb_d = din("router_bias", [1, E])
    w1_d = din("w1", [E, D, FE])
    w3_d = din("w3", [E, D, FE])
    w2_d = din("w2", [E, FE, D])
    ws1_d = din("ws1", [D, FE])
    ws3_d = din("ws3", [D, FE])
    ws2_d = din("ws2", [FE, D])
    out_d = nc.dram_tensor("out", [OWN * 128, D], F32, kind="ExternalOutput").ap()

    qT_s = dscr("qT_s", [OWN, 128, 8 * 128], BF16)
    qiT_s = dscr("qiT_s", [OWN, 128, 8 * 128], BF16)
    sgn_s = dscr("sgn_s", [OWN, 128, 16], F32)
    mc_s = dscr("mc_s", [NT, 128, 16 * 512], BF16)
    sgb_s = dscr("sgb_s", [NT, 128, 16 * 512], BF16)
    attn_s = dscr("attn_s", [OWN, 64, 16 * 128], BF16)
    x1_s = dscr("x1_s", [OWN * 128, D], F32)
    mod_s = dscr("mod_s", [96, 128], F32)

    from contextlib import ExitStack
    es = ExitStack()

    def sb(name, shape, dt):
        return es.enter_context(nc.sbuf_tensor(name, list(shape), dt))

    def pst(name, shape, dt):
        return es.enter_context(nc.psum_tensor(name, list(shape), dt))

    ident_b = sb("ident_b", [128, 128], BF16)
    ident_f = sb("ident_f", [128, 128], F32)
    ones_b = sb("ones_b", [128, 128], BF16)
    ones_f = sb("ones_f", [1, 128], F32)
    modT = sb("modT", [128, 96], F32)
    a1 = sb("a1", [128, 16], F32)
    a2 = sb("a2", [128, 16], F32)
    colv = sb("colv", [128, 8], F32)
    convw = sb("convw", [128, 24], F32)
    hmask = sb("hmask_t", [128, 32], F32)
    cmask = sb("cmask_t", [128, 256], F32)
    rbias = sb("rbias", [128, E], F32)
    ring = [sb(f"ring{i}", [128, 16 * 512], BF16) for i in range(6)]
    ARENA = 53000
    arena = sb("arena", [128, ARENA], BF16)
    ps = [pst(f"ps{i}", [128, 512], F32) for i in range(8)]
    psb = [p[:].bitcast(BF16) for p in ps]

    aoff = [0]

    def aview(n_elems, dt):
        nb = n_elems * (2 if dt == F32 else 1)
        nb = (nb + 1) // 2 * 2
        o = aoff[0]
        assert o + nb <= ARENA, (o, nb)
        aoff[0] = o + nb
        v = arena[:, o:o + nb]
        return v.bitcast(F32) if dt == F32 else v

    def areset():
        aoff[0] = 0

    rstate = {"i": 0}

    def wload(parts):
        i = rstate["i"]
        rstate["i"] += 1
        buf = ring[i % len(ring)]
        key = ("ring", i % len(ring))
        for dst_fn, src in parts:
            sch.dma("pool", dst_fn(buf), src, w=[key])
        return buf, key

    def wchunk(wd, c0, ncols=512, kc=KC):
        src = wd[:, c0:c0 + ncols].rearrange("(k p) n -> p k n", p=128)
        return wload([(lambda b: b[:, 0:kc * ncols].rearrange("p (k n) -> p k n", k=kc), src)])

    V3 = lambda ap, k: ap.rearrange("p (k n) -> p k n", k=k)

    sch.op("pool", lambda e: e.memset(ident_b[:], 0.0), w=["ident_b0"])
    sch.op("pool", lambda e: e.affine_select(out=ident_b[:], in_=ident_b[:], pattern=[[-1, 128]],
                                             compare_op=ALU.not_equal, fill=1.0, base=0, channel_multiplier=1),
           r=["ident_b0"], w=["ident_b"])
    sch.op("pool", lambda e: e.memset(ident_f[:], 0.0), w=["ident_f0"])
    sch.op("pool", lambda e: e.affine_select(out=ident_f[:], in_=ident_f[:], pattern=[[-1, 128]],
                                             compare_op=ALU.not_equal, fill=1.0, base=0, channel_multiplier=1),
           r=["ident_f0"], w=["ident_f"])
    sch.op("pool", lambda e: e.memset(ones_b[:], 1.0), w=["ones_b"])
    sch.op("pool", lambda e: e.memset(ones_f[:], 1.0), w=["ones_f"])
    sch.dma("sp", hmask[:], hmask_d, w=["hmask"])
    sch.dma("sp", cmask[:], cmask_d, w=["cmask"])
    sch.dma("sp", rbias[:], rb_d.partition_broadcast(128), w=["rbias"])
    sch.dma("sp", colv[0:64, 3:4], qnw_d, w=["colv_raw"])
    sch.dma("sp", colv[64:128, 3:4], qnw_d, w=["colv_raw"])
    sch.dma("sp", colv[0:64, 4:5], knw_d, w=["colv_raw"])
    sch.dma("sp", colv[64:128, 4:5], knw_d, w=["colv_raw"])
    sch.dma("sp", colv[0:64, 1:2], ikw_d, w=["colv_raw"])
    sch.dma("sp", colv[64:128, 1:2], ikw_d, w=["colv_raw"])
    sch.dma("sp", colv[0:64, 2:3], ikb_d, w=["colv_raw"])
    sch.dma("sp", colv[64:128, 2:3], ikb_d, w=["colv_raw"])
    sch.op("dve", lambda e: e.scalar_tensor_tensor(out=colv[:, 0:1], in0=colv[:, 3:4], scalar=0.125, in1=colv[:, 4:5],
                                                   op0=ALU.mult, op1=ALU.mult), r=["colv_raw"], w=["colv"])

    areset()
    rows = aview(128, F32)
    silu_c = aview(16, BF16)
    tmpc = aview(96, F32)

    def vec_to_cols(src_d, n, dst_key, psum_ap):
        sch.dma("sp", rows[0:n, :], src_d, w=["rows"])
        sch.op("pe", lambda e: e.transpose(psum_ap, rows[0:n, :], ident_f[0:n, 0:n]), r=["rows", "ident_f"], w=[dst_key])

    vec_to_cols(c_d, 16, "ps0", ps[0][:, 0:16])
    sch.op("act", lambda e: e.activation(out=silu_c[:], in_=ps[0][:, 0:16], func=AF.Silu), r=["ps0"], w=["silu_c"])
    vec_to_cols(n1_d, 16, "ps1", ps[1][:, 0:16])
    sch.op("dve", lambda e: e.tensor_copy(out=a1[:], in_=ps[1][:, 0:16]), r=["ps1"], w=["a1raw"])
    vec_to_cols(n2_d, 16, "ps1", ps[1][:, 0:16])
    sch.op("dve", lambda e: e.tensor_copy(out=a2[:], in_=ps[1][:, 0:16]), r=["ps1"], w=["a2raw"])
    vec_to_cols(bada_d, 96, "ps2", ps[2][:, 0:96])
    sch.op("dve", lambda e: e.tensor_copy(out=tmpc[:], in_=ps[2][:, 0:96]), r=["ps2"], w=["tmpc"])
    vec_to_cols(convw_d, 24, "ps1", ps[1][:, 0:24])
    sch.op("dve", lambda e: e.tensor_copy(out=convw[:], in_=ps[1][:, 0:24]), r=["ps1"], w=["convw"])
    for c in range(24):
        buf, key = wchunk(wada_d, c * 512)
        b3 = V3(buf[:], 16)

        def mm(e, b3=b3, c=c):
            ins = None
            for q in range(4):
                ch = c * 4 + q
                for k in range(KC):
                    ins = e.matmul(ps[3][:, ch:ch + 1], b3[:, k, q * 128:(q + 1) * 128], silu_c[:, k:k + 1],
                                   start=(k == 0), stop=(k == KC - 1))
            return ins
        sch.op("pe", mm, r=[key, "silu_c"], w=["ps3"])
    sch.op("dve", lambda e: e.tensor_tensor(out=modT[:], in0=ps[3][:, 0:96], in1=tmpc[:], op=ALU.add),
           r=["ps3", "tmpc"], w=["modT"])
    sch.op("dve", lambda e: e.scalar_tensor_tensor(out=a1[:], in0=modT[:, 16:32], scalar=1.0, in1=a1[:],
                                                   op0=ALU.add, op1=ALU.mult), r=["modT", "a1raw"], w=["a1"])
    sch.op("dve", lambda e: e.scalar_tensor_tensor(out=a2[:], in0=modT[:, 64:80], scalar=1.0, in1=a2[:],
                                                   op0=ALU.add, op1=ALU.mult), r=["modT", "a2raw"], w=["a2"])
    sh1 = modT[:, 0:16]
    sh2 = modT[:, 48:64]
    sch.op("pe", lambda e: e.transpose(ps[0][0:96, 0:128], modT[:], ident_f[:]), r=["modT", "ident_f"], w=["ps0"])
    sch.op("dve", lambda e: e.tensor_copy(out=rows[0:96, :], in_=ps[0][0:96, 0:128]), r=["ps0"], w=["rows"])
    sch.dma("sp", mod_s, rows[0:96, :], r=["rows"], w=["mod_s"])
    mod_flat = mod_s.rearrange("a b -> (a b)").rearrange("(o n) -> o n", o=1)
    sch.barrier()

    def norm_T(tag, xt, n, acol, shcol, hT_dst, xnb, st, pT, xn_f32=None):
        sch.op("act", lambda e: e.activation(out=xnb[0:n, :], in_=xt, func=AF.Square, accum_out=st[0:n, 0:1]),
               r=[tag + "x"], w=[tag + "xnb", tag + "st0"])
        sch.op("dve", lambda e: e.tensor_scalar(out=st[0:n, 1:2], in0=st[0:n, 0:1], scalar1=1.0 / D, scalar2=EPS,
                                                op0=ALU.mult, op1=ALU.add), r=[tag + "st0"], w=[tag + "st1"])
        sch.op("act", lambda e: e.activation(out=st[0:n, 2:3], in_=st[0:n, 1:2], func=AF.Sqrt),
               r=[tag + "st1"], w=[tag + "st2"])
        sch.op("dve", lambda e: e.reciprocal(out=st[0:n, 3:4], in_=st[0:n, 2:3]), r=[tag + "st2"], w=[tag + "st3"])
        if xn_f32 is not None:
            sch.op("dve", lambda e: e.tensor_scalar(out=xn_f32[0:n, :], in0=xt, scalar1=st[0:n, 3:4], scalar2=None,
                                                    op0=ALU.mult), r=[tag + "x", tag + "st3"], w=[tag + "xnf"])
        sch.op("dve", lambda e: e.tensor_scalar(out=xnb[0:n, :], in0=xt, scalar1=st[0:n, 3:4], scalar2=None,
                                                op0=ALU.mult), r=[tag + "x", tag + "st3"], w=[tag + "xnb"])
        pT3 = [V3(pT[0], 8), V3(pT[1], 8)]

        def tr(e):
            ins = None
            for k in range(KC):
                ins = e.transpose(pT3[k // 8][:, k % 8, 0:n], xnb[0:n, k * 128:(k + 1) * 128], ident_b[0:n, 0:n])
            return ins
        sch.op("pe", tr, r=[tag + "xnb", "ident_b"], w=["pT0", "pT1"])
        for hf in range(2):
            sch.op("dve", lambda e, hf=hf: e.tensor_tensor(
                out=hT_dst[:, hf * 8:(hf + 1) * 8, :], in0=pT3[hf][:, :, 0:n],
                in1=acol[:, hf * 8:(hf + 1) * 8].unsqueeze(2).to_broadcast([128, 8, n]), op=ALU.mult),
                r=["pT%d" % hf, "a1", "a2"], w=[tag + "hT%d" % hf])
            sch.op("dve", lambda e, hf=hf: e.tensor_tensor(
                out=hT_dst[:, hf * 8:(hf + 1) * 8, :], in0=hT_dst[:, hf * 8:(hf + 1) * 8, :],
                in1=shcol[:, hf * 8:(hf + 1) * 8].unsqueeze(2).to_broadcast([128, 8, n]), op=ALU.add),
                r=[tag + "hT%d" % hf, "modT"], w=[tag + "hT%d" % hf])

    pTb = [psb[6], psb[7]]

    areset()
    hT = V3(aview(16 * 520, BF16), 16)
    xblk = [aview(D, F32) for _ in range(2)]
    xnb = aview(D, BF16)
    stt = aview(8, F32)
    qtm = aview(4 * 512, F32)
    qn = aview(4 * 512, BF16)
    qsqb = aview(512, F32)
    qst = aview(64, F32)
    qTt = [aview(8 * 128, BF16) for _ in range(4)]
    wit = aview(64, F32)
    sgn = aview(64, F32)
    absw = aview(64, F32)
    wwi = aview(16 * 16, BF16)
    uT = V3(aview(8 * 512, BF16), 8)
    vbuf = aview(4 * 130, F32)
    cusb = aview(512, F32)
    cuh = aview(8, F32)
    ybuf = aview(512, F32)
    sga = aview(512, F32)
    mcb = [aview(512, BF16) for _ in range(2)]
    sgbb = [aview(512, BF16) for _ in range(2)]
    sch.dma("pool", V3(wwi[:], 16), win_d[:, 5696:5712].rearrange("(k p) n -> p k n", p=128), w=["wwi"])

    for tl in range(NT):
        tg = "p2_"
        for blk in range(4):
            xb = xblk[blk % 2]
            sch.dma("sp", xb[:], xo[(tl * 4 + blk) * 128:(tl * 4 + blk + 1) * 128, :], w=[tg + "x"])
            norm_T(tg, xb[:], 128, a1, sh1, hT[:, :, blk * 128:(blk + 1) * 128], xnb, stt, pTb)
        xb = xblk[0]
        sch.dma("sp", xb[0:8, :], xh[tl * 8:(tl + 1) * 8, :], w=[tg + "x"])
        norm_T(tg, xb[0:8, :], 8, a1, sh1, hT[:, :, 512:520], xnb, stt, pTb)
        HT = [tg + "hT0", tg + "hT1"]

        for blk in range(4):
            def mmwi(e, blk=blk):
                ins = None
                for k in range(KC):
                    ins = e.matmul(ps[4][:, blk * 16:(blk + 1) * 16], hT[:, k, blk * 128:(blk + 1) * 128],
                                   V3(wwi[:], 16)[:, k, :], start=(k == 0), stop=(k == KC - 1))
                return ins
            sch.op("pe", mmwi, r=HT + ["wwi"], w=["ps4"])
        sch.op("dve", lambda e: e.tensor_copy(out=wit[:], in_=ps[4][:, 0:64]), r=["ps4"], w=["wit"])
        sch.op("dve", lambda e: e.tensor_scalar(out=sgn[:], in0=wit[:], scalar1=0.0, scalar2=2.0, op0=ALU.is_gt,
                                                op1=ALU.mult), r=["wit"], w=["sgn0"])
        sch.op("dve", lambda e: e.tensor_scalar(out=sgn[:], in0=sgn[:], scalar1=-1.0, scalar2=None, op0=ALU.add),
               r=["sgn0"], w=["sgn"])
        sch.op("dve", lambda e: e.tensor_tensor(out=absw[:], in0=wit[:], in1=sgn[:], op=ALU.mult),
               r=["wit", "sgn"], w=["absw"])
        for blk in range(4):
            sch.dma("sp", sgn_s[tl * 4 + blk], sgn[:, blk * 16:(blk + 1) * 16], r=["sgn"], w=["sgn_s"])

        for which, c0s in (("q", (3072, 3584)), ("qi", (4608, 5120))):
            for ci, c0 in enumerate(c0s):
                buf, key = wchunk(win_d, c0)
                b3 = V3(buf[:], 16)
                for blk in range(4):
                    pp = ps[blk % 2]

                    def mmq(e, blk=blk, pp=pp, b3=b3):
                        ins = None
                        for k in range(KC):
                            ins = e.matmul(pp[:], hT[:, k, blk * 128:(blk + 1) * 128], b3[:, k, :],
                                           start=(k == 0), stop=(k == KC - 1))
                        return ins
                    sch.op("pe", mmq, r=HT + [key], w=["ps%d" % (blk % 2)])
                    sch.op("act", lambda e, blk=blk, pp=pp: e.activation(out=qtm[:, blk * 512:(blk + 1) * 512], in_=pp[:],
                                                                        func=AF.Copy),
                           r=["ps%d" % (blk % 2)], w=["qtm%d" % blk])
                for blk in range(4):
                    j = tl * 4 + blk
                    q3 = V3(qtm[:, blk * 512:(blk + 1) * 512], 8)
                    qn3 = V3(qn[:, blk * 512:(blk + 1) * 512], 8)
                    if which == "q":
                        sch.op("act", lambda e, blk=blk: e.activation(out=qsqb[:], in_=qtm[:, blk * 512:(blk + 1) * 512],
                                                                      func=AF.Square), r=["qtm%d" % blk], w=["qsq"])
                        sch.op("dve", lambda e: e.tensor_reduce(out=qst[:, 0:8], in_=V3(qsqb[:], 8), axis=AX.X,
                                                                op=ALU.add), r=["qsq"], w=["qst0"])
                        sch.op("dve", lambda e: e.tensor_scalar(out=qst[:, 8:16], in0=qst[:, 0:8], scalar1=1.0 / 64,
                                                                scalar2=EPS, op0=ALU.mult, op1=ALU.add),
                               r=["qst0"], w=["qst1"])
                        sch.op("act", lambda e: e.activation(out=qst[:, 16:24], in_=qst[:, 8:16], func=AF.Sqrt),
                               r=["qst1"], w=["qst2"])
                        sch.op("dve", lambda e: e.reciprocal(out=qst[:, 24:32], in_=qst[:, 16:24]), r=["qst2"], w=["qst3"])
                        qo4 = qn[:, blk * 512:(blk + 1) * 512].rearrange("p (m hi d) -> p hi m d", m=4, hi=2)
                        qi4 = qtm[:, blk * 512:(blk + 1) * 512].rearrange("p (hi m d) -> p hi m d", m=4, hi=2)
                        rs4 = qst[:, 24:32].rearrange("p (hi m) -> p hi m", hi=2).unsqueeze(3).to_broadcast([128, 2, 4, 64])
                        sch.op("dve", lambda e, qo4=qo4, qi4=qi4, rs4=rs4: e.tensor_tensor(out=qo4, in0=qi4, in1=rs4, op=ALU.mult),
                               r=["qtm%d" % blk, "qst3"], w=["qn%d" % blk])
                    else:
                        sch.op("dve", lambda e, q3=q3, qn3=qn3, blk=blk, ci=ci: e.tensor_tensor(
                            out=qn3, in0=q3,
                            in1=absw[:, blk * 16 + ci * 8: blk * 16 + ci * 8 + 8].unsqueeze(2).to_broadcast([128, 8, 64]),
                            op=ALU.mult), r=["qtm%d" % blk, "absw"], w=["qn%d" % blk])
                    qt = qTt[blk]
                    qt3 = V3(qt[:], 8)
                    pT3 = V3(pTb[0], 8)

                    def trq(e, blk=blk):
                        ins = None
                        for m in range(4):
                            src = qn[:, blk * 512 + m * 128: blk * 512 + (m + 1) * 128]
                            ins = e.transpose(pT3[:, m, :], src, ident_b[:])
                        return ins
                    sch.op("pe", trq, r=["qn%d" % blk, "ident_b"], w=["pT0"])
                    sch.op("act", lambda e, qt3=qt3, ci=ci: e.activation(out=qt3[:, ci * 4:(ci + 1) * 4, :], in_=pT3[:, 0:4, :],
                                                                        func=AF.Copy),
                           r=["pT0"], w=["qTt%d_%d" % (blk, ci)])
                    if ci == 1:
                        dst = qT_s if which == "q" else qiT_s
                        sch.dma("sp", dst[j], qt[:], r=["qTt%d_0" % blk, "qTt%d_1" % blk], w=[which + "T_s"])

        for half in range(2):
            bufs = [wchunk(win_d, base + half * 512) for base in (0, 1024, 2048)]
            (bcb, kcb), (bcc, kcc), (bcu, kcu) = [(V3(b[:], 16), k) for b, k in bufs]
            for q4 in range(4):
                ch = half * 4 + q4
                cs = slice(q4 * 128, (q4 + 1) * 128)

                def mmc(e, cs=cs, bcb=bcb, bcc=bcc, bcu=bcu):
                    ins = None
                    for pi, bw in ((0, bcb), (1, bcc), (2, bcu)):
                        for k in range(KC):
                            ins = e.matmul(ps[pi][:], bw[:, k, cs], hT[:, k, 0:512], start=(k == 0), stop=(k == KC - 1))
                    for hi, bw in ((0, bcc), (1, bcu)):
                        for k in range(KC):
                            ins = e.matmul(ps[3][:, hi * 8:(hi + 1) * 8], bw[:, k, cs], hT[:, k, 512:520],
                                           start=(k == 0), stop=(k == KC - 1))
                    return ins
                sch.op("pe", mmc, r=HT + [kcb, kcc, kcu], w=["ps0", "ps1", "ps2", "ps3"])
                vb3 = V3(vbuf[:], 4)
                sch.op("act", lambda e: e.activation(out=cusb[:], in_=ps[2][:], func=AF.Copy), r=["ps2"], w=["cusb"])
                sch.op("act", lambda e: e.activation(out=cuh[:], in_=ps[3][:, 8:16], func=AF.Copy), r=["ps3"], w=["cuh"])
                sch.op("dve", lambda e: e.tensor_tensor(out=vb3[:, :, 2:130], in0=V3(ps[1][:], 4), in1=V3(cusb[:], 4),
                                                        op=ALU.mult), r=["ps1", "cusb"], w=["vbuf_a"])
                sch.op("dve", lambda e: e.tensor_tensor(out=cuh[:], in0=ps[3][:, 0:8], in1=cuh[:], op=ALU.mult),
                       r=["ps3", "cuh"], w=["cuh2"])
                sch.op("dve", lambda e, tl=tl: e.tensor_tensor(out=vb3[:, :, 0:2], in0=V3(cuh[:], 4),
                                                               in1=V3(hmask[:, tl * 8:(tl + 1) * 8], 4), op=ALU.mult),
                       r=["cuh2", "hmask"], w=["vbuf_b"])
                y3 = V3(ybuf[:], 4)
                sch.op("dve", lambda e, ch=ch: e.tensor_scalar(out=y3, in0=vb3[:, :, 2:130], scalar1=convw[:, 16 + ch:17 + ch],
                                                               scalar2=None, op0=ALU.mult),
                       r=["vbuf_a", "vbuf_b", "convw"], w=["ybuf"])
                sch.op("dve", lambda e, ch=ch: e.scalar_tensor_tensor(out=y3, in0=vb3[:, :, 1:129], scalar=convw[:, 8 + ch:9 + ch],
                                                                      in1=y3, op0=ALU.mult, op1=ALU.add),
                       r=["vbuf_a", "vbuf_b", "ybuf"], w=["ybuf"])
                sch.op("dve", lambda e, ch=ch: e.scalar_tensor_tensor(out=y3, in0=vb3[:, :, 0:128], scalar=convw[:, ch:ch + 1],
                                                                      in1=y3, op0=ALU.mult, op1=ALU.add),
                       r=["vbuf_a", "vbuf_b", "ybuf"], w=["ybuf"])
                sch.op("dve", lambda e, ch=ch: e.tensor_tensor(out=uT[:, ch, :], in0=ps[0][:], in1=ybuf[:], op=ALU.mult),
                       r=["ps0", "ybuf"], w=["uT"])

        for c4 in range(4):
            bga, kga = wchunk(win_d, 5712 + c4 * 512)
            bgb, kgb = wchunk(win_d, 7760 + c4 * 512)
            bco, kco = wchunk(wco_d, c4 * 512, kc=8)
            bga3, bgb3, bco3 = V3(bga[:], 16), V3(bgb[:], 16), V3(bco[:, 0:8 * 512], 8)
            for q4 in range(4):
                cc = c4 * 4 + q4
                cs = slice(q4 * 128, (q4 + 1) * 128)

                def mmg(e, cs=cs, bga3=bga3, bgb3=bgb3, bco3=bco3):
                    ins = None
                    for k in range(KC):
                        ins = e.matmul(ps[0][:], bga3[:, k, cs], hT[:, k, 0:512], start=(k == 0), stop=(k == KC - 1))
                    for k in range(KC):
                        ins = e.matmul(ps[1][:], bgb3[:, k, cs], hT[:, k, 0:512], start=(k == 0), stop=(k == KC - 1))
                    for k in range(8):
                        ins = e.matmul(ps[2][:], bco3[:, k, cs], uT[:, k, :], start=(k == 0), stop=(k == 7))
                    return ins
                sch.op("pe", mmg, r=HT + [kga, kgb, kco, "uT"], w=["ps0", "ps1", "ps2"])
                sch.op("act", lambda e: e.activation(out=sga[:], in_=ps[0][:], func=AF.Sigmoid), r=["ps0"], w=["sga"])
                sgo, mco = sgbb[cc % 2], mcb[cc % 2]
                sch.op("act", lambda e, sgo=sgo: e.activation(out=sgo[:], in_=ps[1][:], func=AF.Sigmoid),
                       r=["ps1"], w=["sgbb%d" % (cc % 2)])
                sch.op("dve", lambda e, mco=mco: e.tensor_tensor(out=mco[:], in0=ps[2][:], in1=sga[:],
                                                                 op=ALU.mult), r=["ps2", "sga"], w=["mcb%d" % (cc % 2)])
                sch.dma("sp", mc_s[tl][:, cc * 512:(cc + 1) * 512], mco[:], r=["mcb%d" % (cc % 2)], w=["mc_s"])
                sch.dma("sp", sgb_s[tl][:, cc * 512:(cc + 1) * 512], sgo[:], r=["sgbb%d" % (cc % 2)], w=["sgb_s"])
    sch.barrier()
    if "stop2" in dbg:
        return nc, sch, es

    areset()
    kT = V3(aview(2 * S, BF16), 2)
    Vt = V3(aview(NBLK * 320, BF16), NBLK)
    kiT = aview(S, BF16)
    bT = aview(4 * 16 * 128, BF16)
    P3BASE = aoff[0]
    hT1 = [V3(aview(16 * 128, BF16), 16) for _ in range(2)]
    xblk = [ring[0][:, 0:2 * D].bitcast(F32), ring[1][:, 0:2 * D].bitcast(F32)]
    kjunk = aview(64, F32)
    xnb = aview(D, BF16)
    stt = aview(8, F32)
    wkv = V3(aview(16 * 576, BF16), 16)
    ksq = aview(256, F32)
    kst = aview(32, F32)
    kn = aview(256, BF16)
    kic = aview(64, F32)
    kicb = aview(128, BF16)
    sch.dma("pool", wkv[:, :, 0:512], win_d[:, 4096:4608].rearrange("(k p) n -> p k n", p=128), w=["wkv"])
    sch.dma("pool", wkv[:, :, 512:576], win_d[:, 5632:5696].rearrange("(k p) n -> p k n", p=128), w=["wkv"])
    sch.dma("pool", bT[:], biasT_d, w=["bT"], max_dma_last_dim=8192)
    sch.op("pool", lambda e: e.memset(Vt[:, :, 256:320], 0.0), w=["Vpad"])
    for g in range(NBLK):
        tg = "p1_%d" % (g % 2)
        xb = xblk[g % 2]
        h1 = hT1[g % 2]
        sch.dma("sp", xb[:], xa[g * 128:(g + 1) * 128, :], w=[tg + "x"])
        norm_T(tg, xb[:], 128, a1, sh1, h1, xnb, stt, pTb)
        HT = [tg + "hT0", tg + "hT1"]

        def mmk(e, h1=h1):
            ins = None
            for k in range(KC):
                ins = e.matmul(ps[0][:], h1[:, k, :], wkv[:, k, 0:512], start=(k == 0), stop=(k == KC - 1))
            for k in range(KC):
                ins = e.matmul(ps[1][:, 0:64], h1[:, k, :], wkv[:, k, 512:576], start=(k == 0), stop=(k == KC - 1))
            return ins
        sch.op("pe", mmk, r=HT + ["wkv"], w=["ps0", "ps1"])
        sch.op("act", lambda e, g=g: e.activation(out=Vt[:, g, 0:256], in_=ps[0][:, 256:512], func=AF.Copy), r=["ps0"], w=["V"])
        sch.op("act", lambda e: e.activation(out=ksq[:], in_=ps[0][:, 0:256], func=AF.Square), r=["ps0"], w=["ksq"])
        sch.op("dve", lambda e: e.tensor_reduce(out=kst[:, 0:4], in_=V3(ksq[:], 4), axis=AX.X, op=ALU.add),
               r=["ksq"], w=["kst0"])
        sch.op("dve", lambda e: e.tensor_scalar(out=kst[:, 4:8], in0=kst[:, 0:4], scalar1=1.0 / 64, scalar2=EPS,
                                                op0=ALU.mult, op1=ALU.add), r=["kst0"], w=["kst1"])
        sch.op("act", lambda e: e.activation(out=kst[:, 8:12], in_=kst[:, 4:8], func=AF.Sqrt), r=["kst1"], w=["kst2"])
        sch.op("dve", lambda e: e.reciprocal(out=kst[:, 12:16], in_=kst[:, 8:12]), r=["kst2"], w=["kst3"])
        sch.op("dve", lambda e: e.tensor_tensor(out=V3(kn[:], 4), in0=V3(ps[0][:, 0:256], 4),
                                                in1=kst[:, 12:16].unsqueeze(2).to_broadcast([128, 4, 64]), op=ALU.mult),
               r=["ps0", "kst3"], w=["kn"])
        sch.op("dve", lambda e: e.tensor_reduce(out=kst[:, 16:17], in_=ps[1][:, 0:64], axis=AX.X, op=ALU.add),
               r=["ps1"], w=["ki0"])
        sch.op("dve", lambda e: e.tensor_scalar(out=kst[:, 17:18], in0=kst[:, 16:17], scalar1=-1.0 / 64, scalar2=None,
                                                op0=ALU.mult), r=["ki0"], w=["ki1"])
        sch.op("dve", lambda e: e.tensor_scalar(out=kic[:], in0=ps[1][:, 0:64], scalar1=kst[:, 17:18], scalar2=None,
                                                op0=ALU.add), r=["ps1", "ki1"], w=["kic"])
        sch.op("act", lambda e: e.activation(out=kjunk[:], in_=kic[:], func=AF.Square, accum_out=kst[:, 18:19]),
               r=["kic"], w=["ki2", "p1junk"])
        sch.op("dve", lambda e: e.tensor_scalar(out=kst[:, 19:20], in0=kst[:, 18:19], scalar1=1.0 / 64, scalar2=EPS,
                                                op0=ALU.mult, op1=ALU.add), r=["ki2"], w=["ki3"])
        sch.op("act", lambda e: e.activation(out=kst[:, 20:21], in_=kst[:, 19:20], func=AF.Sqrt), r=["ki3"], w=["ki4"])
        sch.op("dve", lambda e: e.reciprocal(out=kst[:, 21:22], in_=kst[:, 20:21]), r=["ki4"], w=["ki5"])
        for hf in range(2):
            sch.op("dve", lambda e, hf=hf: e.tensor_scalar(out=kicb[:, hf * 64:(hf + 1) * 64], in0=kic[:],
                                                           scalar1=kst[:, 21:22], scalar2=None, op0=ALU.mult),
                   r=["kic", "ki5"], w=["kicb%d" % hf])
        pT3 = V3(pTb[0], 8)

        def trk(e):
            e.transpose(pT3[:, 0, :], kn[:, 0:128], ident_b[:])
            e.transpose(pT3[:, 1, :], kn[:, 128:256], ident_b[:])
            return e.transpose(pT3[:, 2, :], kicb[:], ident_b[:])
        sch.op("pe", trk, r=["kn", "kicb0", "kicb1", "ident_b"], w=["pT0"])
        for pr in range(2):
            sch.op("act", lambda e, pr=pr, g=g: e.activation(out=kT[:, pr, g * 128:(g + 1) * 128], in_=pT3[:, pr, :],
                                                            func=AF.Identity, scale=colv[:, 0:1]),
                   r=["pT0", "colv"], w=["kT"])
        sch.op("act", lambda e, g=g: e.activation(out=kiT[:, g * 128:(g + 1) * 128], in_=pT3[:, 2, :], func=AF.Identity,
                                                  scale=colv[:, 1:2], bias=colv[:, 2:3]), r=["pT0", "colv_raw"], w=["kiT"])
    sch.barrier()
    if "stop1" in dbg:
        return nc, sch, es

    aoff[0] = P3BASE
    qzb = [aview(16 * 128, BF16) for _ in range(3)]
    for qq_ in qzb:
        sch.op("pool", lambda e, qq_=qq_: e.memset(qq_[:], 0.0), w=["qz_init"])
    sch.barrier()
    qiTb = [aview(8 * 128, BF16) for _ in range(3)]
    sgb_ = [aview(16, F32) for _ in range(3)]
    scoresb = [ring[0][:, 0:2 * S].bitcast(F32), ring[3][:, 0:2 * S].bitcast(F32)]
    m01 = ring[1][:, 0:S]
    sjunk = ring[1][:, S:2 * S]
    mT = V3(ring[2][:, 0:NBLK * 128], NBLK)
    Dmb = [V3(ring[2][:, 4096:4096 + 2048], 16), V3(ring[4][:, 0:2048], 16)]
    rbuf = [ring[2][:, 6144 + i * 512:6144 + (i + 1) * 512] for i in range(4)]
    amb = [aview(16, F32) for _ in range(2)]
    bst = aview(16, F32)
    pbuf = [aview(512, BF16) for _ in range(3)]
    rl = aview(512, F32)
    attn = [aview(16 * 128, BF16) for _ in range(2)]
    bT4 = bT[:].rearrange("p (r h t) -> p r h t", r=4, h=16)
    NQ = OWN if "p3n" not in dbg else 3
    pTm = V3(psb[7], 8)

    def geom(j):
        nkb = 2 * j + 2
        n = nkb * 128
        nch = (n + 511) // 512
        return nkb, n, nch

    def gen_indexer(j):
        nkb, n, nch = geom(j)
        t3, t2 = j % 3, j % 2
        tg = "p3_%d" % t3
        qz3 = V3(qzb[t3][:], 16)
        qiT3 = V3(qiTb[t3][:], 8)
        sg = sgb_[t3]
        Dm = Dmb[t2]
        scores = scoresb[t2]
        am = amb[t2]
        DK, SK, AK = "Dm%d" % t2, "scores%d" % t2, "am%d" % t2
        qsrc = qT_s[j].rearrange("p (s t) -> p s t", s=8)
        for ci in range(2):
            sch.dma("sp", qz3[0:64, 8 * ci:8 * ci + 4, :], qsrc[0:64, 4 * ci:4 * ci + 4, :], w=[tg + "qT"])
            sch.dma("sp", qz3[64:128, 8 * ci + 4:8 * ci + 8, :], qsrc[64:128, 4 * ci:4 * ci + 4, :], w=[tg + "qT"])
        sch.dma("sp", qiTb[t3][:], qiT_s[j], w=[tg + "qiT"])
        sch.dma("sp", sg[:], sgn_s[j], w=[tg + "sg"])
        sch.op("dve", lambda e: e.tensor_tensor(out=Dm, in0=ident_b[:].unsqueeze(1).to_broadcast([128, 16, 128]),
                                                in1=sg[:].unsqueeze(2).to_broadcast([128, 16, 128]), op=ALU.mult),
               r=[tg + "sg"], w=[DK])
        yield
        for c in range(nch):
            w_ = min(512, n - c * 512)
            last = (c == nch - 1)

            def dots(h, c=c, w_=w_):
                half, slot = h % 2, h // 2
                pd = ps[h % 2]
                sch.op("pe", lambda e, pd=pd, half=half, slot=slot: e.matmul(
                    pd[:, 0:w_], qiT3[half * 64:(half + 1) * 64, slot, :],
                    kiT[half * 64:(half + 1) * 64, c * 512:c * 512 + w_], start=True, stop=True),
                    r=[tg + "qiT"], w=["ps%d" % (h % 2)])
                rb = rbuf[h % 4]
                if h % 2 == 0:
                    sch.op("act", lambda e, pd=pd, rb=rb: e.activation(out=rb[:, 0:w_], in_=pd[:, 0:w_], func=AF.Relu),
                           r=["ps%d" % (h % 2)], w=["rbuf%d" % (h % 4)])
                else:
                    sch.op("dve", lambda e, pd=pd, rb=rb: e.tensor_scalar(out=rb[:, 0:w_], in0=pd[:, 0:w_], scalar1=0.0,
                                                                          scalar2=None, op0=ALU.max),
                           r=["ps%d" % (h % 2)], w=["rbuf%d" % (h % 4)])

            dots(0)
            dots(1)
            for h in range(16):
                rb = rbuf[h % 4]
                sch.op("pe", lambda e, h=h, rb=rb, w_=w_: e.matmul(ps[2][:, 0:w_], Dm[:, h, :], rb[:, 0:w_],
                                                                  start=(h == 0), stop=(h == 15)),
                       r=["rbuf%d" % (h % 4), DK], w=["ps2"])
                if h + 2 < 16:
                    dots(h + 2)
                yield
            sch.op("dve", lambda e, c=c, w_=w_: e.tensor_reduce(out=am[:, c:c + 1], in_=ps[2][:, 0:w_], axis=AX.X, op=ALU.max,
                                                                apply_absolute_value=True), r=["ps2"], w=[AK])
            if last:
                if w_ > 256:
                    sch.op("dve", lambda e, c=c, w_=w_: e.tensor_copy(out=scores[:, c * 512:c * 512 + w_ - 256],
                                                                      in_=ps[2][:, 0:w_ - 256]),
                           r=["ps2"], w=[SK])
                sch.op("dve", lambda e, c=c, w_=w_: e.tensor_tensor(out=scores[:, c * 512 + w_ - 256:c * 512 + w_],
                                                                    in0=ps[2][:, w_ - 256:w_], in1=cmask[:], op=ALU.add),
                       r=["ps2"], w=[SK])
            else:
                sch.op("dve", lambda e, c=c: e.tensor_copy(out=scores[:, c * 512:(c + 1) * 512], in_=ps[2][:]),
                       r=["ps2"], w=[SK])
            yield

    def gen_bisect(j):
        nkb, n, nch = geom(j)
        t2 = j % 2
        scores = scoresb[t2]
        am = amb[t2]
        SK, AK = "scores%d" % t2, "am%d" % t2
        sch.op("dve", lambda e: e.tensor_reduce(out=bst[:, 0:1], in_=am[:, 0:nch], axis=AX.X, op=ALU.max),
               r=[AK], w=["b_am0"])
        sch.op("dve", lambda e: e.tensor_scalar(out=bst[:, 0:1], in0=bst[:, 0:1], scalar1=1.001, scalar2=1e-6, op0=ALU.mult,
                                                op1=ALU.add), r=["b_am0"], w=["b_am"])
        sch.op("dve", lambda e: e.tensor_scalar(out=bst[:, 1:2], in0=bst[:, 0:1], scalar1=-1.0, scalar2=None, op0=ALU.mult),
               r=["b_am"], w=["b_lo"])
        yield
        thr_cnt = 512.0 - n - 0.5
        for it in range(NBIS):
            sc_ = 2.0 ** (-it)
            sch.op("dve", lambda e, sc_=sc_: e.scalar_tensor_tensor(out=bst[:, 2:3], in0=bst[:, 0:1], scalar=-sc_,
                                                                    in1=bst[:, 1:2], op0=ALU.mult, op1=ALU.subtract),
                   r=["b_am", "b_lo"], w=["b_nm"])
            sch.op("act", lambda e: e.activation(out=sjunk[:, 0:n], in_=scores[:, 0:n], func=AF.Sign, bias=bst[:, 2:3],
                                                 scale=1.0, accum_out=bst[:, 3:4]),
                   r=[SK, "b_nm"], w=["b_cnt", "sjunk"])
            sch.op("dve", lambda e, sc_=sc_: e.tensor_scalar(out=bst[:, 4:5], in0=bst[:, 3:4], scalar1=thr_cnt, scalar2=sc_,
                                                             op0=ALU.is_ge, op1=ALU.mult), r=["b_cnt"], w=["b_c2"])
            sch.op("dve", lambda e: e.scalar_tensor_tensor(out=bst[:, 1:2], in0=bst[:, 4:5], scalar=bst[:, 0:1],
                                                           in1=bst[:, 1:2], op0=ALU.mult, op1=ALU.add),
                   r=["b_c2", "b_am", "b_lo"], w=["b_lo"])
            yield

    def emit_masks(j):
        nkb, n, nch = geom(j)
        scores = scoresb[j % 2]
        SK = "scores%d" % (j % 2)
        sch.op("dve", lambda e: e.tensor_scalar(out=m01[:, 0:n], in0=scores[:, 0:n], scalar1=bst[:, 1:2], scalar2=None,
                                                op0=ALU.is_ge), r=[SK, "b_lo"], w=["m01"])
        for b0 in range(0, nkb, 8):
            nb_ = min(8, nkb - b0)

            def trm(e, b0=b0, nb_=nb_):
                ins = None
                for i in range(nb_):
                    ins = e.transpose(pTm[:, i, :], m01[:, (b0 + i) * 128:(b0 + i + 1) * 128], ident_b[:])
                return ins
            sch.op("pe", trm, r=["m01"], w=["ps7"])
            sch.op("act", lambda e, b0=b0, nb_=nb_: e.activation(out=mT[:, b0:b0 + nb_, :], in_=pTm[:, 0:nb_, :],
                                                               func=AF.Copy), r=["ps7"], w=["mT"])

    def gen_main(j):
        nkb, n, nch = geom(j)
        t3, t2 = j % 3, j % 2
        tg = "p3_%d" % t3
        qz3 = V3(qzb[t3][:], 16)
        at = attn[t2]
        at3 = V3(at[:], 16)
        ATK = "attn%d" % t2
        for g in range(4):
            pr = g // 2
            qg = qz3[:, 4 * g:4 * g + 4, :]

            def qk(kb, g=g, qg=qg, pr=pr):
                pq = ps[3 + kb % 2]
                r_ = min(2 * j + 1 - kb, 3)

                def mmqk(e):
                    e.matmul(pq[:], kT[:, pr, kb * 128:(kb + 1) * 128], qg, start=True, stop=False)
                    return e.matmul(pq[:], ident_b[:], bT4[:, r_, 4 * g:4 * g + 4, :], start=False, stop=True)
                sch.op("pe", mmqk, r=[tg + "qT"], w=["ps%d" % (3 + kb % 2)])

            qk(0)
            for kb in range(nkb):
                if kb + 1 < nkb:
                    qk(kb + 1)
                pq = ps[3 + kb % 2]
                pqk = "ps%d" % (3 + kb % 2)
                pbf = pbuf[kb % 3]
                pbk = "pbuf%d" % (kb % 3)
                sch.op("act", lambda e, pq=pq, pbf=pbf: e.activation(out=pbf[:], in_=pq[:], func=AF.Exp), r=[pqk], w=[pbk])
                sch.op("dve", lambda e, pbf=pbf, kb=kb: e.tensor_tensor(
                    out=V3(pbf[:], 4), in0=V3(pbf[:], 4), in1=mT[:, kb, :].unsqueeze(1).to_broadcast([128, 4, 128]),
                    op=ALU.mult), r=[pbk, "mT"], w=[pbk])

                def mmpv(e, pbf=pbf, kb=kb, g=g):
                    e.matmul(ps[5][:], Vt[:, kb, g * 64:g * 64 + 128], pbf[:], start=(kb == 0), stop=(kb == nkb - 1))
                    return e.matmul(ps[6][:], ones_b[:], pbf[:], start=(kb == 0), stop=(kb == nkb - 1))
                sch.op("pe", mmpv, r=[pbk], w=["ps5", "ps6"])
                yield
            sch.op("dve", lambda e: e.reciprocal(out=rl[0:64, :], in_=ps[6][0:64, :]), r=["ps6"], w=["rl"])
            sch.op("dve", lambda e, g=g: e.tensor_tensor(out=at3[0:64, 4 * g:4 * g + 4, :], in0=V3(ps[5][0:64, :], 4),
                                                         in1=V3(rl[0:64, :], 4), op=ALU.mult),
                   r=["ps5", "rl"], w=[ATK])
            yield
        sch.dma("sp", attn_s[j], at[0:64, :], r=[ATK], w=["attn_s"])
        yield

    def run_all(g):
        for _ in g:
            pass

    def take(g, k):
        if g is None:
            return None
        for _ in range(k):
            try:
                next(g)
            except StopIteration:
                return None
        return g

    run_all(gen_indexer(0))
    gM = None
    for j in range(NQ):
        gB = gen_bisect(j)
        gI = gen_indexer(j + 1) if j + 1 < NQ else None
        lenI = (17 * geom(j + 1)[2] + 1) if j + 1 < NQ else 0
        lenM = (4 * geom(j - 1)[0] + 5) if j >= 1 else 0
        kI = (lenI + NBIS - 1) // NBIS
        kM = (lenM + NBIS - 1) // NBIS
        while gB is not None:
            gB = take(gB, 1)
            gI = take(gI, kI)
            gM = take(gM, kM)
        if gI is not None:
            run_all(gI)
        if gM is not None:
            run_all(gM)
        emit_masks(j)
        gM = gen_main(j)
    run_all(gM)
    sch.barrier()
    if "stop3" in dbg:
        return nc, sch, es

    areset()
    attnT = V3(aview(16 * 512, BF16), 16)
    mcT = aview(16 * 512, BF16)
    sgbT = aview(16 * 512, BF16)
    tmpb = aview(512, BF16)
    xt4 = aview(4 * D, F32)
    tmpf = aview(512, F32)
    g1_bc = aview(D, F32)
    sch.dma("sp", g1_bc[:], mod_flat[:, 32 * 128:48 * 128].partition_broadcast(128), w=["g1_bc"])
    for tl in range(NT):
        for blk in range(4):
            sch.dma("sp", attnT[0:64, :, blk * 128:(blk + 1) * 128],
                    attn_s[tl * 4 + blk].rearrange("p (h t) -> p h t", h=16), w=["attnT"])
        sch.dma("sp", mcT[:], mc_s[tl], w=["mcT"])
        sch.dma("sp", sgbT[:], sgb_s[tl], w=["sgbT"])
        sch.dma("sp", V3(xt4[:], 4), xo[tl * 512:(tl + 1) * 512, :].rearrange("(b p) n -> p b n", p=128), w=["xt4"])
        for c4 in range(4):
            i = rstate["i"]
            rstate["i"] += 1
            buf = ring[i % len(ring)]
            key = ("ring", i % len(ring))
            sch.dma("pool", V3(buf[0:64, :], 16), wao_d[:, c4 * 512:(c4 + 1) * 512].rearrange("(h p) n -> p h n", p=64), w=[key])
            b3 = V3(buf[:], 16)
            for q4 in range(4):
                cc = c4 * 4 + q4
                cs = slice(q4 * 128, (q4 + 1) * 128)
                pp = ps[cc % 2]
                pk = "ps%d" % (cc % 2)

                def mma(e, cs=cs, pp=pp, b3=b3):
                    ins = None
                    for h in range(16):
                        ins = e.matmul(pp[:], b3[0:64, h, cs], attnT[0:64, h, :], start=(h == 0), stop=(h == 15))
                    return ins
                sch.op("pe", mma, r=[key, "attnT"], w=[pk])
                sch.op("dve", lambda e, cc=cc, pp=pp: e.tensor_tensor(out=tmpb[:], in0=pp[:], in1=sgbT[:, cc * 512:(cc + 1) * 512],
                                                                     op=ALU.mult), r=[pk, "sgbT"], w=["tmpb"])
                sch.op("dve", lambda e, cc=cc: e.tensor_tensor(out=mcT[:, cc * 512:(cc + 1) * 512], in0=mcT[:, cc * 512:(cc + 1) * 512],
                                                               in1=tmpb[:], op=ALU.add), r=["tmpb", "mcT"], w=["mixT"])
        mx3 = V3(mcT[:], 16)
        for c4 in range(4):
            buf, key = wchunk(wo_d, c4 * 512)
            b3 = V3(buf[:], 16)
            for blk in range(4):
                pp = ps[2 + blk % 2]
                pk = "ps%d" % (2 + blk % 2)

                def mmo(e, blk=blk, pp=pp, b3=b3):
                    ins = None
                    for k in range(KC):
                        ins = e.matmul(pp[:], mx3[:, k, blk * 128:(blk + 1) * 128], b3[:, k, :], start=(k == 0), stop=(k == KC - 1))
                    return ins
                sch.op("pe", mmo, r=[key, "mixT", "mcT"], w=[pk])
                xs = xt4[:, blk * D + c4 * 512: blk * D + (c4 + 1) * 512]
                sch.op("dve", lambda e, pp=pp, c4=c4: e.tensor_tensor(out=tmpf[:], in0=pp[:], in1=g1_bc[:, c4 * 512:(c4 + 1) * 512],
                                                                     op=ALU.mult), r=[pk, "g1_bc"], w=["tmpf"])
                sch.op("dve", lambda e, xs=xs: e.tensor_tensor(out=xs, in0=xs, in1=tmpf[:], op=ALU.add), r=["tmpf", "xt4"], w=["x1t"])
        sch.dma("sp", x1_s[tl * 512:(tl + 1) * 512, :].rearrange("(b p) n -> p b n", p=128), V3(xt4[:], 4),
                r=["x1t", "xt4"], w=["x1_s"])
    sch.barrier()
    if "stop4" in dbg:
        return nc, sch, es

    areset()
    xb2 = [aview(D, F32) for _ in range(2)]
    acc = aview(4 * D, F32)
    h2T = V3(aview(16 * 512, BF16), 16)
    xnb = aview(D, BF16)
    stt = aview(8, F32)
    wr_f = V3(aview(16 * E, F32), 16)
    wr2 = V3(aview(16 * E, F32), 16)
    brow = aview(E, F32)
    rt = aview(8 * E, F32)
    comb = aview(4 * (E + 1) + 4, F32)
    g2_bc = aview(D, F32)
    XBASE = aoff[0]
    xnf = aview(D, F32)
    xnT = V3(aview(16 * 128, F32), 16)
    aoff[0] = XBASE
    sil = [aview(512, F32) for _ in range(2)]
    gT = [V3(aview(4 * 512, BF16), 4) for _ in range(2)]
    comb3 = V3(comb[:, 0:4 * (E + 1)], 4)
    sch.dma("sp", g2_bc[:], mod_flat[:, 80 * 128:96 * 128].partition_broadcast(128), w=["g2_bc"])
    sch.dma("sp", wr_f, wr_d.rearrange("(k p) n -> p k n", p=128), w=["wr_f"])
    sch.op("dve", lambda e: e.tensor_tensor(out=wr2, in0=wr_f, in1=a2[:].unsqueeze(2).to_broadcast([128, 16, E]), op=ALU.mult),
           r=["wr_f", "a2"], w=["wr2"])

    def mmb(e):
        ins = None
        for k in range(KC):
            ins = e.matmul(ps[7][0:1, 0:E], sh2[:, k:k + 1], wr_f[:, k, :], start=(k == 0), stop=(k == KC - 1))
        return ins
    sch.op("pe", mmb, r=["wr_f", "modT"], w=["ps7"])
    sch.op("dve", lambda e: e.tensor_copy(out=brow[0:1, :], in_=ps[7][0:1, 0:E]), r=["ps7"], w=["brow"])
    sch.op("pool", lambda e: e.memset(comb[:], 1.0), w=["comb"])
    sch.barrier()

    for tl in range(NT):
        tg = "p5_"
        for blk in range(4):
            xs_t = xb2[blk % 2]
            xs = xs_t[:]
            sch.dma("sp", xs, x1_s[(tl * 4 + blk) * 128:(tl * 4 + blk + 1) * 128, :], w=[tg + "x"])
            norm_T(tg, xs, 128, a2, sh2, h2T[:, :, blk * 128:(blk + 1) * 128], xnb, stt, pTb, xn_f32=xnf)
            for hf in range(2):
                def trf(e, hf=hf):
                    ins = None
                    for k in range(8):
                        kk = hf * 8 + k
                        ins = e.transpose(ps[hf * 2 + k // 4][:, (k % 4) * 128:(k % 4 + 1) * 128], xnf[:, kk * 128:(kk + 1) * 128],
                                          ident_f[:])
                    return ins
                sch.op("pe", trf, r=[tg + "xnf", "ident_f"], w=["ps%d" % (hf * 2), "ps%d" % (hf * 2 + 1)])
                for q in range(2):
                    pi = hf * 2 + q
                    sch.op("act", lambda e, pi=pi: e.activation(out=xnT[:, pi * 4:(pi + 1) * 4, :], in_=V3(ps[pi][:], 4), func=AF.Copy),
                           r=["ps%d" % pi], w=["xnT%d" % pi])

            def mmr(e):
                for k in range(KC):
                    e.matmul(ps[4][:, 0:E], xnT[:, k, :], wr2[:, k, :], start=(k == 0), stop=False)
                return e.matmul(ps[4][:, 0:E], ones_f[0:1, :], brow[0:1, :], start=False, stop=True)
            sch.op("pe", mmr, r=["xnT0", "xnT1", "xnT2", "xnT3", "wr2", "brow", "ones_f"], w=["ps4"])
            R = lambda i: rt[:, i * E:(i + 1) * E]
            sch.op("act", lambda e: e.activation(out=R(0), in_=ps[4][:, 0:E], func=AF.Sigmoid), r=["ps4"], w=["r0"])
            sch.op("dve", lambda e: e.tensor_tensor(out=R(1), in0=R(0), in1=rbias[:], op=ALU.add), r=["r0", "rbias"], w=["r1"])
            g3 = V3(R(1), 8)
            sch.op("dve", lambda e: e.tensor_reduce(out=R(7)[:, 0:8], in_=g3, axis=AX.X, op=ALU.max), r=["r1"], w=["m1"])
            sch.op("dve", lambda e: e.tensor_tensor(out=V3(R(2), 8), in0=g3, in1=R(7)[:, 0:8].unsqueeze(2).to_broadcast([128, 8, 8]),
                                                    op=ALU.is_equal), r=["r1", "m1"], w=["r2"])
            sch.op("dve", lambda e: e.scalar_tensor_tensor(out=R(2), in0=R(2), scalar=-BIG, in1=R(1), op0=ALU.mult, op1=ALU.add),
                   r=["r2", "r1"], w=["r2b"])
            sch.op("dve", lambda e: e.tensor_reduce(out=R(7)[:, 8:16], in_=V3(R(2), 8), axis=AX.X, op=ALU.max), r=["r2b"], w=["m2"])
            sch.op("dve", lambda e: e.tensor_tensor(out=R(7)[:, 16:24], in0=R(7)[:, 0:8], in1=R(7)[:, 8:16], op=ALU.add),
                   r=["m1", "m2"], w=["gs"])
            sch.op("dve", lambda e: e.max(out=R(7)[:, 24:32], in_=R(7)[:, 16:24]), r=["gs"], w=["gsort"])
            sch.op("dve", lambda e: e.tensor_scalar(out=R(7)[:, 32:40], in0=R(7)[:, 16:24], scalar1=R(7)[:, 27:28], scalar2=None,
                                                    op0=ALU.is_ge), r=["gs", "gsort"], w=["gmask"])
            sch.op("dve", lambda e: e.tensor_tensor(out=V3(R(3), 8), in0=g3, in1=R(7)[:, 32:40].unsqueeze(2).to_broadcast([128, 8, 8]),
                                                    op=ALU.mult), r=["r1", "gmask"], w=["r3"])
            sch.op("dve", lambda e: e.tensor_scalar(out=R(7)[:, 40:48], in0=R(7)[:, 32:40], scalar1=-1.0, scalar2=BIG,
                                                    op0=ALU.add, op1=ALU.mult), r=["gmask"], w=["gneg"])
            sch.op("dve", lambda e: e.tensor_tensor(out=V3(R(3), 8), in0=V3(R(3), 8),
                                                    in1=R(7)[:, 40:48].unsqueeze(2).to_broadcast([128, 8, 8]), op=ALU.add),
                   r=["r3", "gneg"], w=["r3b"])
            sch.op("dve", lambda e: e.max(out=R(7)[:, 48:56], in_=R(3)), r=["r3b"], w=["esort"])
            sch.op("dve", lambda e: e.tensor_scalar(out=R(4), in0=R(3), scalar1=R(7)[:, 55:56], scalar2=None, op0=ALU.is_ge),
                   r=["r3b", "esort"], w=["r4"])
            sch.op("dve", lambda e: e.tensor_tensor(out=R(5), in0=R(4), in1=R(0), op=ALU.mult), r=["r4", "r0"], w=["r5"])
            sch.op("dve", lambda e: e.tensor_reduce(out=R(7)[:, 56:57], in_=R(5), axis=AX.X, op=ALU.add), r=["r5"], w=["den"])
            sch.op("dve", lambda e: e.reciprocal(out=R(7)[:, 57:58], in_=R(7)[:, 56:57]), r=["den"], w=["rden"])
            sch.op("dve", lambda e, blk=blk: e.tensor_scalar(out=comb3[:, blk, 0:E], in0=R(5), scalar1=R(7)[:, 57:58], scalar2=2.5,
                                                             op0=ALU.mult, op1=ALU.mult), r=["r5", "rden", "comb"], w=["comb"])
        sch.barrier()
        acc3 = V3(acc[:], 4)
        def load_e(e_):
            if e_ < E:
                s1, s3, s2 = w1_d[e_], w3_d[e_], w2_d[e_]
            else:
                s1, s3, s2 = ws1_d, ws3_d, ws2_d
            b1, k1 = wchunk(s1, 0)
            b3_, k3 = wchunk(s3, 0)
            i = rstate["i"]
            rstate["i"] += 1
            b2 = ring[i % len(ring)]
            k2 = ("ring", i % len(ring))
            sch.dma("pool", V3(b2[:], 4), s2.rearrange("(k p) n -> p k n", p=128), w=[k2], max_dma_last_dim=8192)
            return dict(w1v=V3(b1[:], 16), w3v=V3(b3_[:], 16), w2v=V3(b2[:], 4), k1=k1, k3=k3, k2=k2)

        def emit_H(e_, W, fs):
            gt = gT[e_ % 2]
            gk = "gT%d" % (e_ % 2)
            w1v, w3v = W["w1v"], W["w3v"]
            for f in fs:
                pa, pb2 = ps[(f % 2) * 2], ps[(f % 2) * 2 + 1]
                ka, kb2 = "ps%d" % ((f % 2) * 2), "ps%d" % ((f % 2) * 2 + 1)

                def mmh(e, f=f, pa=pa, pb2=pb2):
                    ins = None
                    for k in range(KC):
                        ins = e.matmul(pa[:], w1v[:, k, f * 128:(f + 1) * 128], h2T[:, k, :], start=(k == 0), stop=(k == KC - 1))
                    for k in range(KC):
                        ins = e.matmul(pb2[:], w3v[:, k, f * 128:(f + 1) * 128], h2T[:, k, :], start=(k == 0), stop=(k == KC - 1))
                    return ins
                sch.op("pe", mmh, r=[W["k1"], W["k3"]], w=[ka, kb2])
                sl = sil[f % 2]
                sch.op("act", lambda e, pa=pa, sl=sl: e.activation(out=sl[:], in_=pa[:], func=AF.Silu), r=[ka], w=["sil%d" % (f % 2)])
                sch.op("dve", lambda e, pb2=pb2, sl=sl, f=f: e.tensor_tensor(out=gt[:, f, :], in0=pb2[:], in1=sl[:], op=ALU.mult),
                       r=[kb2, "sil%d" % (f % 2)], w=[gk + "_%d" % f])

        def emit_Y(e_, W):
            gt = gT[e_ % 2]
            gk = "gT%d" % (e_ % 2)
            w2v = W["w2v"]
            for blk in range(4):
                for c4 in range(4):
                    pi = 4 + (blk * 4 + c4) % 4
                    px, pk = ps[pi], "ps%d" % pi

                    def mmy(e, blk=blk, c4=c4, px=px):
                        ins = None
                        for f in range(4):
                            ins = e.matmul(px[:], gt[:, f, blk * 128:(blk + 1) * 128], w2v[:, f, c4 * 512:(c4 + 1) * 512],
                                           start=(f == 0), stop=(f == 3))
                        return ins
                    sch.op("pe", mmy, r=[gk + "_0", gk + "_1", gk + "_2", gk + "_3", W["k2"]], w=[pk])
                    av = acc3[:, blk, c4 * 512:(c4 + 1) * 512]
                    if e_ == 0:
                        sch.op("dve", lambda e, px=px, av=av, blk=blk: e.tensor_scalar(
                            out=av, in0=px[:], scalar1=comb3[:, blk, e_:e_ + 1], scalar2=None, op0=ALU.mult),
                            r=[pk], w=["acc"])
                    else:
                        sch.op("dve", lambda e, px=px, av=av, blk=blk: e.scalar_tensor_tensor(
                            out=av, in0=px[:], scalar=comb3[:, blk, e_:e_ + 1], in1=av, op0=ALU.mult, op1=ALU.add),
                            r=[pk, "acc"], w=["acc"])

        Wc = load_e(0)
        emit_H(0, Wc, range(4))
        for e_ in range(E + 1):
            Wn = None
            if e_ + 1 <= E:
                Wn = load_e(e_ + 1)
                emit_H(e_ + 1, Wn, [0])
            emit_Y(e_, Wc)
            if Wn is not None:
                emit_H(e_ + 1, Wn, [1, 2, 3])
            Wc = Wn
        for blk in range(4):
            xs_t = xb2[blk % 2]
            sch.dma("sp", xs_t[:], x1_s[(tl * 4 + blk) * 128:(tl * 4 + blk + 1) * 128, :], w=["xfin%d" % (blk % 2)])
            for c4 in range(4):
                av = acc3[:, blk, c4 * 512:(c4 + 1) * 512]
                xs = xs_t[:, c4 * 512:(c4 + 1) * 512]
                sch.op("dve", lambda e, av=av, c4=c4: e.tensor_tensor(out=av, in0=av, in1=g2_bc[:, c4 * 512:(c4 + 1) * 512], op=ALU.mult),
                       r=["acc"], w=["acc"])
                sch.op("dve", lambda e, av=av, xs=xs: e.tensor_tensor(out=av, in0=av, in1=xs, op=ALU.add),
                       r=["acc", "xfin%d" % (blk % 2)], w=["acc"])
        sch.dma("sp", out_d[tl * 512:(tl + 1) * 512, :].rearrange("(b p) n -> p b n", p=128), acc3, r=["acc"], w=["out"])
        sch.barrier()
    return nc, sch, es


def finish(nc, sch, es):
    from contextlib import ExitStack
    with es:
        sem_names = ["pe", "act", "dve", "pool"]
        sems = {}
        for n_ in sem_names:
            sems[n_] = es.enter_context(nc.semaphore("s_" + n_))
        dpool = {"sp": [es.enter_context(nc.semaphore("dsp%d" % i)) for i in range(12)],
                 "pool": [es.enter_context(nc.semaphore("dpl%d" % i)) for i in range(8)]}
        with nc.Block() as block:
            @block.tensor
            def _(e):
                sch_emit_one(nc, sch, "pe", e, sems, dpool)

            @block.scalar
            def _(e):
                sch_emit_one(nc, sch, "act", e, sems, dpool)

            @block.vector
            def _(e):
                sch_emit_one(nc, sch, "dve", e, sems, dpool)

            @block.gpsimd
            def _(e):
                sch_emit_one(nc, sch, "pool", e, sems, dpool)

            @block.sync
            def _(e):
                sch_emit_one(nc, sch, "sp", e, sems, dpool)
    return nc


_assigned = {}


def _assign_events(sch, sems, dpool):
    if id(sch) in _assigned:
        return
    _assigned[id(sch)] = True
    for e, lst in sch.ops.items():
        cnt = 0
        dcount = {}
        di = 0
        for o in lst:
            if o.is_dma:
                pool = dpool[e]
                s = pool[di % len(pool)]
                di += 1
                c = dcount.get(id(s), 0)
                o.presem = (s, c * 16)
                dcount[id(s)] = c + 1
                o.event = (s, (c + 1) * 16)
            elif o.fn is not None and o.needed:
                cnt += 1
                o.event = (sems[e], cnt)


def sch_emit_one(nc, sch, e, eng, sems, dpool):
    _assign_events(sch, sems, dpool)
    waited = {}
    finals = {}

    def wait(ev):
        s, v = ev
        if waited.get(id(s), 0) < v:
            eng.wait_ge(s, v)
            waited[id(s)] = v

    for o in sch.ops[e]:
        for d in o.deps:
            if d.event is None:
                continue
            if d.eng == e and e == "pe" and not d.is_dma:
                continue
            wait(d.event)
        if o.fn is None:
            continue
        if o.is_dma:
            if o.presem[1] > 0:
                wait(o.presem)
            ins = o.fn(eng)
            ins.then_inc(o.event[0], 16)
            finals[id(o.event[0])] = o.event
        else:
            ins = o.fn(eng)
            if o.event is not None:
                ins.then_inc(o.event[0], 1)
    for ev in finals.values():
        wait(ev)


def make_inputs(core, x, c, rel_bias, norm1_w, norm2_w, w_ada, b_ada, w_in, conv_w, w_conv_out, q_norm_w, k_norm_w,
                idx_k_norm_w, idx_k_norm_b, w_attn_out, w_o, w_router, router_bias, w1, w3, w2, ws1, ws3, ws2):
    b, p = core // 2, core % 2
    f = lambda a: np.ascontiguousarray(a, dtype=np.float32)
    xb = x[b]
    xo = xb.reshape(NBLK, 128, D)[p::2].reshape(OWN * 128, D)
    xh = np.zeros((32, D), np.float32)
    hmask = np.ones((128, 32), np.float32)
    for j in range(OWN):
        st = (2 * j + p) * 128
        if st == 0:
            hmask[:, 0:2] = 0.0
        else:
            xh[2 * j:2 * j + 2] = xb[st - 2:st]
    tri = np.where(np.arange(128)[None, :] <= np.arange(128)[:, None], 0.0, -BIG).astype(np.float32)
    cmask = np.zeros((128, 256), np.float32)
    if p == 0:
        cmask[:, 0:128] = tri
        cmask[:, 128:256] = -BIG
    else:
        cmask[:, 128:256] = tri
    sl = np.arange(128)[:, None]
    tl = np.arange(128)[None, :]
    biasT = np.zeros((128, 4, 16, 128), np.float32)
    for r in range(4):
        delta = p - 1 + r
        if delta < 0:
            continue
        dist = 128 * delta + tl - sl
        bk = t5_bucket_np(dist.astype(np.int32))
        biasT[:, r] = np.transpose(rel_bias[bk], (0, 2, 1))
    return {
        "xa": f(xb), "xo": f(xo), "xh": xh, "hmask": hmask, "cmask": cmask, "biasT": f(biasT.reshape(128, -1)),
        "c": f(c[b].reshape(16, 128)), "norm1_w": f(norm1_w[0].reshape(16, 128)), "norm2_w": f(norm2_w[0].reshape(16, 128)),
        "w_ada": f(w_ada[0]), "b_ada": f(b_ada[0].reshape(96, 128)), "w_in": f(w_in[0]),
        "conv_w": f(conv_w[0].reshape(24, 128)), "w_conv_out": f(w_conv_out[0]),
        "q_norm_w": f(q_norm_w[0].reshape(64, 1)), "k_norm_w": f(k_norm_w[0].reshape(64, 1)),
        "idx_k_norm_w": f(idx_k_norm_w[0].reshape(64, 1)), "idx_k_norm_b": f(idx_k_norm_b[0].reshape(64, 1)),
        "w_attn_out": f(w_attn_out[0]), "w_o": f(w_o[0]), "w_router": f(w_router[0]), "router_bias": f(router_bias[0].reshape(1, E)),
        "w1": f(w1[0]), "w3": f(w3[0]), "w2": f(w2[0]), "ws1": f(ws1[0]), "ws3": f(ws3[0]), "ws2": f(ws2[0]),
    }


def kernel(**inputs):
    inputs = {k: np.asarray(v) for k, v in inputs.items()}
    nc, sch, es = build_program()
    nc = finish(nc, sch, es)
    shared = None
    in_maps = []
    for core in range(8):
        m = make_inputs(core, **inputs)
        if shared is None:
            shared = m
        else:
            for k in ("w_ada", "w_in", "w_conv_out", "w_attn_out", "w_o", "w_router", "w1", "w3", "w2", "ws1", "ws3", "ws2"):
                m[k] = shared[k]
        in_maps.append(m)
    res = run_bass_kernel_spmd(nc, in_maps, core_ids=list(range(8)))
    out = np.zeros((4, S, D), np.float32)
    for core in range(8):
        b, p = core // 2, core % 2
        o = np.asarray(res.results[core]["out"]).reshape(OWN, 128, D)
        out[b].reshape(NBLK, 128, D)[p::2] = o
    return out
```

```python
import math
import numpy as np
import concourse.bass as bass
import concourse.mybir as mybir
from concourse.bass_utils import run_bass_kernel_spmd

F32 = mybir.dt.float32
BF16 = mybir.dt.bfloat16
AF = mybir.ActivationFunctionType
ALU = mybir.AluOpType
AX = mybir.AxisListType

D = 2048
KC = 16
S = 4096
NBLK = 32
OWN = 16
NT = 4
E = 64
FE = 512
BIG = 1.0e30
EPS = 1e-6
NBIS = 18
COMPUTE = ("pe", "act", "dve", "pool")
DEBUG = {}


class Op:
    __slots__ = ("eng", "fn", "deps", "needed", "event", "is_dma", "presem")

    def __init__(self, eng, fn, is_dma=False):
        self.eng, self.fn, self.is_dma = eng, fn, is_dma
        self.deps, self.needed, self.event, self.presem = [], False, None, None


class Sched:
    def __init__(self):
        self.ops = {e: [] for e in ("pe", "act", "dve", "pool", "sp")}
        self.bufw, self.bufr = {}, {}
        self.since = []
        self.limit = None
        self.count = 0

    def op(self, eng, fn, r=(), w=(), is_dma=False):
        o = Op(eng, fn, is_dma)
        self.count += 1
        if self.limit is not None and self.count > self.limit:
            return o
        deps = set()
        for k in r:
            if k in self.bufw:
                deps.add(self.bufw[k])
        for k in w:
            if k in self.bufw:
                deps.add(self.bufw[k])
            deps.update(self.bufr.get(k, ()))
        deps.discard(o)
        o.deps = list(deps)
        for d in o.deps:
            d.needed = True
        for k in r:
            self.bufr.setdefault(k, []).append(o)
        for k in w:
            self.bufw[k] = o
            self.bufr[k] = []
        self.ops[eng].append(o)
        self.since.append(o)
        return o

    def dma(self, q, out, in_, r=(), w=(), **kw):
        return self.op(q, lambda e: e.dma_start(out=out, in_=in_, **kw), r, w, is_dma=True)

    def barrier(self):
        tails = []
        last = {}
        for o in self.since:
            if o.is_dma:
                tails.append(o)
            else:
                last[o.eng] = o
        tails += list(last.values())
        for t in tails:
            t.needed = True
        for e in self.ops:
            o = Op(e, None)
            o.deps = list(tails)
            self.ops[e].append(o)
        self.since = []
        self.bufw, self.bufr = {}, {}


def t5_bucket_np(n):
    n = np.maximum(n, 0)
    nf = np.maximum(n, 1).astype(np.float32)
    large = 16 + (np.log(nf / np.float32(16)) / np.float32(math.log(8.0)) * np.float32(16)).astype(np.int32)
    large = np.minimum(large, 31)
    return np.where(n < 16, n, large)


def build_program(dbg=()):
    nc = bass.Bass("TRN2", target_bir_lowering=False)
    sch = Sched()
    for d_ in dbg:
        if isinstance(d_, str) and d_.startswith("maxops="):
            sch.limit = int(d_.split("=")[1])

    def din(name, shape, dt=F32):
        return nc.dram_tensor(name, list(shape), dt, kind="ExternalInput").ap()

    def dscr(name, shape, dt):
        kind = "ExternalOutput" if name in dbg else "Internal"
        return nc.dram_tensor(name, list(shape), dt, kind=kind).ap()

    xa = din("xa", [S, D])
    xo = din("xo", [OWN * 128, D])
    xh = din("xh", [32, D])
    hmask_d = din("hmask", [128, 32])
    cmask_d = din("cmask", [128, 256])
    biasT_d = din("biasT", [128, 4 * 16 * 128])
    c_d = din("c", [16, 128])
    n1_d = din("norm1_w", [16, 128])
    n2_d = din("norm2_w", [16, 128])
    wada_d = din("w_ada", [D, 6 * D])
    bada_d = din("b_ada", [96, 128])
    win_d = din("w_in", [D, 9808])
    convw_d = din("conv_w", [24, 128])
    wco_d = din("w_conv_out", [1024, D])
    qnw_d = din("q_norm_w", [64, 1])
    knw_d = din("k_norm_w", [64, 1])
    ikw_d = din("idx_k_norm_w", [64, 1])
    ikb_d = din("idx_k_norm_b", [64, 1])
    wao_d = din("w_attn_out", [1024, D])
    wo_d = din("w_o", [D, D])
    wr_d = din("w_router", [D, E])
    rb_d = din("router_bias", [1, E])
    w1_d = din("w1", [E, D, FE])
    w3_d = din("w3", [E, D, FE])
    w2_d = din("w2", [E, FE, D])
    ws1_d = din("ws1", [D, FE])
    ws3_d = din("ws3", [D, FE])
    ws2_d = din("ws2", [FE, D])
    out_d = nc.dram_tensor("out", [OWN * 128, D], F32, kind="ExternalOutput").ap()

    qT_s = dscr("qT_s", [OWN, 128, 8 * 128], BF16)
    qiT_s = dscr("qiT_s", [OWN, 128, 8 * 128], BF16)
    sgn_s = dscr("sgn_s", [OWN, 128, 16], F32)
    mc_s = dscr("mc_s", [NT, 128, 16 * 512], BF16)
    sgb_s = dscr("sgb_s", [NT, 128, 16 * 512], BF16)
    attn_s = dscr("attn_s", [OWN, 64, 16 * 128], BF16)
    x1_s = dscr("x1_s", [OWN * 128, D], F32)
    mod_s = dscr("mod_s", [96, 128], F32)

    from contextlib import ExitStack
    es = ExitStack()

    def sb(name, shape, dt):
        return es.enter_context(nc.sbuf_tensor(name, list(shape), dt))

    def pst(name, shape, dt):
        return es.enter_context(nc.psum_tensor(name, list(shape), dt))

    ident_b = sb("ident_b", [128, 128], BF16)
    ident_f = sb("ident_f", [128, 128], F32)
    ones_b = sb("ones_b", [128, 128], BF16)
    ones_f = sb("ones_f", [1, 128], F32)
    modT = sb("modT", [128, 96], F32)
    a1 = sb("a1", [128, 16], F32)
    a2 = sb("a2", [128, 16], F32)
    colv = sb("colv", [128, 8], F32)
    convw = sb("convw", [128, 24], F32)
    hmask = sb("hmask_t", [128, 32], F32)
    cmask = sb("cmask_t", [128, 256], F32)
    rbias = sb("rbias", [128, E], F32)
    ring = [sb(f"ring{i}", [128, 16 * 512], BF16) for i in range(6)]
    ARENA = 53000
    arena = sb("arena", [128, ARENA], BF16)
    ps = [pst(f"ps{i}", [128, 512], F32) for i in range(8)]
    psb = [p[:].bitcast(BF16) for p in ps]

    aoff = [0]

    def aview(n_elems, dt):
        nb = n_elems * (2 if dt == F32 else 1)
        nb = (nb + 1) // 2 * 2
        o = aoff[0]
        assert o + nb <= ARENA, (o, nb)
        aoff[0] = o + nb
        v = arena[:, o:o + nb]
        return v.bitcast(F32) if dt == F32 else v

    def areset():
        aoff[0] = 0

    rstate = {"i": 0}

    def wload(parts):
        i = rstate["i"]
        rstate["i"] += 1
        buf = ring[i % len(ring)]
        key = ("ring", i % len(ring))
        for dst_fn, src in parts:
            sch.dma("pool", dst_fn(buf), src, w=[key])
        return buf, key

    def wchunk(wd, c0, ncols=512, kc=KC):
        src = wd[:, c0:c0 + ncols].rearrange("(k p) n -> p k n", p=128)
        return wload([(lambda b: b[:, 0:kc * ncols].rearrange("p (k n) -> p k n", k=kc), src)])

    V3 = lambda ap, k: ap.rearrange("p (k n) -> p k n", k=k)

    sch.op("pool", lambda e: e.memset(ident_b[:], 0.0), w=["ident_b0"])
    sch.op("pool", lambda e: e.affine_select(out=ident_b[:], in_=ident_b[:], pattern=[[-1, 128]],
                                             compare_op=ALU.not_equal, fill=1.0, base=0, channel_multiplier=1),
           r=["ident_b0"], w=["ident_b"])
    sch.op("pool", lambda e: e.memset(ident_f[:], 0.0), w=["ident_f0"])
    sch.op("pool", lambda e: e.affine_select(out=ident_f[:], in_=ident_f[:], pattern=[[-1, 128]],
                                             compare_op=ALU.not_equal, fill=1.0, base=0, channel_multiplier=1),
           r=["ident_f0"], w=["ident_f"])
    sch.op("pool", lambda e: e.memset(ones_b[:], 1.0), w=["ones_b"])
    sch.op("pool", lambda e: e.memset(ones_f[:], 1.0), w=["ones_f"])
    sch.dma("sp", hmask[:], hmask_d, w=["hmask"])
    sch.dma("sp", cmask[:], cmask_d, w=["cmask"])
    sch.dma("sp", rbias[:], rb_d.partition_broadcast(128), w=["rbias"])
    sch.dma("sp", colv[0:64, 3:4], qnw_d, w=["colv_raw"])
    sch.dma("sp", colv[64:128, 3:4], qnw_d, w=["colv_raw"])
    sch.dma("sp", colv[0:64, 4:5], knw_d, w=["colv_raw"])
    sch.dma("sp", colv[64:128, 4:5], knw_d, w=["colv_raw"])
    sch.dma("sp", colv[0:64, 1:2], ikw_d, w=["colv_raw"])
    sch.dma("sp", colv[64:128, 1:2], ikw_d, w=["colv_raw"])
    sch.dma("sp", colv[0:64, 2:3], ikb_d, w=["colv_raw"])
    sch.dma("sp", colv[64:128, 2:3], ikb_d, w=["colv_raw"])
    sch.op("dve", lambda e: e.scalar_tensor_tensor(out=colv[:, 0:1], in0=colv[:, 3:4], scalar=0.125, in1=colv[:, 4:5],
                                                   op0=ALU.mult, op1=ALU.mult), r=["colv_raw"], w=["colv"])

    areset()
    rows = aview(128, F32)
    silu_c = aview(16, BF16)
    tmpc = aview(96, F32)

    def vec_to_cols(src_d, n, dst_key, psum_ap):
        sch.dma("sp", rows[0:n, :], src_d, w=["rows"])
        sch.op("pe", lambda e: e.transpose(psum_ap, rows[0:n, :], ident_f[0:n, 0:n]), r=["rows", "ident_f"], w=[dst_key])

    vec_to_cols(c_d, 16, "ps0", ps[0][:, 0:16])
    sch.op("act", lambda e: e.activation(out=silu_c[:], in_=ps[0][:, 0:16], func=AF.Silu), r=["ps0"], w=["silu_c"])
    vec_to_cols(n1_d, 16, "ps1", ps[1][:, 0:16])
    sch.op("dve", lambda e: e.tensor_copy(out=a1[:], in_=ps[1][:, 0:16]), r=["ps1"], w=["a1raw"])
    vec_to_cols(n2_d, 16, "ps1", ps[1][:, 0:16])
    sch.op("dve", lambda e: e.tensor_copy(out=a2[:], in_=ps[1][:, 0:16]), r=["ps1"], w=["a2raw"])
    vec_to_cols(bada_d, 96, "ps2", ps[2][:, 0:96])
    sch.op("dve", lambda e: e.tensor_copy(out=tmpc[:], in_=ps[2][:, 0:96]), r=["ps2"], w=["tmpc"])
    vec_to_cols(convw_d, 24, "ps1", ps[1][:, 0:24])
    sch.op("dve", lambda e: e.tensor_copy(out=convw[:], in_=ps[1][:, 0:24]), r=["ps1"], w=["convw"])
    for c in range(24):
        buf, key = wchunk(wada_d, c * 512)
        b3 = V3(buf[:], 16)

        def mm(e, b3=b3, c=c):
            ins = None
            for q in range(4):
                ch = c * 4 + q
                for k in range(KC):
                    ins = e.matmul(ps[3][:, ch:ch + 1], b3[:, k, q * 128:(q + 1) * 128], silu_c[:, k:k + 1],
                                   start=(k == 0), stop=(k == KC - 1))
            return ins
        sch.op("pe", mm, r=[key, "silu_c"], w=["ps3"])
    sch.op("dve", lambda e: e.tensor_tensor(out=modT[:], in0=ps[3][:, 0:96], in1=tmpc[:], op=ALU.add),
           r=["ps3", "tmpc"], w=["modT"])
    sch.op("dve", lambda e: e.scalar_tensor_tensor(out=a1[:], in0=modT[:, 16:32], scalar=1.0, in1=a1[:],
                                                   op0=ALU.add, op1=ALU.mult), r=["modT", "a1raw"], w=["a1"])
    sch.op("dve", lambda e: e.scalar_tensor_tensor(out=a2[:], in0=modT[:, 64:80], scalar=1.0, in1=a2[:],
                                                   op0=ALU.add, op1=ALU.mult), r=["modT", "a2raw"], w=["a2"])
    sh1 = modT[:, 0:16]
    sh2 = modT[:, 48:64]
    sch.op("pe", lambda e: e.transpose(ps[0][0:96, 0:128], modT[:], ident_f[:]), r=["modT", "ident_f"], w=["ps0"])
    sch.op("dve", lambda e: e.tensor_copy(out=rows[0:96, :], in_=ps[0][0:96, 0:128]), r=["ps0"], w=["rows"])
    sch.dma("sp", mod_s, rows[0:96, :], r=["rows"], w=["mod_s"])
    mod_flat = mod_s.rearrange("a b -> (a b)").rearrange("(o n) -> o n", o=1)
    sch.barrier()

    def norm_T(tag, xt, n, acol, shcol, hT_dst, xnb, st, pT, xn_f32=None):
        sch.op("act", lambda e: e.activation(out=xnb[0:n, :], in_=xt, func=AF.Square, accum_out=st[0:n, 0:1]),
               r=[tag + "x"], w=[tag + "xnb", tag + "st0"])
        sch.op("dve", lambda e: e.tensor_scalar(out=st[0:n, 1:2], in0=st[0:n, 0:1], scalar1=1.0 / D, scalar2=EPS,
                                                op0=ALU.mult, op1=ALU.add), r=[tag + "st0"], w=[tag + "st1"])
        sch.op("act", lambda e: e.activation(out=st[0:n, 2:3], in_=st[0:n, 1:2], func=AF.Sqrt),
               r=[tag + "st1"], w=[tag + "st2"])
        sch.op("dve", lambda e: e.reciprocal(out=st[0:n, 3:4], in_=st[0:n, 2:3]), r=[tag + "st2"], w=[tag + "st3"])
        if xn_f32 is not None:
            sch.op("dve", lambda e: e.tensor_scalar(out=xn_f32[0:n, :], in0=xt, scalar1=st[0:n, 3:4], scalar2=None,
                                                    op0=ALU.mult), r=[tag + "x", tag + "st3"], w=[tag + "xnf"])
        sch.op("dve", lambda e: e.tensor_scalar(out=xnb[0:n, :], in0=xt, scalar1=st[0:n, 3:4], scalar2=None,
                                                op0=ALU.mult), r=[tag + "x", tag + "st3"], w=[tag + "xnb"])
        pT3 = [V3(pT[0], 8), V3(pT[1], 8)]

        def tr(e):
            ins = None
            for k in range(KC):
                ins = e.transpose(pT3[k // 8][:, k % 8, 0:n], xnb[0:n, k * 128:(k + 1) * 128], ident_b[0:n, 0:n])
            return ins
        sch.op("pe", tr, r=[tag + "xnb", "ident_b"], w=["pT0", "pT1"])
        for hf in range(2):
            sch.op("dve", lambda e, hf=hf: e.tensor_tensor(
                out=hT_dst[:, hf * 8:(hf + 1) * 8, :], in0=pT3[hf][:, :, 0:n],
                in1=acol[:, hf * 8:(hf + 1) * 8].unsqueeze(2).to_broadcast([128, 8, n]), op=ALU.mult),
                r=["pT%d" % hf, "a1", "a2"], w=[tag + "hT%d" % hf])
            sch.op("dve", lambda e, hf=hf: e.tensor_tensor(
                out=hT_dst[:, hf * 8:(hf + 1) * 8, :], in0=hT_dst[:, hf * 8:(hf + 1) * 8, :],
                in1=shcol[:, hf * 8:(hf + 1) * 8].unsqueeze(2).to_broadcast([128, 8, n]), op=ALU.add),
                r=[tag + "hT%d" % hf, "modT"], w=[tag + "hT%d" % hf])

    pTb = [psb[6], psb[7]]

    areset()
    hT = V3(aview(16 * 520, BF16), 16)
    xblk = [aview(D, F32) for _ in range(2)]
    xnb = aview(D, BF16)
    stt = aview(8, F32)
    qtm = aview(4 * 512, F32)
    qn = aview(4 * 512, BF16)
    qsqb = aview(512, F32)
    qst = aview(64, F32)
    qTt = [aview(8 * 128, BF16) for _ in range(4)]
    wit = aview(64, F32)
    sgn = aview(64, F32)
    absw = aview(64, F32)
    wwi = aview(16 * 16, BF16)
    uT = V3(aview(8 * 512, BF16), 8)
    vbuf2 = [aview(4 * 130, F32) for _ in range(2)]
    cusb2 = [aview(512, F32) for _ in range(2)]
    cuh2 = [aview(8, F32) for _ in range(2)]
    ybuf2 = [aview(512, F32) for _ in range(2)]
    sga2 = [aview(512, F32) for _ in range(2)]
    mcb = [aview(512, BF16) for _ in range(2)]
    sgbb = [aview(512, BF16) for _ in range(2)]
    sch.dma("pool", V3(wwi[:], 16), win_d[:, 5696:5712].rearrange("(k p) n -> p k n", p=128), w=["wwi"])

    for tl in range(NT):
        tg = "p2_"
        for blk in range(4):
            xb = xblk[blk % 2]
            sch.dma("sp", xb[:], xo[(tl * 4 + blk) * 128:(tl * 4 + blk + 1) * 128, :], w=[tg + "x"])
            norm_T(tg, xb[:], 128, a1, sh1, hT[:, :, blk * 128:(blk + 1) * 128], xnb, stt, pTb)
        xb = xblk[0]
        sch.dma("sp", xb[0:8, :], xh[tl * 8:(tl + 1) * 8, :], w=[tg + "x"])
        norm_T(tg, xb[0:8, :], 8, a1, sh1, hT[:, :, 512:520], xnb, stt, pTb)
        HT = [tg + "hT0", tg + "hT1"]

        for blk in range(4):
            def mmwi(e, blk=blk):
                ins = None
                for k in range(KC):
                    ins = e.matmul(ps[4][:, blk * 16:(blk + 1) * 16], hT[:, k, blk * 128:(blk + 1) * 128],
                                   V3(wwi[:], 16)[:, k, :], start=(k == 0), stop=(k == KC - 1))
                return ins
            sch.op("pe", mmwi, r=HT + ["wwi"], w=["ps4"])
        sch.op("dve", lambda e: e.tensor_copy(out=wit[:], in_=ps[4][:, 0:64]), r=["ps4"], w=["wit"])
        sch.op("dve", lambda e: e.tensor_scalar(out=sgn[:], in0=wit[:], scalar1=0.0, scalar2=2.0, op0=ALU.is_gt,
                                                op1=ALU.mult), r=["wit"], w=["sgn0"])
        sch.op("dve", lambda e: e.tensor_scalar(out=sgn[:], in0=sgn[:], scalar1=-1.0, scalar2=None, op0=ALU.add),
               r=["sgn0"], w=["sgn"])
        sch.op("dve", lambda e: e.tensor_tensor(out=absw[:], in0=wit[:], in1=sgn[:], op=ALU.mult),
               r=["wit", "sgn"], w=["absw"])
        for blk in range(4):
            sch.dma("sp", sgn_s[tl * 4 + blk], sgn[:, blk * 16:(blk + 1) * 16], r=["sgn"], w=["sgn_s"])

        for which, c0s in (("q", (3072, 3584)), ("qi", (4608, 5120))):
            for ci, c0 in enumerate(c0s):
                buf, key = wchunk(win_d, c0)
                b3 = V3(buf[:], 16)
                for blk in range(4):
                    pp = ps[blk % 2]

                    def mmq(e, blk=blk, pp=pp, b3=b3):
                        ins = None
                        for k in range(KC):
                            ins = e.matmul(pp[:], hT[:, k, blk * 128:(blk + 1) * 128], b3[:, k, :],
                                           start=(k == 0), stop=(k == KC - 1))
                        return ins
                    sch.op("pe", mmq, r=HT + [key], w=["ps%d" % (blk % 2)])
                    sch.op("act", lambda e, blk=blk, pp=pp: e.activation(out=qtm[:, blk * 512:(blk + 1) * 512], in_=pp[:],
                                                                        func=AF.Copy),
                           r=["ps%d" % (blk % 2)], w=["qtm%d" % blk])
                for blk in range(4):
                    j = tl * 4 + blk
                    q3 = V3(qtm[:, blk * 512:(blk + 1) * 512], 8)
                    qn3 = V3(qn[:, blk * 512:(blk + 1) * 512], 8)
                    if which == "q":
                        sch.op("act", lambda e, blk=blk: e.activation(out=qsqb[:], in_=qtm[:, blk * 512:(blk + 1) * 512],
                                                                      func=AF.Square), r=["qtm%d" % blk], w=["qsq"])
                        sch.op("dve", lambda e: e.tensor_reduce(out=qst[:, 0:8], in_=V3(qsqb[:], 8), axis=AX.X,
                                                                op=ALU.add), r=["qsq"], w=["qst0"])
                        sch.op("dve", lambda e: e.tensor_scalar(out=qst[:, 8:16], in0=qst[:, 0:8], scalar1=1.0 / 64,
                                                                scalar2=EPS, op0=ALU.mult, op1=ALU.add),
                               r=["qst0"], w=["qst1"])
                        sch.op("act", lambda e: e.activation(out=qst[:, 16:24], in_=qst[:, 8:16], func=AF.Sqrt),
                               r=["qst1"], w=["qst2"])
                        sch.op("dve", lambda e: e.reciprocal(out=qst[:, 24:32], in_=qst[:, 16:24]), r=["qst2"], w=["qst3"])
                        qo4 = qn[:, blk * 512:(blk + 1) * 512].rearrange("p (m hi d) -> p hi m d", m=4, hi=2)
                        qi4 = qtm[:, blk * 512:(blk + 1) * 512].rearrange("p (hi m d) -> p hi m d", m=4, hi=2)
                        rs4 = qst[:, 24:32].rearrange("p (hi m) -> p hi m", hi=2).unsqueeze(3).to_broadcast([128, 2, 4, 64])
                        sch.op("dve", lambda e, qo4=qo4, qi4=qi4, rs4=rs4: e.tensor_tensor(out=qo4, in0=qi4, in1=rs4, op=ALU.mult),
                               r=["qtm%d" % blk, "qst3"], w=["qn%d" % blk])
                    else:
                        sch.op("dve", lambda e, q3=q3, qn3=qn3, blk=blk, ci=ci: e.tensor_tensor(
                            out=qn3, in0=q3,
                            in1=absw[:, blk * 16 + ci * 8: blk * 16 + ci * 8 + 8].unsqueeze(2).to_broadcast([128, 8, 64]),
                            op=ALU.mult), r=["qtm%d" % blk, "absw"], w=["qn%d" % blk])
                    qt = qTt[blk]
                    qt3 = V3(qt[:], 8)
                    pT3 = V3(pTb[0], 8)

                    def trq(e, blk=blk):
                        ins = None
                        for m in range(4):
                            src = qn[:, blk * 512 + m * 128: blk * 512 + (m + 1) * 128]
                            ins = e.transpose(pT3[:, m, :], src, ident_b[:])
                        return ins
                    sch.op("pe", trq, r=["qn%d" % blk, "ident_b"], w=["pT0"])
                    sch.op("act", lambda e, qt3=qt3, ci=ci: e.activation(out=qt3[:, ci * 4:(ci + 1) * 4, :], in_=pT3[:, 0:4, :],
                                                                        func=AF.Copy),
                           r=["pT0"], w=["qTt%d_%d" % (blk, ci)])
                    if ci == 1:
                        dst = qT_s if which == "q" else qiT_s
                        sch.dma("sp", dst[j], qt[:], r=["qTt%d_0" % blk, "qTt%d_1" % blk], w=[which + "T_s"])

        for half in range(2):
            bufs = [wchunk(win_d, base + half * 512) for base in (0, 1024, 2048)]
            (bcb, kcb), (bcc, kcc), (bcu, kcu) = [(V3(b[:], 16), k) for b, k in bufs]
            for q4 in range(4):
                ch = half * 4 + q4
                cs = slice(q4 * 128, (q4 + 1) * 128)

                pz = ch % 2
                P0_, P1_, P2_, P3_ = ps[4 * pz], ps[4 * pz + 1], ps[4 * pz + 2], ps[4 * pz + 3]
                K0_, K1_, K2_, K3_ = [{6: "pT0", 7: "pT1"}.get(4 * pz + i_, "ps%d" % (4 * pz + i_)) for i_ in range(4)]
                vbuf, cusb, cuh, ybuf = vbuf2[pz], cusb2[pz], cuh2[pz], ybuf2[pz]
                sfx = "_%d" % pz

                def mmc(e, cs=cs, bcb=bcb, bcc=bcc, bcu=bcu, P0_=P0_, P1_=P1_, P2_=P2_, P3_=P3_):
                    ins = None
                    for pp_, bw in ((P0_, bcb), (P1_, bcc), (P2_, bcu)):
                        for k in range(KC):
                            ins = e.matmul(pp_[:], bw[:, k, cs], hT[:, k, 0:512], start=(k == 0), stop=(k == KC - 1))
                    for hi, bw in ((0, bcc), (1, bcu)):
                        for k in range(KC):
                            ins = e.matmul(P3_[:, hi * 8:(hi + 1) * 8], bw[:, k, cs], hT[:, k, 512:520],
                                           start=(k == 0), stop=(k == KC - 1))
                    return ins
                sch.op("pe", mmc, r=HT + [kcb, kcc, kcu], w=[K0_, K1_, K2_, K3_])
                vb3 = V3(vbuf[:], 4)
                sch.op("act", lambda e, cusb=cusb, P2_=P2_: e.activation(out=cusb[:], in_=P2_[:], func=AF.Copy), r=[K2_], w=["cusb" + sfx])
                sch.op("act", lambda e, cuh=cuh, P3_=P3_: e.activation(out=cuh[:], in_=P3_[:, 8:16], func=AF.Copy), r=[K3_], w=["cuh" + sfx])
                sch.op("dve", lambda e, vb3=vb3, P1_=P1_, cusb=cusb: e.tensor_tensor(out=vb3[:, :, 2:130], in0=V3(P1_[:], 4), in1=V3(cusb[:], 4),
                                                                               op=ALU.mult), r=[K1_, "cusb" + sfx], w=["vbuf_a" + sfx])
                sch.op("dve", lambda e, cuh=cuh, P3_=P3_: e.tensor_tensor(out=cuh[:], in0=P3_[:, 0:8], in1=cuh[:], op=ALU.mult),
                       r=[K3_, "cuh" + sfx], w=["cuh2" + sfx])
                sch.op("dve", lambda e, tl=tl, vb3=vb3, cuh=cuh: e.tensor_tensor(out=vb3[:, :, 0:2], in0=V3(cuh[:], 4),
                                                                               in1=V3(hmask[:, tl * 8:(tl + 1) * 8], 4), op=ALU.mult),
                       r=["cuh2" + sfx], w=["vbuf_b" + sfx])
                y3 = V3(ybuf[:], 4)
                VK = ["vbuf_a" + sfx, "vbuf_b" + sfx]
                sch.op("dve", lambda e, ch=ch, y3=y3, vb3=vb3: e.tensor_scalar(out=y3, in0=vb3[:, :, 2:130], scalar1=convw[:, 16 + ch:17 + ch],
                                                                             scalar2=None, op0=ALU.mult),
                       r=VK, w=["ybuf" + sfx])
                sch.op("dve", lambda e, ch=ch, y3=y3, vb3=vb3: e.scalar_tensor_tensor(out=y3, in0=vb3[:, :, 1:129], scalar=convw[:, 8 + ch:9 + ch],
                                                                                    in1=y3, op0=ALU.mult, op1=ALU.add),
                       r=VK + ["ybuf" + sfx], w=["ybuf" + sfx])
                sch.op("dve", lambda e, ch=ch, y3=y3, vb3=vb3: e.scalar_tensor_tensor(out=y3, in0=vb3[:, :, 0:128], scalar=convw[:, ch:ch + 1],
                                                                                    in1=y3, op0=ALU.mult, op1=ALU.add),
                       r=VK + ["ybuf" + sfx], w=["ybuf" + sfx])
                sch.op("dve", lambda e, ch=ch, P0_=P0_, ybuf=ybuf: e.tensor_tensor(out=uT[:, ch, :], in0=P0_[:], in1=ybuf[:], op=ALU.mult),
                       r=[K0_, "ybuf" + sfx], w=["uT"])

        for c4 in range(4):
            bga, kga = wchunk(win_d, 5712 + c4 * 512)
            bgb, kgb = wchunk(win_d, 7760 + c4 * 512)
            bco, kco = wchunk(wco_d, c4 * 512, kc=8)
            bga3, bgb3, bco3 = V3(bga[:], 16), V3(bgb[:], 16), V3(bco[:, 0:8 * 512], 8)
            for q4 in range(4):
                cc = c4 * 4 + q4
                cs = slice(q4 * 128, (q4 + 1) * 128)

                pz = cc % 2
                G0, G1, G2 = ps[3 * pz], ps[3 * pz + 1], ps[3 * pz + 2]
                GK0, GK1, GK2 = ["ps%d" % (3 * pz + i_) for i_ in range(3)]
                sga = sga2[pz]

                def mmg(e, cs=cs, bga3=bga3, bgb3=bgb3, bco3=bco3, G0=G0, G1=G1, G2=G2):
                    ins = None
                    for k in range(KC):
                        ins = e.matmul(G0[:], bga3[:, k, cs], hT[:, k, 0:512], start=(k == 0), stop=(k == KC - 1))
                    for k in range(KC):
                        ins = e.matmul(G1[:], bgb3[:, k, cs], hT[:, k, 0:512], start=(k == 0), stop=(k == KC - 1))
                    for k in range(8):
                        ins = e.matmul(G2[:], bco3[:, k, cs], uT[:, k, :], start=(k == 0), stop=(k == 7))
                    return ins
                sch.op("pe", mmg, r=HT + [kga, kgb, kco, "uT"], w=[GK0, GK1, GK2])
                sch.op("act", lambda e, sga=sga, G0=G0: e.activation(out=sga[:], in_=G0[:], func=AF.Sigmoid), r=[GK0], w=["sga%d" % pz])
                sgo, mco = sgbb[cc % 2], mcb[cc % 2]
                sch.op("act", lambda e, sgo=sgo, G1=G1: e.activation(out=sgo[:], in_=G1[:], func=AF.Sigmoid),
                       r=[GK1], w=["sgbb%d" % (cc % 2)])
                sch.op("dve", lambda e, mco=mco, G2=G2, sga=sga: e.tensor_tensor(out=mco[:], in0=G2[:], in1=sga[:],
                                                                             op=ALU.mult), r=[GK2, "sga%d" % pz], w=["mcb%d" % (cc % 2)])
                sch.dma("sp", mc_s[tl][:, cc * 512:(cc + 1) * 512], mco[:], r=["mcb%d" % (cc % 2)], w=["mc_s"])
                sch.dma("sp", sgb_s[tl][:, cc * 512:(cc + 1) * 512], sgo[:], r=["sgbb%d" % (cc % 2)], w=["sgb_s"])
    sch.barrier()
    if "stop2" in dbg:
        return nc, sch, es

    areset()
    kT = V3(aview(2 * S, BF16), 2)
    Vt = V3(aview(NBLK * 320, BF16), NBLK)
    kiT = aview(S, BF16)
    bT = aview(4 * 16 * 128, BF16)
    P3BASE = aoff[0]
    hT1 = [V3(aview(16 * 128, BF16), 16) for _ in range(2)]
    xblk = [ring[0][:, 0:2 * D].bitcast(F32), ring[1][:, 0:2 * D].bitcast(F32)]
    kjunk = aview(64, F32)
    xnb = aview(D, BF16)
    stt = aview(8, F32)
    wkv = V3(aview(16 * 576, BF16), 16)
    ksq = aview(256, F32)
    kst = aview(32, F32)
    kn = aview(256, BF16)
    kic = aview(64, F32)
    kicb = aview(128, BF16)
    sch.dma("pool", wkv[:, :, 0:512], win_d[:, 4096:4608].rearrange("(k p) n -> p k n", p=128), w=["wkv"])
    sch.dma("pool", wkv[:, :, 512:576], win_d[:, 5632:5696].rearrange("(k p) n -> p k n", p=128), w=["wkv"])
    sch.dma("pool", bT[:], biasT_d, w=["bT"], max_dma_last_dim=8192)
    sch.op("pool", lambda e: e.memset(Vt[:, :, 256:320], 0.0), w=["Vpad"])
    for g in range(NBLK):
        tg = "p1_%d" % (g % 2)
        xb = xblk[g % 2]
        h1 = hT1[g % 2]
        sch.dma("sp", xb[:], xa[g * 128:(g + 1) * 128, :], w=[tg + "x"])
        norm_T(tg, xb[:], 128, a1, sh1, h1, xnb, stt, pTb)
        HT = [tg + "hT0", tg + "hT1"]

        def mmk(e, h1=h1):
            ins = None
            for k in range(KC):
                ins = e.matmul(ps[0][:], h1[:, k, :], wkv[:, k, 0:512], start=(k == 0), stop=(k == KC - 1))
            for k in range(KC):
                ins = e.matmul(ps[1][:, 0:64], h1[:, k, :], wkv[:, k, 512:576], start=(k == 0), stop=(k == KC - 1))
            return ins
        sch.op("pe", mmk, r=HT + ["wkv"], w=["ps0", "ps1"])
        sch.op("act", lambda e, g=g: e.activation(out=Vt[:, g, 0:256], in_=ps[0][:, 256:512], func=AF.Copy), r=["ps0"], w=["V"])
        sch.op("act", lambda e: e.activation(out=ksq[:], in_=ps[0][:, 0:256], func=AF.Square), r=["ps0"], w=["ksq"])
        sch.op("dve", lambda e: e.tensor_reduce(out=kst[:, 0:4], in_=V3(ksq[:], 4), axis=AX.X, op=ALU.add),
               r=["ksq"], w=["kst0"])
        sch.op("dve", lambda e: e.tensor_scalar(out=kst[:, 4:8], in0=kst[:, 0:4], scalar1=1.0 / 64, scalar2=EPS,
                                                op0=ALU.mult, op1=ALU.add), r=["kst0"], w=["kst1"])
        sch.op("act", lambda e: e.activation(out=kst[:, 8:12], in_=kst[:, 4:8], func=AF.Sqrt), r=["kst1"], w=["kst2"])
        sch.op("dve", lambda e: e.reciprocal(out=kst[:, 12:16], in_=kst[:, 8:12]), r=["kst2"], w=["kst3"])
        sch.op("dve", lambda e: e.tensor_tensor(out=V3(kn[:], 4), in0=V3(ps[0][:, 0:256], 4),
                                                in1=kst[:, 12:16].unsqueeze(2).to_broadcast([128, 4, 64]), op=ALU.mult),
               r=["ps0", "kst3"], w=["kn"])
        sch.op("dve", lambda e: e.tensor_reduce(out=kst[:, 16:17], in_=ps[1][:, 0:64], axis=AX.X, op=ALU.add),
               r=["ps1"], w=["ki0"])
        sch.op("dve", lambda e: e.tensor_scalar(out=kst[:, 17:18], in0=kst[:, 16:17], scalar1=-1.0 / 64, scalar2=None,
                                                op0=ALU.mult), r=["ki0"], w=["ki1"])
        sch.op("dve", lambda e: e.tensor_scalar(out=kic[:], in0=ps[1][:, 0:64], scalar1=kst[:, 17:18], scalar2=None,
                                                op0=ALU.add), r=["ps1", "ki1"], w=["kic"])
        sch.op("act", lambda e: e.activation(out=kjunk[:], in_=kic[:], func=AF.Square, accum_out=kst[:, 18:19]),
               r=["kic"], w=["ki2", "p1junk"])
        sch.op("dve", lambda e: e.tensor_scalar(out=kst[:, 19:20], in0=kst[:, 18:19], scalar1=1.0 / 64, scalar2=EPS,
                                                op0=ALU.mult, op1=ALU.add), r=["ki2"], w=["ki3"])
        sch.op("act", lambda e: e.activation(out=kst[:, 20:21], in_=kst[:, 19:20], func=AF.Sqrt), r=["ki3"], w=["ki4"])
        sch.op("dve", lambda e: e.reciprocal(out=kst[:, 21:22], in_=kst[:, 20:21]), r=["ki4"], w=["ki5"])
        for hf in range(2):
            sch.op("dve", lambda e, hf=hf: e.tensor_scalar(out=kicb[:, hf * 64:(hf + 1) * 64], in0=kic[:],
                                                           scalar1=kst[:, 21:22], scalar2=None, op0=ALU.mult),
                   r=["kic", "ki5"], w=["kicb%d" % hf])
        pT3 = V3(pTb[0], 8)

        def trk(e):
            e.transpose(pT3[:, 0, :], kn[:, 0:128], ident_b[:])
            e.transpose(pT3[:, 1, :], kn[:, 128:256], ident_b[:])
            return e.transpose(pT3[:, 2, :], kicb[:], ident_b[:])
        sch.op("pe", trk, r=["kn", "kicb0", "kicb1", "ident_b"], w=["pT0"])
        for pr in range(2):
            sch.op("act", lambda e, pr=pr, g=g: e.activation(out=kT[:, pr, g * 128:(g + 1) * 128], in_=pT3[:, pr, :],
                                                            func=AF.Identity, scale=colv[:, 0:1]),
                   r=["pT0", "colv"], w=["kT"])
        sch.op("act", lambda e, g=g: e.activation(out=kiT[:, g * 128:(g + 1) * 128], in_=pT3[:, 2, :], func=AF.Identity,
                                                  scale=colv[:, 1:2], bias=colv[:, 2:3]), r=["pT0", "colv_raw"], w=["kiT"])
    sch.barrier()
    if "stop1" in dbg:
        return nc, sch, es

    aoff[0] = P3BASE
    qzb = [aview(16 * 128, BF16) for _ in range(3)]
    for qq_ in qzb:
        sch.op("pool", lambda e, qq_=qq_: e.memset(qq_[:], 0.0), w=["qz_init"])
    sch.barrier()
    qiTb = [aview(8 * 128, BF16) for _ in range(3)]
    sgb_ = [aview(16, F32) for _ in range(3)]
    scoresb = [ring[0][:, 0:2 * S].bitcast(F32), ring[3][:, 0:2 * S].bitcast(F32)]
    m01 = ring[1][:, 0:S]
    sjunk = ring[1][:, S:2 * S]
    mT = V3(ring[2][:, 0:NBLK * 128], NBLK)
    Dmb = [V3(ring[2][:, 4096:4096 + 2048], 16), V3(ring[4][:, 0:2048], 16)]
    rbuf = [ring[2][:, 6144 + i * 512:6144 + (i + 1) * 512] for i in range(4)]
    amb = [aview(16, F32) for _ in range(2)]
    bst = aview(16, F32)
    pbuf = [aview(512, BF16) for _ in range(3)]
    rl = aview(512, F32)
    attn = [aview(16 * 128, BF16) for _ in range(2)]
    bT4 = bT[:].rearrange("p (r h t) -> p r h t", r=4, h=16)
    NQ = OWN if "p3n" not in dbg else 3
    pTm = V3(psb[7], 8)

    def geom(j):
        nkb = 2 * j + 2
        n = nkb * 128
        nch = (n + 511) // 512
        return nkb, n, nch

    def gen_indexer(j):
        nkb, n, nch = geom(j)
        t3, t2 = j % 3, j % 2
        tg = "p3_%d" % t3
        qz3 = V3(qzb[t3][:], 16)
        qiT3 = V3(qiTb[t3][:], 8)
        sg = sgb_[t3]
        Dm = Dmb[t2]
        scores = scoresb[t2]
        am = amb[t2]
        DK, SK, AK = "Dm%d" % t2, "scores%d" % t2, "am%d" % t2
        qsrc = qT_s[j].rearrange("p (s t) -> p s t", s=8)
        for ci in range(2):
            sch.dma("sp", qz3[0:64, 8 * ci:8 * ci + 4, :], qsrc[0:64, 4 * ci:4 * ci + 4, :], w=[tg + "qT"])
            sch.dma("sp", qz3[64:128, 8 * ci + 4:8 * ci + 8, :], qsrc[64:128, 4 * ci:4 * ci + 4, :], w=[tg + "qT"])
        sch.dma("sp", qiTb[t3][:], qiT_s[j], w=[tg + "qiT"])
        sch.dma("sp", sg[:], sgn_s[j], w=[tg + "sg"])
        sch.op("dve", lambda e: e.tensor_tensor(out=Dm, in0=ident_b[:].unsqueeze(1).to_broadcast([128, 16, 128]),
                                                in1=sg[:].unsqueeze(2).to_broadcast([128, 16, 128]), op=ALU.mult),
               r=[tg + "sg"], w=[DK])
        yield
        for c in range(nch):
            w_ = min(512, n - c * 512)
            last = (c == nch - 1)

            def dots(h, c=c, w_=w_):
                half, slot = h % 2, h // 2
                pd = ps[h % 2]
                sch.op("pe", lambda e, pd=pd, half=half, slot=slot: e.matmul(
                    pd[:, 0:w_], qiT3[half * 64:(half + 1) * 64, slot, :],
                    kiT[half * 64:(half + 1) * 64, c * 512:c * 512 + w_], start=True, stop=True),
                    r=[tg + "qiT"], w=["ps%d" % (h % 2)])
                rb = rbuf[h % 4]
                if h % 2 == 0:
                    sch.op("act", lambda e, pd=pd, rb=rb: e.activation(out=rb[:, 0:w_], in_=pd[:, 0:w_], func=AF.Relu),
                           r=["ps%d" % (h % 2)], w=["rbuf%d" % (h % 4)])
                else:
                    sch.op("dve", lambda e, pd=pd, rb=rb: e.tensor_scalar(out=rb[:, 0:w_], in0=pd[:, 0:w_], scalar1=0.0,
                                                                          scalar2=None, op0=ALU.max),
                           r=["ps%d" % (h % 2)], w=["rbuf%d" % (h % 4)])

            dots(0)
            dots(1)
            for h in range(16):
                rb = rbuf[h % 4]
                sch.op("pe", lambda e, h=h, rb=rb, w_=w_: e.matmul(ps[2][:, 0:w_], Dm[:, h, :], rb[:, 0:w_],
                                                                  start=(h == 0), stop=(h == 15)),
                       r=["rbuf%d" % (h % 4), DK], w=["ps2"])
                if h + 2 < 16:
                    dots(h + 2)
                yield
            sch.op("dve", lambda e, c=c, w_=w_: e.tensor_reduce(out=am[:, c:c + 1], in_=ps[2][:, 0:w_], axis=AX.X, op=ALU.max,
                                                                apply_absolute_value=True), r=["ps2"], w=[AK])
            if last:
                if w_ > 256:
                    sch.op("dve", lambda e, c=c, w_=w_: e.tensor_copy(out=scores[:, c * 512:c * 512 + w_ - 256],
                                                                      in_=ps[2][:, 0:w_ - 256]),
                           r=["ps2"], w=[SK])
                sch.op("dve", lambda e, c=c, w_=w_: e.tensor_tensor(out=scores[:, c * 512 + w_ - 256:c * 512 + w_],
                                                                    in0=ps[2][:, w_ - 256:w_], in1=cmask[:], op=ALU.add),
                       r=["ps2"], w=[SK])
            else:
                sch.op("dve", lambda e, c=c: e.tensor_copy(out=scores[:, c * 512:(c + 1) * 512], in_=ps[2][:]),
                       r=["ps2"], w=[SK])
            yield

    def gen_bisect(j):
        nkb, n, nch = geom(j)
        t2 = j % 2
        scores = scoresb[t2]
        am = amb[t2]
        SK, AK = "scores%d" % t2, "am%d" % t2
        sch.op("dve", lambda e: e.tensor_reduce(out=bst[:, 0:1], in_=am[:, 0:nch], axis=AX.X, op=ALU.max),
               r=[AK], w=["b_am0"])
        sch.op("dve", lambda e: e.tensor_scalar(out=bst[:, 0:1], in0=bst[:, 0:1], scalar1=1.001, scalar2=1e-6, op0=ALU.mult,
                                                op1=ALU.add), r=["b_am0"], w=["b_am"])
        sch.op("dve", lambda e: e.tensor_scalar(out=bst[:, 1:2], in0=bst[:, 0:1], scalar1=-1.0, scalar2=None, op0=ALU.mult),
               r=["b_am"], w=["b_lo"])
        yield
        thr_cnt = 512.0 - n - 0.5
        for it in range(NBIS):
            sc_ = 2.0 ** (-it)
            sch.op("dve", lambda e, sc_=sc_: e.scalar_tensor_tensor(out=bst[:, 2:3], in0=bst[:, 0:1], scalar=-sc_,
                                                                    in1=bst[:, 1:2], op0=ALU.mult, op1=ALU.subtract),
                   r=["b_am", "b_lo"], w=["b_nm"])
            sch.op("act", lambda e: e.activation(out=sjunk[:, 0:n], in_=scores[:, 0:n], func=AF.Sign, bias=bst[:, 2:3],
                                                 scale=1.0, accum_out=bst[:, 3:4]),
                   r=[SK, "b_nm"], w=["b_cnt", "sjunk"])
            sch.op("dve", lambda e, sc_=sc_: e.tensor_scalar(out=bst[:, 4:5], in0=bst[:, 3:4], scalar1=thr_cnt, scalar2=sc_,
                                                             op0=ALU.is_ge, op1=ALU.mult), r=["b_cnt"], w=["b_c2"])
            sch.op("dve", lambda e: e.scalar_tensor_tensor(out=bst[:, 1:2], in0=bst[:, 4:5], scalar=bst[:, 0:1],
                                                           in1=bst[:, 1:2], op0=ALU.mult, op1=ALU.add),
                   r=["b_c2", "b_am", "b_lo"], w=["b_lo"])
            yield

    def emit_masks(j):
        nkb, n, nch = geom(j)
        scores = scoresb[j % 2]
        SK = "scores%d" % (j % 2)
        sch.op("dve", lambda e: e.tensor_scalar(out=m01[:, 0:n], in0=scores[:, 0:n], scalar1=bst[:, 1:2], scalar2=None,
                                                op0=ALU.is_ge), r=[SK, "b_lo"], w=["m01"])
        for b0 in range(0, nkb, 8):
            nb_ = min(8, nkb - b0)

            def trm(e, b0=b0, nb_=nb_):
                ins = None
                for i in range(nb_):
                    ins = e.transpose(pTm[:, i, :], m01[:, (b0 + i) * 128:(b0 + i + 1) * 128], ident_b[:])
                return ins
            sch.op("pe", trm, r=["m01"], w=["ps7"])
            sch.op("act", lambda e, b0=b0, nb_=nb_: e.activation(out=mT[:, b0:b0 + nb_, :], in_=pTm[:, 0:nb_, :],
                                                               func=AF.Copy), r=["ps7"], w=["mT"])

    def gen_main(j):
        nkb, n, nch = geom(j)
        t3, t2 = j % 3, j % 2
        tg = "p3_%d" % t3
        qz3 = V3(qzb[t3][:], 16)
        at = attn[t2]
        at3 = V3(at[:], 16)
        ATK = "attn%d" % t2
        for g in range(4):
            pr = g // 2
            qg = qz3[:, 4 * g:4 * g + 4, :]

            def qk(kb, g=g, qg=qg, pr=pr):
                pq = ps[3 + kb % 2]
                r_ = min(2 * j + 1 - kb, 3)

                def mmqk(e):
                    e.matmul(pq[:], kT[:, pr, kb * 128:(kb + 1) * 128], qg, start=True, stop=False)
                    return e.matmul(pq[:], ident_b[:], bT4[:, r_, 4 * g:4 * g + 4, :], start=False, stop=True)
                sch.op("pe", mmqk, r=[tg + "qT"], w=["ps%d" % (3 + kb % 2)])

            qk(0)
            for kb in range(nkb):
                if kb + 1 < nkb:
                    qk(kb + 1)
                pq = ps[3 + kb % 2]
                pqk = "ps%d" % (3 + kb % 2)
                pbf = pbuf[kb % 3]
                pbk = "pbuf%d" % (kb % 3)
                sch.op("act", lambda e, pq=pq, pbf=pbf: e.activation(out=pbf[:], in_=pq[:], func=AF.Exp), r=[pqk], w=[pbk])
                sch.op("dve", lambda e, pbf=pbf, kb=kb: e.tensor_tensor(
                    out=V3(pbf[:], 4), in0=V3(pbf[:], 4), in1=mT[:, kb, :].unsqueeze(1).to_broadcast([128, 4, 128]),
                    op=ALU.mult), r=[pbk, "mT"], w=[pbk])

                def mmpv(e, pbf=pbf, kb=kb, g=g):
                    e.matmul(ps[5][:], Vt[:, kb, g * 64:g * 64 + 128], pbf[:], start=(kb == 0), stop=(kb == nkb - 1))
                    return e.matmul(ps[6][:], ones_b[:], pbf[:], start=(kb == 0), stop=(kb == nkb - 1))
                sch.op("pe", mmpv, r=[pbk], w=["ps5", "ps6"])
                yield
            sch.op("dve", lambda e: e.reciprocal(out=rl[0:64, :], in_=ps[6][0:64, :]), r=["ps6"], w=["rl"])
            sch.op("dve", lambda e, g=g: e.tensor_tensor(out=at3[0:64, 4 * g:4 * g + 4, :], in0=V3(ps[5][0:64, :], 4),
                                                         in1=V3(rl[0:64, :], 4), op=ALU.mult),
                   r=["ps5", "rl"], w=[ATK])
            yield
        sch.dma("sp", attn_s[j], at[0:64, :], r=[ATK], w=["attn_s"])
        yield

    def run_all(g):
        for _ in g:
            pass

    def take(g, k):
        if g is None:
            return None
        for _ in range(k):
            try:
                next(g)
            except StopIteration:
                return None
        return g

    run_all(gen_indexer(0))
    gM = None
    for j in range(NQ):
        gB = gen_bisect(j)
        gI = gen_indexer(j + 1) if j + 1 < NQ else None
        lenI = (17 * geom(j + 1)[2] + 1) if j + 1 < NQ else 0
        lenM = (4 * geom(j - 1)[0] + 5) if j >= 1 else 0
        kI = (lenI + NBIS - 1) // NBIS
        kM = (lenM + NBIS - 1) // NBIS
        while gB is not None:
            gB = take(gB, 1)
            gI = take(gI, kI)
            gM = take(gM, kM)
        if gI is not None:
            run_all(gI)
        if gM is not None:
            run_all(gM)
        emit_masks(j)
        gM = gen_main(j)
    run_all(gM)
    sch.barrier()
    if "stop3" in dbg:
        return nc, sch, es

    areset()
    attnT = V3(aview(16 * 512, BF16), 16)
    mcT = aview(16 * 512, BF16)
    sgbT = aview(16 * 512, BF16)
    tmpb = aview(512, BF16)
    xt4 = aview(4 * D, F32)
    tmpf = aview(512, F32)
    g1_bc = aview(D, F32)
    sch.dma("sp", g1_bc[:], mod_flat[:, 32 * 128:48 * 128].partition_broadcast(128), w=["g1_bc"])
    for tl in range(NT):
        for blk in range(4):
            sch.dma("sp", attnT[0:64, :, blk * 128:(blk + 1) * 128],
                    attn_s[tl * 4 + blk].rearrange("p (h t) -> p h t", h=16), w=["attnT"])
        sch.dma("sp", mcT[:], mc_s[tl], w=["mcT"])
        sch.dma("sp", sgbT[:], sgb_s[tl], w=["sgbT"])
        sch.dma("sp", V3(xt4[:], 4), xo[tl * 512:(tl + 1) * 512, :].rearrange("(b p) n -> p b n", p=128), w=["xt4"])
        for c4 in range(4):
            i = rstate["i"]
            rstate["i"] += 1
            buf = ring[i % len(ring)]
            key = ("ring", i % len(ring))
            sch.dma("pool", V3(buf[0:64, :], 16), wao_d[:, c4 * 512:(c4 + 1) * 512].rearrange("(h p) n -> p h n", p=64), w=[key])
            b3 = V3(buf[:], 16)
            for q4 in range(4):
                cc = c4 * 4 + q4
                cs = slice(q4 * 128, (q4 + 1) * 128)
                pp = ps[cc % 2]
                pk = "ps%d" % (cc % 2)

                def mma(e, cs=cs, pp=pp, b3=b3):
                    ins = None
                    for h in range(16):
                        ins = e.matmul(pp[:], b3[0:64, h, cs], attnT[0:64, h, :], start=(h == 0), stop=(h == 15))
                    return ins
                sch.op("pe", mma, r=[key, "attnT"], w=[pk])
                sch.op("dve", lambda e, cc=cc, pp=pp: e.tensor_tensor(out=tmpb[:], in0=pp[:], in1=sgbT[:, cc * 512:(cc + 1) * 512],
                                                                     op=ALU.mult), r=[pk, "sgbT"], w=["tmpb"])
                sch.op("dve", lambda e, cc=cc: e.tensor_tensor(out=mcT[:, cc * 512:(cc + 1) * 512], in0=mcT[:, cc * 512:(cc + 1) * 512],
                                                               in1=tmpb[:], op=ALU.add), r=["tmpb", "mcT"], w=["mixT"])
        mx3 = V3(mcT[:], 16)
        for c4 in range(4):
            buf, key = wchunk(wo_d, c4 * 512)
            b3 = V3(buf[:], 16)
            for blk in range(4):
                pp = ps[2 + blk % 2]
                pk = "ps%d" % (2 + blk % 2)

                def mmo(e, blk=blk, pp=pp, b3=b3):
                    ins = None
                    for k in range(KC):
                        ins = e.matmul(pp[:], mx3[:, k, blk * 128:(blk + 1) * 128], b3[:, k, :], start=(k == 0), stop=(k == KC - 1))
                    return ins
                sch.op("pe", mmo, r=[key, "mixT", "mcT"], w=[pk])
                xs = xt4[:, blk * D + c4 * 512: blk * D + (c4 + 1) * 512]
                sch.op("dve", lambda e, pp=pp, c4=c4: e.tensor_tensor(out=tmpf[:], in0=pp[:], in1=g1_bc[:, c4 * 512:(c4 + 1) * 512],
                                                                     op=ALU.mult), r=[pk, "g1_bc"], w=["tmpf"])
                sch.op("dve", lambda e, xs=xs: e.tensor_tensor(out=xs, in0=xs, in1=tmpf[:], op=ALU.add), r=["tmpf", "xt4"], w=["x1t"])
        sch.dma("sp", x1_s[tl * 512:(tl + 1) * 512, :].rearrange("(b p) n -> p b n", p=128), V3(xt4[:], 4),
                r=["x1t", "xt4"], w=["x1_s"])
    sch.barrier()
    if "stop4" in dbg:
        return nc, sch, es

    areset()
    xb2 = [aview(D, F32) for _ in range(2)]
    acc = aview(4 * D, F32)
    h2T = V3(aview(16 * 512, BF16), 16)
    xnb = aview(D, BF16)
    stt = aview(8, F32)
    wr_f = V3(aview(16 * E, F32), 16)
    wr2 = V3(aview(16 * E, F32), 16)
    brow = aview(E, F32)
    rt = aview(8 * E, F32)
    comb = aview(4 * (E + 1) + 4, F32)
    g2_bc = aview(D, F32)
    XBASE = aoff[0]
    xnf = aview(D, F32)
    xnT = V3(aview(16 * 128, F32), 16)
    aoff[0] = XBASE
    sil = [aview(512, F32) for _ in range(2)]
    gT = [V3(aview(4 * 512, BF16), 4) for _ in range(2)]
    comb3 = V3(comb[:, 0:4 * (E + 1)], 4)
    sch.dma("sp", g2_bc[:], mod_flat[:, 80 * 128:96 * 128].partition_broadcast(128), w=["g2_bc"])
    sch.dma("sp", wr_f, wr_d.rearrange("(k p) n -> p k n", p=128), w=["wr_f"])
    sch.op("dve", lambda e: e.tensor_tensor(out=wr2, in0=wr_f, in1=a2[:].unsqueeze(2).to_broadcast([128, 16, E]), op=ALU.mult),
           r=["wr_f", "a2"], w=["wr2"])

    def mmb(e):
        ins = None
        for k in range(KC):
            ins = e.matmul(ps[7][0:1, 0:E], sh2[:, k:k + 1], wr_f[:, k, :], start=(k == 0), stop=(k == KC - 1))
        return ins
    sch.op("pe", mmb, r=["wr_f", "modT"], w=["ps7"])
    sch.op("dve", lambda e: e.tensor_copy(out=brow[0:1, :], in_=ps[7][0:1, 0:E]), r=["ps7"], w=["brow"])
    sch.op("pool", lambda e: e.memset(comb[:], 1.0), w=["comb"])
    sch.barrier()

    for tl in range(NT):
        tg = "p5_"
        for blk in range(4):
            xs_t = xb2[blk % 2]
            xs = xs_t[:]
            sch.dma("sp", xs, x1_s[(tl * 4 + blk) * 128:(tl * 4 + blk + 1) * 128, :], w=[tg + "x"])
            norm_T(tg, xs, 128, a2, sh2, h2T[:, :, blk * 128:(blk + 1) * 128], xnb, stt, pTb, xn_f32=xnf)
            for hf in range(2):
                def trf(e, hf=hf):
                    ins = None
                    for k in range(8):
                        kk = hf * 8 + k
                        ins = e.transpose(ps[hf * 2 + k // 4][:, (k % 4) * 128:(k % 4 + 1) * 128], xnf[:, kk * 128:(kk + 1) * 128],
                                          ident_f[:])
                    return ins
                sch.op("pe", trf, r=[tg + "xnf", "ident_f"], w=["ps%d" % (hf * 2), "ps%d" % (hf * 2 + 1)])
                for q in range(2):
                    pi = hf * 2 + q
                    sch.op("act", lambda e, pi=pi: e.activation(out=xnT[:, pi * 4:(pi + 1) * 4, :], in_=V3(ps[pi][:], 4), func=AF.Copy),
                           r=["ps%d" % pi], w=["xnT%d" % pi])

            def mmr(e):
                for k in range(KC):
                    e.matmul(ps[4][:, 0:E], xnT[:, k, :], wr2[:, k, :], start=(k == 0), stop=False)
                return e.matmul(ps[4][:, 0:E], ones_f[0:1, :], brow[0:1, :], start=False, stop=True)
            sch.op("pe", mmr, r=["xnT0", "xnT1", "xnT2", "xnT3", "wr2", "brow", "ones_f"], w=["ps4"])
            R = lambda i: rt[:, i * E:(i + 1) * E]
            sch.op("act", lambda e: e.activation(out=R(0), in_=ps[4][:, 0:E], func=AF.Sigmoid), r=["ps4"], w=["r0"])
            sch.op("dve", lambda e: e.tensor_tensor(out=R(1), in0=R(0), in1=rbias[:], op=ALU.add), r=["r0", "rbias"], w=["r1"])
            g3 = V3(R(1), 8)
            sch.op("dve", lambda e: e.tensor_reduce(out=R(7)[:, 0:8], in_=g3, axis=AX.X, op=ALU.max), r=["r1"], w=["m1"])
            sch.op("dve", lambda e: e.tensor_tensor(out=V3(R(2), 8), in0=g3, in1=R(7)[:, 0:8].unsqueeze(2).to_broadcast([128, 8, 8]),
                                                    op=ALU.is_equal), r=["r1", "m1"], w=["r2"])
            sch.op("dve", lambda e: e.scalar_tensor_tensor(out=R(2), in0=R(2), scalar=-BIG, in1=R(1), op0=ALU.mult, op1=ALU.add),
                   r=["r2", "r1"], w=["r2b"])
            sch.op("dve", lambda e: e.tensor_reduce(out=R(7)[:, 8:16], in_=V3(R(2), 8), axis=AX.X, op=ALU.max), r=["r2b"], w=["m2"])
            sch.op("dve", lambda e: e.tensor_tensor(out=R(7)[:, 16:24], in0=R(7)[:, 0:8], in1=R(7)[:, 8:16], op=ALU.add),
                   r=["m1", "m2"], w=["gs"])
            sch.op("dve", lambda e: e.max(out=R(7)[:, 24:32], in_=R(7)[:, 16:24]), r=["gs"], w=["gsort"])
            sch.op("dve", lambda e: e.tensor_scalar(out=R(7)[:, 32:40], in0=R(7)[:, 16:24], scalar1=R(7)[:, 27:28], scalar2=None,
                                                    op0=ALU.is_ge), r=["gs", "gsort"], w=["gmask"])
            sch.op("dve", lambda e: e.tensor_tensor(out=V3(R(3), 8), in0=g3, in1=R(7)[:, 32:40].unsqueeze(2).to_broadcast([128, 8, 8]),
                                                    op=ALU.mult), r=["r1", "gmask"], w=["r3"])
            sch.op("dve", lambda e: e.tensor_scalar(out=R(7)[:, 40:48], in0=R(7)[:, 32:40], scalar1=-1.0, scalar2=BIG,
                                                    op0=ALU.add, op1=ALU.mult), r=["gmask"], w=["gneg"])
            sch.op("dve", lambda e: e.tensor_tensor(out=V3(R(3), 8), in0=V3(R(3), 8),
                                                    in1=R(7)[:, 40:48].unsqueeze(2).to_broadcast([128, 8, 8]), op=ALU.add),
                   r=["r3", "gneg"], w=["r3b"])
            sch.op("dve", lambda e: e.max(out=R(7)[:, 48:56], in_=R(3)), r=["r3b"], w=["esort"])
            sch.op("dve", lambda e: e.tensor_scalar(out=R(4), in0=R(3), scalar1=R(7)[:, 55:56], scalar2=None, op0=ALU.is_ge),
                   r=["r3b", "esort"], w=["r4"])
            sch.op("dve", lambda e: e.tensor_tensor(out=R(5), in0=R(4), in1=R(0), op=ALU.mult), r=["r4", "r0"], w=["r5"])
            sch.op("dve", lambda e: e.tensor_reduce(out=R(7)[:, 56:57], in_=R(5), axis=AX.X, op=ALU.add), r=["r5"], w=["den"])
            sch.op("dve", lambda e: e.reciprocal(out=R(7)[:, 57:58], in_=R(7)[:, 56:57]), r=["den"], w=["rden"])
            sch.op("dve", lambda e, blk=blk: e.tensor_scalar(out=comb3[:, blk, 0:E], in0=R(5), scalar1=R(7)[:, 57:58], scalar2=2.5,
                                                             op0=ALU.mult, op1=ALU.mult), r=["r5", "rden", "comb"], w=["comb"])
        sch.barrier()
        acc3 = V3(acc[:], 4)
        def load_e(e_):
            if e_ < E:
                s1, s3, s2 = w1_d[e_], w3_d[e_], w2_d[e_]
            else:
                s1, s3, s2 = ws1_d, ws3_d, ws2_d
            b1, k1 = wchunk(s1, 0)
            b3_, k3 = wchunk(s3, 0)
            i = rstate["i"]
            rstate["i"] += 1
            b2 = ring[i % len(ring)]
            k2 = ("ring", i % len(ring))
            sch.dma("pool", V3(b2[:], 4), s2.rearrange("(k p) n -> p k n", p=128), w=[k2], max_dma_last_dim=8192)
            return dict(w1v=V3(b1[:], 16), w3v=V3(b3_[:], 16), w2v=V3(b2[:], 4), k1=k1, k3=k3, k2=k2)

        def emit_H(e_, W, fs):
            gt = gT[e_ % 2]
            gk = "gT%d" % (e_ % 2)
            w1v, w3v = W["w1v"], W["w3v"]
            for f in fs:
                pa, pb2 = ps[(f % 2) * 2], ps[(f % 2) * 2 + 1]
                ka, kb2 = "ps%d" % ((f % 2) * 2), "ps%d" % ((f % 2) * 2 + 1)

                def mmh(e, f=f, pa=pa, pb2=pb2):
                    ins = None
                    for k in range(KC):
                        ins = e.matmul(pa[:], w1v[:, k, f * 128:(f + 1) * 128], h2T[:, k, :], start=(k == 0), stop=(k == KC - 1))
                    for k in range(KC):
                        ins = e.matmul(pb2[:], w3v[:, k, f * 128:(f + 1) * 128], h2T[:, k, :], start=(k == 0), stop=(k == KC - 1))
                    return ins
                sch.op("pe", mmh, r=[W["k1"], W["k3"]], w=[ka, kb2])
                sl = sil[f % 2]
                sch.op("act", lambda e, pa=pa, sl=sl: e.activation(out=sl[:], in_=pa[:], func=AF.Silu), r=[ka], w=["sil%d" % (f % 2)])
                sch.op("dve", lambda e, pb2=pb2, sl=sl, f=f: e.tensor_tensor(out=gt[:, f, :], in0=pb2[:], in1=sl[:], op=ALU.mult),
                       r=[kb2, "sil%d" % (f % 2)], w=[gk + "_%d" % f])

        def emit_Y(e_, W):
            gt = gT[e_ % 2]
            gk = "gT%d" % (e_ % 2)
            w2v = W["w2v"]
            for blk in range(4):
                for c4 in range(4):
                    pi = 4 + (blk * 4 + c4) % 4
                    px, pk = ps[pi], "ps%d" % pi

                    def mmy(e, blk=blk, c4=c4, px=px):
                        ins = None
                        for f in range(4):
                            ins = e.matmul(px[:], gt[:, f, blk * 128:(blk + 1) * 128], w2v[:, f, c4 * 512:(c4 + 1) * 512],
                                           start=(f == 0), stop=(f == 3))
                        return ins
                    sch.op("pe", mmy, r=[gk + "_0", gk + "_1", gk + "_2", gk + "_3", W["k2"]], w=[pk])
                    av = acc3[:, blk, c4 * 512:(c4 + 1) * 512]
                    if e_ == 0:
                        sch.op("dve", lambda e, px=px, av=av, blk=blk: e.tensor_scalar(
                            out=av, in0=px[:], scalar1=comb3[:, blk, e_:e_ + 1], scalar2=None, op0=ALU.mult),
                            r=[pk], w=["acc"])
                    else:
                        sch.op("dve", lambda e, px=px, av=av, blk=blk: e.scalar_tensor_tensor(
                            out=av, in0=px[:], scalar=comb3[:, blk, e_:e_ + 1], in1=av, op0=ALU.mult, op1=ALU.add),
                            r=[pk, "acc"], w=["acc"])

        Wc = load_e(0)
        emit_H(0, Wc, range(4))
        for e_ in range(E + 1):
            Wn = None
            if e_ + 1 <= E:
                Wn = load_e(e_ + 1)
                emit_H(e_ + 1, Wn, [0])
            emit_Y(e_, Wc)
            if Wn is not None:
                emit_H(e_ + 1, Wn, [1, 2, 3])
            Wc = Wn
        for blk in range(4):
            xs_t = xb2[blk % 2]
            sch.dma("sp", xs_t[:], x1_s[(tl * 4 + blk) * 128:(tl * 4 + blk + 1) * 128, :], w=["xfin%d" % (blk % 2)])
            for c4 in range(4):
                av = acc3[:, blk, c4 * 512:(c4 + 1) * 512]
                xs = xs_t[:, c4 * 512:(c4 + 1) * 512]
                sch.op("dve", lambda e, av=av, c4=c4: e.tensor_tensor(out=av, in0=av, in1=g2_bc[:, c4 * 512:(c4 + 1) * 512], op=ALU.mult),
                       r=["acc"], w=["acc"])
                sch.op("dve", lambda e, av=av, xs=xs: e.tensor_tensor(out=av, in0=av, in1=xs, op=ALU.add),
                       r=["acc", "xfin%d" % (blk % 2)], w=["acc"])
        sch.dma("sp", out_d[tl * 512:(tl + 1) * 512, :].rearrange("(b p) n -> p b n", p=128), acc3, r=["acc"], w=["out"])
        sch.barrier()
    return nc, sch, es


def finish(nc, sch, es):
    from contextlib import ExitStack
    with es:
        sem_names = ["pe", "act", "dve", "pool"]
        sems = {}
        for n_ in sem_names:
            sems[n_] = es.enter_context(nc.semaphore("s_" + n_))
        dpool = {"sp": [es.enter_context(nc.semaphore("dsp%d" % i)) for i in range(12)],
                 "pool": [es.enter_context(nc.semaphore("dpl%d" % i)) for i in range(8)]}
        with nc.Block() as block:
            @block.tensor
            def _(e):
                sch_emit_one(nc, sch, "pe", e, sems, dpool)

            @block.scalar
            def _(e):
                sch_emit_one(nc, sch, "act", e, sems, dpool)

            @block.vector
            def _(e):
                sch_emit_one(nc, sch, "dve", e, sems, dpool)

            @block.gpsimd
            def _(e):
                sch_emit_one(nc, sch, "pool", e, sems, dpool)

            @block.sync
            def _(e):
                sch_emit_one(nc, sch, "sp", e, sems, dpool)
    return nc


_assigned = {}


def _assign_events(sch, sems, dpool):
    if id(sch) in _assigned:
        return
    _assigned[id(sch)] = True
    for e, lst in sch.ops.items():
        cnt = 0
        dcount = {}
        di = 0
        for o in lst:
            if o.is_dma:
                pool = dpool[e]
                s = pool[di % len(pool)]
                di += 1
                c = dcount.get(id(s), 0)
                o.presem = (s, c * 16)
                dcount[id(s)] = c + 1
                o.event = (s, (c + 1) * 16)
            elif o.fn is not None and o.needed:
                cnt += 1
                o.event = (sems[e], cnt)


def sch_emit_one(nc, sch, e, eng, sems, dpool):
    _assign_events(sch, sems, dpool)
    waited = {}
    finals = {}

    def wait(ev):
        s, v = ev
        if waited.get(id(s), 0) < v:
            eng.wait_ge(s, v)
            waited[id(s)] = v

    for o in sch.ops[e]:
        for d in o.deps:
            if d.event is None:
                continue
            if d.eng == e and e == "pe" and not d.is_dma:
                continue
            wait(d.event)
        if o.fn is None:
            continue
        if o.is_dma:
            if o.presem[1] > 0:
                wait(o.presem)
            ins = o.fn(eng)
            ins.then_inc(o.event[0], 16)
            finals[id(o.event[0])] = o.event
        else:
            ins = o.fn(eng)
            if o.event is not None:
                ins.then_inc(o.event[0], 1)
    for ev in finals.values():
        wait(ev)


def make_inputs(core, x, c, rel_bias, norm1_w, norm2_w, w_ada, b_ada, w_in, conv_w, w_conv_out, q_norm_w, k_norm_w,
                idx_k_norm_w, idx_k_norm_b, w_attn_out, w_o, w_router, router_bias, w1, w3, w2, ws1, ws3, ws2):
    b, p = core // 2, core % 2
    f = lambda a: np.ascontiguousarray(a, dtype=np.float32)
    xb = x[b]
    xo = xb.reshape(NBLK, 128, D)[p::2].reshape(OWN * 128, D)
    xh = np.zeros((32, D), np.float32)
    hmask = np.ones((128, 32), np.float32)
    for j in range(OWN):
        st = (2 * j + p) * 128
        if st == 0:
            hmask[:, 0:2] = 0.0
        else:
            xh[2 * j:2 * j + 2] = xb[st - 2:st]
    tri = np.where(np.arange(128)[None, :] <= np.arange(128)[:, None], 0.0, -BIG).astype(np.float32)
    cmask = np.zeros((128, 256), np.float32)
    if p == 0:
        cmask[:, 0:128] = tri
        cmask[:, 128:256] = -BIG
    else:
        cmask[:, 128:256] = tri
    sl = np.arange(128)[:, None]
    tl = np.arange(128)[None, :]
    biasT = np.zeros((128, 4, 16, 128), np.float32)
    for r in range(4):
        delta = p - 1 + r
        if delta < 0:
            continue
        dist = 128 * delta + tl - sl
        bk = t5_bucket_np(dist.astype(np.int32))
        biasT[:, r] = np.transpose(rel_bias[bk], (0, 2, 1))
    return {
        "xa": f(xb), "xo": f(xo), "xh": xh, "hmask": hmask, "cmask": cmask, "biasT": f(biasT.reshape(128, -1)),
        "c": f(c[b].reshape(16, 128)), "norm1_w": f(norm1_w[0].reshape(16, 128)), "norm2_w": f(norm2_w[0].reshape(16, 128)),
        "w_ada": f(w_ada[0]), "b_ada": f(b_ada[0].reshape(96, 128)), "w_in": f(w_in[0]),
        "conv_w": f(conv_w[0].reshape(24, 128)), "w_conv_out": f(w_conv_out[0]),
        "q_norm_w": f(q_norm_w[0].reshape(64, 1)), "k_norm_w": f(k_norm_w[0].reshape(64, 1)),
        "idx_k_norm_w": f(idx_k_norm_w[0].reshape(64, 1)), "idx_k_norm_b": f(idx_k_norm_b[0].reshape(64, 1)),
        "w_attn_out": f(w_attn_out[0]), "w_o": f(w_o[0]), "w_router": f(w_router[0]), "router_bias": f(router_bias[0].reshape(1, E)),
        "w1": f(w1[0]), "w3": f(w3[0]), "w2": f(w2[0]), "ws1": f(ws1[0]), "ws3": f(ws3[0]), "ws2": f(ws2[0]),
    }


def kernel(**inputs):
    inputs = {k: np.asarray(v) for k, v in inputs.items()}
    nc, sch, es = build_program()
    nc = finish(nc, sch, es)
    shared = None
    in_maps = []
    for core in range(8):
        m = make_inputs(core, **inputs)
        if shared is None:
            shared = m
        else:
            for k in ("w_ada", "w_in", "w_conv_out", "w_attn_out", "w_o", "w_router", "w1", "w3", "w2", "ws1", "ws3", "ws2"):
                m[k] = shared[k]
        in_maps.append(m)
    res = run_bass_kernel_spmd(nc, in_maps, core_ids=list(range(8)))
    out = np.zeros((4, S, D), np.float32)
    for core in range(8):
        b, p = core // 2, core % 2
        o = np.asarray(res.results[core]["out"]).reshape(OWN, 128, D)
        out[b].reshape(NBLK, 128, D)[p::2] = o
    return out
```

```python
import math
import numpy as np
import concourse.bass as bass
import concourse.mybir as mybir
from concourse.bass_utils import run_bass_kernel_spmd

F32 = mybir.dt.float32
BF16 = mybir.dt.bfloat16
AF = mybir.ActivationFunctionType
ALU = mybir.AluOpType
AX = mybir.AxisListType

D = 2048
KC = 16
S = 4096
NBLK = 32
OWN = 16
NT = 4
E = 64
FE = 512
BIG = 1.0e30
EPS = 1e-6
NBIS = 18
COMPUTE = ("pe", "act", "dve", "pool")
DEBUG = {}


class Op:
    __slots__ = ("eng", "fn", "deps", "needed", "event", "is_dma", "presem")

    def __init__(self, eng, fn, is_dma=False):
        self.eng, self.fn, self.is_dma = eng, fn, is_dma
        self.deps, self.needed, self.event, self.presem = [], False, None, None


class Sched:
    def __init__(self):
        self.ops = {e: [] for e in ("pe", "act", "dve", "pool", "sp")}
        self.bufw, self.bufr = {}, {}
        self.since = []
        self.limit = None
        self.count = 0

    def op(self, eng, fn, r=(), w=(), is_dma=False):
        o = Op(eng, fn, is_dma)
        self.count += 1
        if self.limit is not None and self.count > self.limit:
            return o
        deps = set()
        for k in r:
            if k in self.bufw:
                deps.add(self.bufw[k])
        for k in w:
            if k in self.bufw:
                deps.add(self.bufw[k])
            deps.update(self.bufr.get(k, ()))
        deps.discard(o)
        o.deps = list(deps)
        for d in o.deps:
            d.needed = True
        for k in r:
            self.bufr.setdefault(k, []).append(o)
        for k in w:
            self.bufw[k] = o
            self.bufr[k] = []
        self.ops[eng].append(o)
        self.since.append(o)
        return o

    def dma(self, q, out, in_, r=(), w=(), **kw):
        return self.op(q, lambda e: e.dma_start(out=out, in_=in_, **kw), r, w, is_dma=True)

    def barrier(self):
        tails = []
        last = {}
        for o in self.since:
            if o.is_dma:
                tails.append(o)
            else:
                last[o.eng] = o
        tails += list(last.values())
        for t in tails:
            t.needed = True
        for e in self.ops:
            o = Op(e, None)
            o.deps = list(tails)
            self.ops[e].append(o)
        self.since = []
        self.bufw, self.bufr = {}, {}


def t5_bucket_np(n):
    n = np.maximum(n, 0)
    nf = np.maximum(n, 1).astype(np.float32)
    large = 16 + (np.log(nf / np.float32(16)) / np.float32(math.log(8.0)) * np.float32(16)).astype(np.int32)
    large = np.minimum(large, 31)
    return np.where(n < 16, n, large)


def build_program(dbg=()):
    nc = bass.Bass("TRN2", target_bir_lowering=False)
    sch = Sched()
    for d_ in dbg:
        if isinstance(d_, str) and d_.startswith("maxops="):
            sch.limit = int(d_.split("=")[1])

    def din(name, shape, dt=F32):
        return nc.dram_tensor(name, list(shape), dt, kind="ExternalInput").ap()

    def dscr(name, shape, dt):
        kind = "ExternalOutput" if name in dbg else "Internal"
        return nc.dram_tensor(name, list(shape), dt, kind=kind).ap()

    xa = din("xa", [S, D])
    xo = din("xo", [OWN * 128, D])
    xh = din("xh", [32, D])
    hmask_d = din("hmask", [128, 32])
    cmask_d = din("cmask", [128, 256])
    biasT_d = din("biasT", [128, 4 * 16 * 128])
    c_d = din("c", [16, 128])
    n1_d = din("norm1_w", [16, 128])
    n2_d = din("norm2_w", [16, 128])
    wada_d = din("w_ada", [D, 6 * D])
    bada_d = din("b_ada", [96, 128])
    win_d = din("w_in", [D, 9808])
    convw_d = din("conv_w", [24, 128])
    wco_d = din("w_conv_out", [1024, D])
    qnw_d = din("q_norm_w", [64, 1])
    knw_d = din("k_norm_w", [64, 1])
    ikw_d = din("idx_k_norm_w", [64, 1])
    ikb_d = din("idx_k_norm_b", [64, 1])
    wao_d = din("w_attn_out", [1024, D])
    wo_d = din("w_o", [D, D])
    wr_d = din("w_router", [D, E])
    rb_d = din("router_bias", [1, E])
    w1_d = din("w1", [E, D, FE])
    w3_d = din("w3", [E, D, FE])
    w2_d = din("w2", [E, FE, D])
    ws1_d = din("ws1", [D, FE])
    ws3_d = din("ws3", [D, FE])
    ws2_d = din("ws2", [FE, D])
    out_d = nc.dram_tensor("out", [OWN * 128, D], F32, kind="ExternalOutput").ap()

    qT_s = dscr("qT_s", [OWN, 128, 8 * 128], BF16)
    qiT_s = dscr("qiT_s", [OWN, 128, 8 * 128], BF16)
    sgn_s = dscr("sgn_s", [OWN, 128, 16], F32)
    mc_s = dscr("mc_s", [NT, 128, 16 * 512], BF16)
    sgb_s = dscr("sgb_s", [NT, 128, 16 * 512], BF16)
    attn_s = dscr("attn_s", [OWN, 64, 16 * 128], BF16)
    x1_s = dscr("x1_s", [OWN * 128, D], F32)
    mod_s = dscr("mod_s", [96, 128], F32)

    from contextlib import ExitStack
    es = ExitStack()

    def sb(name, shape, dt):
        return es.enter_context(nc.sbuf_tensor(name, list(shape), dt))

    def pst(name, shape, dt):
        return es.enter_context(nc.psum_tensor(name, list(shape), dt))

    ident_b = sb("ident_b", [128, 128], BF16)
    ident_f = sb("ident_f", [128, 128], F32)
    ones_b = sb("ones_b", [128, 128], BF16)
    ones_f = sb("ones_f", [1, 128], F32)
    modT = sb("modT", [128, 96], F32)
    a1 = sb("a1", [128, 16], F32)
    a2 = sb("a2", [128, 16], F32)
    colv = sb("colv", [128, 8], F32)
    convw = sb("convw", [128, 24], F32)
    hmask = sb("hmask_t", [128, 32], F32)
    cmask = sb("cmask_t", [128, 256], F32)
    rbias = sb("rbias", [128, E], F32)
    ring = [sb(f"ring{i}", [128, 16 * 512], BF16) for i in range(6)]
    ARENA = 53000
    arena = sb("arena", [128, ARENA], BF16)
    ps = [pst(f"ps{i}", [128, 512], F32) for i in range(8)]
    psb = [p[:].bitcast(BF16) for p in ps]

    aoff = [0]

    def aview(n_elems, dt):
        nb = n_elems * (2 if dt == F32 else 1)
        nb = (nb + 1) // 2 * 2
        o = aoff[0]
        assert o + nb <= ARENA, (o, nb)
        aoff[0] = o + nb
        v = arena[:, o:o + nb]
        return v.bitcast(F32) if dt == F32 else v

    def areset():
        aoff[0] = 0

    rstate = {"i": 0}

    def wload(parts):
        i = rstate["i"]
        rstate["i"] += 1
        buf = ring[i % len(ring)]
        key = ("ring", i % len(ring))
        for dst_fn, src in parts:
            sch.dma("pool", dst_fn(buf), src, w=[key])
        return buf, key

    def wchunk(wd, c0, ncols=512, kc=KC):
        src = wd[:, c0:c0 + ncols].rearrange("(k p) n -> p k n", p=128)
        return wload([(lambda b: b[:, 0:kc * ncols].rearrange("p (k n) -> p k n", k=kc), src)])

    V3 = lambda ap, k: ap.rearrange("p (k n) -> p k n", k=k)

    sch.op("pool", lambda e: e.memset(ident_b[:], 0.0), w=["ident_b0"])
    sch.op("pool", lambda e: e.affine_select(out=ident_b[:], in_=ident_b[:], pattern=[[-1, 128]],
                                             compare_op=ALU.not_equal, fill=1.0, base=0, channel_multiplier=1),
           r=["ident_b0"], w=["ident_b"])
    sch.op("pool", lambda e: e.memset(ident_f[:], 0.0), w=["ident_f0"])
    sch.op("pool", lambda e: e.affine_select(out=ident_f[:], in_=ident_f[:], pattern=[[-1, 128]],
                                             compare_op=ALU.not_equal, fill=1.0, base=0, channel_multiplier=1),
           r=["ident_f0"], w=["ident_f"])
    sch.op("pool", lambda e: e.memset(ones_b[:], 1.0), w=["ones_b"])
    sch.op("pool", lambda e: e.memset(ones_f[:], 1.0), w=["ones_f"])
    sch.dma("sp", hmask[:], hmask_d, w=["hmask"])
    sch.dma("sp", cmask[:], cmask_d, w=["cmask"])
    sch.dma("sp", rbias[:], rb_d.partition_broadcast(128), w=["rbias"])
    sch.dma("sp", colv[0:64, 3:4], qnw_d, w=["colv_raw"])
    sch.dma("sp", colv[64:128, 3:4], qnw_d, w=["colv_raw"])
    sch.dma("sp", colv[0:64, 4:5], knw_d, w=["colv_raw"])
    sch.dma("sp", colv[64:128, 4:5], knw_d, w=["colv_raw"])
    sch.dma("sp", colv[0:64, 1:2], ikw_d, w=["colv_raw"])
    sch.dma("sp", colv[64:128, 1:2], ikw_d, w=["colv_raw"])
    sch.dma("sp", colv[0:64, 2:3], ikb_d, w=["colv_raw"])
    sch.dma("sp", colv[64:128, 2:3], ikb_d, w=["colv_raw"])
    sch.op("dve", lambda e: e.scalar_tensor_tensor(out=colv[:, 0:1], in0=colv[:, 3:4], scalar=0.125, in1=colv[:, 4:5],
                                                   op0=ALU.mult, op1=ALU.mult), r=["colv_raw"], w=["colv"])

    areset()
    rows = aview(128, F32)
    silu_c = aview(16, BF16)
    tmpc = aview(96, F32)

    def vec_to_cols(src_d, n, dst_key, psum_ap):
        sch.dma("sp", rows[0:n, :], src_d, w=["rows"])
        sch.op("pe", lambda e: e.transpose(psum_ap, rows[0:n, :], ident_f[0:n, 0:n]), r=["rows", "ident_f"], w=[dst_key])

    vec_to_cols(c_d, 16, "ps0", ps[0][:, 0:16])
    sch.op("act", lambda e: e.activation(out=silu_c[:], in_=ps[0][:, 0:16], func=AF.Silu), r=["ps0"], w=["silu_c"])
    vec_to_cols(n1_d, 16, "ps1", ps[1][:, 0:16])
    sch.op("dve", lambda e: e.tensor_copy(out=a1[:], in_=ps[1][:, 0:16]), r=["ps1"], w=["a1raw"])
    vec_to_cols(n2_d, 16, "ps1", ps[1][:, 0:16])
    sch.op("dve", lambda e: e.tensor_copy(out=a2[:], in_=ps[1][:, 0:16]), r=["ps1"], w=["a2raw"])
    vec_to_cols(bada_d, 96, "ps2", ps[2][:, 0:96])
    sch.op("dve", lambda e: e.tensor_copy(out=tmpc[:], in_=ps[2][:, 0:96]), r=["ps2"], w=["tmpc"])
    vec_to_cols(convw_d, 24, "ps1", ps[1][:, 0:24])
    sch.op("dve", lambda e: e.tensor_copy(out=convw[:], in_=ps[1][:, 0:24]), r=["ps1"], w=["convw"])
    for c in range(24):
        buf, key = wchunk(wada_d, c * 512)
        b3 = V3(buf[:], 16)

        def mm(e, b3=b3, c=c):
            ins = None
            for q in range(4):
                ch = c * 4 + q
                for k in range(KC):
                    ins = e.matmul(ps[3][:, ch:ch + 1], b3[:, k, q * 128:(q + 1) * 128], silu_c[:, k:k + 1],
                                   start=(k == 0), stop=(k == KC - 1))
            return ins
        sch.op("pe", mm, r=[key, "silu_c"], w=["ps3"])
    sch.op("dve", lambda e: e.tensor_tensor(out=modT[:], in0=ps[3][:, 0:96], in1=tmpc[:], op=ALU.add),
           r=["ps3", "tmpc"], w=["modT"])
    sch.op("dve", lambda e: e.scalar_tensor_tensor(out=a1[:], in0=modT[:, 16:32], scalar=1.0, in1=a1[:],
                                                   op0=ALU.add, op1=ALU.mult), r=["modT", "a1raw"], w=["a1"])
    sch.op("dve", lambda e: e.scalar_tensor_tensor(out=a2[:], in0=modT[:, 64:80], scalar=1.0, in1=a2[:],
                                                   op0=ALU.add, op1=ALU.mult), r=["modT", "a2raw"], w=["a2"])
    sh1 = modT[:, 0:16]
    sh2 = modT[:, 48:64]
    sch.op("pe", lambda e: e.transpose(ps[0][0:96, 0:128], modT[:], ident_f[:]), r=["modT", "ident_f"], w=["ps0"])
    sch.op("dve", lambda e: e.tensor_copy(out=rows[0:96, :], in_=ps[0][0:96, 0:128]), r=["ps0"], w=["rows"])
    sch.dma("sp", mod_s, rows[0:96, :], r=["rows"], w=["mod_s"])
    mod_flat = mod_s.rearrange("a b -> (a b)").rearrange("(o n) -> o n", o=1)
    sch.barrier()

    def norm_T(tag, xt, n, acol, shcol, hT_dst, xnb, st, pT, xn_f32=None):
        sch.op("act", lambda e: e.activation(out=xnb[0:n, :], in_=xt, func=AF.Square, accum_out=st[0:n, 0:1]),
               r=[tag + "x"], w=[tag + "xnb", tag + "st0"])
        sch.op("dve", lambda e: e.tensor_scalar(out=st[0:n, 1:2], in0=st[0:n, 0:1], scalar1=1.0 / D, scalar2=EPS,
                                                op0=ALU.mult, op1=ALU.add), r=[tag + "st0"], w=[tag + "st1"])
        sch.op("act", lambda e: e.activation(out=st[0:n, 2:3], in_=st[0:n, 1:2], func=AF.Sqrt),
               r=[tag + "st1"], w=[tag + "st2"])
        sch.op("dve", lambda e: e.reciprocal(out=st[0:n, 3:4], in_=st[0:n, 2:3]), r=[tag + "st2"], w=[tag + "st3"])
        if xn_f32 is not None:
            sch.op("dve", lambda e: e.tensor_scalar(out=xn_f32[0:n, :], in0=xt, scalar1=st[0:n, 3:4], scalar2=None,
                                                    op0=ALU.mult), r=[tag + "x", tag + "st3"], w=[tag + "xnf"])
        sch.op("dve", lambda e: e.tensor_scalar(out=xnb[0:n, :], in0=xt, scalar1=st[0:n, 3:4], scalar2=None,
                                                op0=ALU.mult), r=[tag + "x", tag + "st3"], w=[tag + "xnb"])
        pT3 = [V3(pT[0], 8), V3(pT[1], 8)]

        def tr(e):
            ins = None
            for k in range(KC):
                ins = e.transpose(pT3[k // 8][:, k % 8, 0:n], xnb[0:n, k * 128:(k + 1) * 128], ident_b[0:n, 0:n])
            return ins
        sch.op("pe", tr, r=[tag + "xnb", "ident_b"], w=["pT0", "pT1"])
        for hf in range(2):
            sch.op("dve", lambda e, hf=hf: e.tensor_tensor(
                out=hT_dst[:, hf * 8:(hf + 1) * 8, :], in0=pT3[hf][:, :, 0:n],
                in1=acol[:, hf * 8:(hf + 1) * 8].unsqueeze(2).to_broadcast([128, 8, n]), op=ALU.mult),
                r=["pT%d" % hf, "a1", "a2"], w=[tag + "hT%d" % hf])
            sch.op("dve", lambda e, hf=hf: e.tensor_tensor(
                out=hT_dst[:, hf * 8:(hf + 1) * 8, :], in0=hT_dst[:, hf * 8:(hf + 1) * 8, :],
                in1=shcol[:, hf * 8:(hf + 1) * 8].unsqueeze(2).to_broadcast([128, 8, n]), op=ALU.add),
                r=[tag + "hT%d" % hf, "modT"], w=[tag + "hT%d" % hf])

    pTb = [psb[6], psb[7]]

    areset()
    hT = V3(aview(16 * 520, BF16), 16)
    xblk = [aview(D, F32) for _ in range(2)]
    xnb = aview(D, BF16)
    stt = aview(8, F32)
    qtm = aview(4 * 512, F32)
    qn = aview(4 * 512, BF16)
    qsqb = aview(512, F32)
    qst = aview(64, F32)
    qTt = [aview(8 * 128, BF16) for _ in range(4)]
    wit = aview(64, F32)
    sgn = aview(64, F32)
    absw = aview(64, F32)
    wwi = aview(16 * 16, BF16)
    uT = V3(aview(8 * 512, BF16), 8)
    vbuf2 = [aview(4 * 130, F32) for _ in range(2)]
    cusb2 = [aview(512, F32) for _ in range(2)]
    cuh2 = [aview(8, F32) for _ in range(2)]
    ybuf2 = [aview(512, F32) for _ in range(2)]
    sga2 = [aview(512, F32) for _ in range(2)]
    mcb = [aview(512, BF16) for _ in range(2)]
    sgbb = [aview(512, BF16) for _ in range(2)]
    sch.dma("pool", V3(wwi[:], 16), win_d[:, 5696:5712].rearrange("(k p) n -> p k n", p=128), w=["wwi"])

    for tl in range(NT):
        tg = "p2_"
        for blk in range(4):
            xb = xblk[blk % 2]
            sch.dma("sp", xb[:], xo[(tl * 4 + blk) * 128:(tl * 4 + blk + 1) * 128, :], w=[tg + "x"])
            norm_T(tg, xb[:], 128, a1, sh1, hT[:, :, blk * 128:(blk + 1) * 128], xnb, stt, pTb)
        xb = xblk[0]
        sch.dma("sp", xb[0:8, :], xh[tl * 8:(tl + 1) * 8, :], w=[tg + "x"])
        norm_T(tg, xb[0:8, :], 8, a1, sh1, hT[:, :, 512:520], xnb, stt, pTb)
        HT = [tg + "hT0", tg + "hT1"]

        for blk in range(4):
            def mmwi(e, blk=blk):
                ins = None
                for k in range(KC):
                    ins = e.matmul(ps[4][:, blk * 16:(blk + 1) * 16], hT[:, k, blk * 128:(blk + 1) * 128],
                                   V3(wwi[:], 16)[:, k, :], start=(k == 0), stop=(k == KC - 1))
                return ins
            sch.op("pe", mmwi, r=HT + ["wwi"], w=["ps4"])
        sch.op("dve", lambda e: e.tensor_copy(out=wit[:], in_=ps[4][:, 0:64]), r=["ps4"], w=["wit"])
        sch.op("dve", lambda e: e.tensor_scalar(out=sgn[:], in0=wit[:], scalar1=0.0, scalar2=2.0, op0=ALU.is_gt,
                                                op1=ALU.mult), r=["wit"], w=["sgn0"])
        sch.op("dve", lambda e: e.tensor_scalar(out=sgn[:], in0=sgn[:], scalar1=-1.0, scalar2=None, op0=ALU.add),
               r=["sgn0"], w=["sgn"])
        sch.op("dve", lambda e: e.tensor_tensor(out=absw[:], in0=wit[:], in1=sgn[:], op=ALU.mult),
               r=["wit", "sgn"], w=["absw"])
        for blk in range(4):
            sch.dma("sp", sgn_s[tl * 4 + blk], sgn[:, blk * 16:(blk + 1) * 16], r=["sgn"], w=["sgn_s"])

        for which, c0s in (("q", (3072, 3584)), ("qi", (4608, 5120))):
            for ci, c0 in enumerate(c0s):
                buf, key = wchunk(win_d, c0)
                b3 = V3(buf[:], 16)
                for blk in range(4):
                    pp = ps[blk % 2]

                    def mmq(e, blk=blk, pp=pp, b3=b3):
                        ins = None
                        for k in range(KC):
                            ins = e.matmul(pp[:], hT[:, k, blk * 128:(blk + 1) * 128], b3[:, k, :],
                                           start=(k == 0), stop=(k == KC - 1))
                        return ins
                    sch.op("pe", mmq, r=HT + [key], w=["ps%d" % (blk % 2)])
                    sch.op("act", lambda e, blk=blk, pp=pp: e.activation(out=qtm[:, blk * 512:(blk + 1) * 512], in_=pp[:],
                                                                        func=AF.Copy),
                           r=["ps%d" % (blk % 2)], w=["qtm%d" % blk])
                for blk in range(4):
                    j = tl * 4 + blk
                    q3 = V3(qtm[:, blk * 512:(blk + 1) * 512], 8)
                    qn3 = V3(qn[:, blk * 512:(blk + 1) * 512], 8)
                    if which == "q":
                        sch.op("act", lambda e, blk=blk: e.activation(out=qsqb[:], in_=qtm[:, blk * 512:(blk + 1) * 512],
                                                                      func=AF.Square), r=["qtm%d" % blk], w=["qsq"])
                        sch.op("dve", lambda e: e.tensor_reduce(out=qst[:, 0:8], in_=V3(qsqb[:], 8), axis=AX.X,
                                                                op=ALU.add), r=["qsq"], w=["qst0"])
                        sch.op("dve", lambda e: e.tensor_scalar(out=qst[:, 8:16], in0=qst[:, 0:8], scalar1=1.0 / 64,
                                                                scalar2=EPS, op0=ALU.mult, op1=ALU.add),
                               r=["qst0"], w=["qst1"])
                        sch.op("act", lambda e: e.activation(out=qst[:, 16:24], in_=qst[:, 8:16], func=AF.Sqrt),
                               r=["qst1"], w=["qst2"])
                        sch.op("dve", lambda e: e.reciprocal(out=qst[:, 24:32], in_=qst[:, 16:24]), r=["qst2"], w=["qst3"])
                        qo4 = qn[:, blk * 512:(blk + 1) * 512].rearrange("p (m hi d) -> p hi m d", m=4, hi=2)
                        qi4 = qtm[:, blk * 512:(blk + 1) * 512].rearrange("p (hi m d) -> p hi m d", m=4, hi=2)
                        rs4 = qst[:, 24:32].rearrange("p (hi m) -> p hi m", hi=2).unsqueeze(3).to_broadcast([128, 2, 4, 64])
                        sch.op("dve", lambda e, qo4=qo4, qi4=qi4, rs4=rs4: e.tensor_tensor(out=qo4, in0=qi4, in1=rs4, op=ALU.mult),
                               r=["qtm%d" % blk, "qst3"], w=["qn%d" % blk])
                    else:
                        sch.op("dve", lambda e, q3=q3, qn3=qn3, blk=blk, ci=ci: e.tensor_tensor(
                            out=qn3, in0=q3,
                            in1=absw[:, blk * 16 + ci * 8: blk * 16 + ci * 8 + 8].unsqueeze(2).to_broadcast([128, 8, 64]),
                            op=ALU.mult), r=["qtm%d" % blk, "absw"], w=["qn%d" % blk])
                    qt = qTt[blk]
                    qt3 = V3(qt[:], 8)
                    pT3 = V3(pTb[0], 8)

                    def trq(e, blk=blk):
                        ins = None
                        for m in range(4):
                            src = qn[:, blk * 512 + m * 128: blk * 512 + (m + 1) * 128]
                            ins = e.transpose(pT3[:, m, :], src, ident_b[:])
                        return ins
                    sch.op("pe", trq, r=["qn%d" % blk, "ident_b"], w=["pT0"])
                    sch.op("act", lambda e, qt3=qt3, ci=ci: e.activation(out=qt3[:, ci * 4:(ci + 1) * 4, :], in_=pT3[:, 0:4, :],
                                                                        func=AF.Copy),
                           r=["pT0"], w=["qTt%d_%d" % (blk, ci)])
                    if ci == 1:
                        dst = qT_s if which == "q" else qiT_s
                        sch.dma("sp", dst[j], qt[:], r=["qTt%d_0" % blk, "qTt%d_1" % blk], w=[which + "T_s"])

        for half in range(2):
            bufs = [wchunk(win_d, base + half * 512) for base in (0, 1024, 2048)]
            (bcb, kcb), (bcc, kcc), (bcu, kcu) = [(V3(b[:], 16), k) for b, k in bufs]
            for q4 in range(4):
                ch = half * 4 + q4
                cs = slice(q4 * 128, (q4 + 1) * 128)

                pz = ch % 2
                P0_, P1_, P2_, P3_ = ps[4 * pz], ps[4 * pz + 1], ps[4 * pz + 2], ps[4 * pz + 3]
                K0_, K1_, K2_, K3_ = [{6: "pT0", 7: "pT1"}.get(4 * pz + i_, "ps%d" % (4 * pz + i_)) for i_ in range(4)]
                vbuf, cusb, cuh, ybuf = vbuf2[pz], cusb2[pz], cuh2[pz], ybuf2[pz]
                sfx = "_%d" % pz

                def mmc(e, cs=cs, bcb=bcb, bcc=bcc, bcu=bcu, P0_=P0_, P1_=P1_, P2_=P2_, P3_=P3_):
                    ins = None
                    for pp_, bw in ((P0_, bcb), (P1_, bcc), (P2_, bcu)):
                        for k in range(KC):
                            ins = e.matmul(pp_[:], bw[:, k, cs], hT[:, k, 0:512], start=(k == 0), stop=(k == KC - 1))
                    for hi, bw in ((0, bcc), (1, bcu)):
                        for k in range(KC):
                            ins = e.matmul(P3_[:, hi * 8:(hi + 1) * 8], bw[:, k, cs], hT[:, k, 512:520],
                                           start=(k == 0), stop=(k == KC - 1))
                    return ins
                sch.op("pe", mmc, r=HT + [kcb, kcc, kcu], w=[K0_, K1_, K2_, K3_])
                vb3 = V3(vbuf[:], 4)
                sch.op("act", lambda e, cusb=cusb, P2_=P2_: e.activation(out=cusb[:], in_=P2_[:], func=AF.Copy), r=[K2_], w=["cusb" + sfx])
                sch.op("act", lambda e, cuh=cuh, P3_=P3_: e.activation(out=cuh[:], in_=P3_[:, 8:16], func=AF.Copy), r=[K3_], w=["cuh" + sfx])
                sch.op("dve", lambda e, vb3=vb3, P1_=P1_, cusb=cusb: e.tensor_tensor(out=vb3[:, :, 2:130], in0=V3(P1_[:], 4), in1=V3(cusb[:], 4),
                                                                               op=ALU.mult), r=[K1_, "cusb" + sfx], w=["vbuf_a" + sfx])
                sch.op("dve", lambda e, cuh=cuh, P3_=P3_: e.tensor_tensor(out=cuh[:], in0=P3_[:, 0:8], in1=cuh[:], op=ALU.mult),
                       r=[K3_, "cuh" + sfx], w=["cuh2" + sfx])
                sch.op("dve", lambda e, tl=tl, vb3=vb3, cuh=cuh: e.tensor_tensor(out=vb3[:, :, 0:2], in0=V3(cuh[:], 4),
                                                                               in1=V3(hmask[:, tl * 8:(tl + 1) * 8], 4), op=ALU.mult),
                       r=["cuh2" + sfx], w=["vbuf_b" + sfx])
                y3 = V3(ybuf[:], 4)
                VK = ["vbuf_a" + sfx, "vbuf_b" + sfx]
                sch.op("dve", lambda e, ch=ch, y3=y3, vb3=vb3: e.tensor_scalar(out=y3, in0=vb3[:, :, 2:130], scalar1=convw[:, 16 + ch:17 + ch],
                                                                             scalar2=None, op0=ALU.mult),
                       r=VK, w=["ybuf" + sfx])
                sch.op("dve", lambda e, ch=ch, y3=y3, vb3=vb3: e.scalar_tensor_tensor(out=y3, in0=vb3[:, :, 1:129], scalar=convw[:, 8 + ch:9 + ch],
                                                                                    in1=y3, op0=ALU.mult, op1=ALU.add),
                       r=VK + ["ybuf" + sfx], w=["ybuf" + sfx])
                sch.op("dve", lambda e, ch=ch, y3=y3, vb3=vb3: e.scalar_tensor_tensor(out=y3, in0=vb3[:, :, 0:128], scalar=convw[:, ch:ch + 1],
                                                                                    in1=y3, op0=ALU.mult, op1=ALU.add),
                       r=VK + ["ybuf" + sfx], w=["ybuf" + sfx])
                sch.op("dve", lambda e, ch=ch, P0_=P0_, ybuf=ybuf: e.tensor_tensor(out=uT[:, ch, :], in0=P0_[:], in1=ybuf[:], op=ALU.mult),
                       r=[K0_, "ybuf" + sfx], w=["uT"])

        for c4 in range(4):
            bga, kga = wchunk(win_d, 5712 + c4 * 512)
            bgb, kgb = wchunk(win_d, 7760 + c4 * 512)
            bco, kco = wchunk(wco_d, c4 * 512, kc=8)
            bga3, bgb3, bco3 = V3(bga[:], 16), V3(bgb[:], 16), V3(bco[:, 0:8 * 512], 8)
            for q4 in range(4):
                cc = c4 * 4 + q4
                cs = slice(q4 * 128, (q4 + 1) * 128)

                pz = cc % 2
                G0, G1, G2 = ps[3 * pz], ps[3 * pz + 1], ps[3 * pz + 2]
                GK0, GK1, GK2 = ["ps%d" % (3 * pz + i_) for i_ in range(3)]
                sga = sga2[pz]

                def mmg(e, cs=cs, bga3=bga3, bgb3=bgb3, bco3=bco3, G0=G0, G1=G1, G2=G2):
                    ins = None
                    for k in range(KC):
                        ins = e.matmul(G0[:], bga3[:, k, cs], hT[:, k, 0:512], start=(k == 0), stop=(k == KC - 1))
                    for k in range(KC):
                        ins = e.matmul(G1[:], bgb3[:, k, cs], hT[:, k, 0:512], start=(k == 0), stop=(k == KC - 1))
                    for k in range(8):
                        ins = e.matmul(G2[:], bco3[:, k, cs], uT[:, k, :], start=(k == 0), stop=(k == 7))
                    return ins
                sch.op("pe", mmg, r=HT + [kga, kgb, kco, "uT"], w=[GK0, GK1, GK2])
                sch.op("act", lambda e, sga=sga, G0=G0: e.activation(out=sga[:], in_=G0[:], func=AF.Sigmoid), r=[GK0], w=["sga%d" % pz])
                sgo, mco = sgbb[cc % 2], mcb[cc % 2]
                sch.op("act", lambda e, sgo=sgo, G1=G1: e.activation(out=sgo[:], in_=G1[:], func=AF.Sigmoid),
                       r=[GK1], w=["sgbb%d" % (cc % 2)])
                sch.op("dve", lambda e, mco=mco, G2=G2, sga=sga: e.tensor_tensor(out=mco[:], in0=G2[:], in1=sga[:],
                                                                             op=ALU.mult), r=[GK2, "sga%d" % pz], w=["mcb%d" % (cc % 2)])
                sch.dma("sp", mc_s[tl][:, cc * 512:(cc + 1) * 512], mco[:], r=["mcb%d" % (cc % 2)], w=["mc_s"])
                sch.dma("sp", sgb_s[tl][:, cc * 512:(cc + 1) * 512], sgo[:], r=["sgbb%d" % (cc % 2)], w=["sgb_s"])
    sch.barrier()
    if "stop2" in dbg:
        return nc, sch, es

    areset()
    kT = V3(aview(2 * S, BF16), 2)
    Vt = V3(aview(NBLK * 320, BF16), NBLK)
    kiT = aview(S, BF16)
    bT = aview(4 * 16 * 128, BF16)
    P3BASE = aoff[0]
    hT1 = [V3(aview(16 * 128, BF16), 16) for _ in range(2)]
    xblk = [ring[0][:, 0:2 * D].bitcast(F32), ring[1][:, 0:2 * D].bitcast(F32)]
    kjunk = aview(64, F32)
    xnb = aview(D, BF16)
    stt = aview(8, F32)
    wkv = V3(aview(16 * 576, BF16), 16)
    ksq = aview(256, F32)
    kst = aview(32, F32)
    kn = aview(256, BF16)
    kic = aview(64, F32)
    kicb = aview(128, BF16)
    sch.dma("pool", wkv[:, :, 0:512], win_d[:, 4096:4608].rearrange("(k p) n -> p k n", p=128), w=["wkv"])
    sch.dma("pool", wkv[:, :, 512:576], win_d[:, 5632:5696].rearrange("(k p) n -> p k n", p=128), w=["wkv"])
    sch.dma("pool", bT[:], biasT_d, w=["bT"], max_dma_last_dim=8192)
    sch.op("pool", lambda e: e.memset(Vt[:, :, 256:320], 0.0), w=["Vpad"])
    for g in range(NBLK):
        tg = "p1_%d" % (g % 2)
        xb = xblk[g % 2]
        h1 = hT1[g % 2]
        sch.dma("sp", xb[:], xa[g * 128:(g + 1) * 128, :], w=[tg + "x"])
        norm_T(tg, xb[:], 128, a1, sh1, h1, xnb, stt, pTb)
        HT = [tg + "hT0", tg + "hT1"]

        def mmk(e, h1=h1):
            ins = None
            for k in range(KC):
                ins = e.matmul(ps[0][:], h1[:, k, :], wkv[:, k, 0:512], start=(k == 0), stop=(k == KC - 1))
            for k in range(KC):
                ins = e.matmul(ps[1][:, 0:64], h1[:, k, :], wkv[:, k, 512:576], start=(k == 0), stop=(k == KC - 1))
            return ins
        sch.op("pe", mmk, r=HT + ["wkv"], w=["ps0", "ps1"])
        sch.op("act", lambda e, g=g: e.activation(out=Vt[:, g, 0:256], in_=ps[0][:, 256:512], func=AF.Copy), r=["ps0"], w=["V"])
        sch.op("act", lambda e: e.activation(out=ksq[:], in_=ps[0][:, 0:256], func=AF.Square), r=["ps0"], w=["ksq"])
        sch.op("dve", lambda e: e.tensor_reduce(out=kst[:, 0:4], in_=V3(ksq[:], 4), axis=AX.X, op=ALU.add),
               r=["ksq"], w=["kst0"])
        sch.op("dve", lambda e: e.tensor_scalar(out=kst[:, 4:8], in0=kst[:, 0:4], scalar1=1.0 / 64, scalar2=EPS,
                                                op0=ALU.mult, op1=ALU.add), r=["kst0"], w=["kst1"])
        sch.op("act", lambda e: e.activation(out=kst[:, 8:12], in_=kst[:, 4:8], func=AF.Sqrt), r=["kst1"], w=["kst2"])
        sch.op("dve", lambda e: e.reciprocal(out=kst[:, 12:16], in_=kst[:, 8:12]), r=["kst2"], w=["kst3"])
        sch.op("dve", lambda e: e.tensor_tensor(out=V3(kn[:], 4), in0=V3(ps[0][:, 0:256], 4),
                                                in1=kst[:, 12:16].unsqueeze(2).to_broadcast([128, 4, 64]), op=ALU.mult),
               r=["ps0", "kst3"], w=["kn"])
        sch.op("dve", lambda e: e.tensor_reduce(out=kst[:, 16:17], in_=ps[1][:, 0:64], axis=AX.X, op=ALU.add),
               r=["ps1"], w=["ki0"])
        sch.op("dve", lambda e: e.tensor_scalar(out=kst[:, 17:18], in0=kst[:, 16:17], scalar1=-1.0 / 64, scalar2=None,
                                                op0=ALU.mult), r=["ki0"], w=["ki1"])
        sch.op("dve", lambda e: e.tensor_scalar(out=kic[:], in0=ps[1][:, 0:64], scalar1=kst[:, 17:18], scalar2=None,
                                                op0=ALU.add), r=["ps1", "ki1"], w=["kic"])
        sch.op("act", lambda e: e.activation(out=kjunk[:], in_=kic[:], func=AF.Square, accum_out=kst[:, 18:19]),
               r=["kic"], w=["ki2", "p1junk"])
        sch.op("dve", lambda e: e.tensor_scalar(out=kst[:, 19:20], in0=kst[:, 18:19], scalar1=1.0 / 64, scalar2=EPS,
                                                op0=ALU.mult, op1=ALU.add), r=["ki2"], w=["ki3"])
        sch.op("act", lambda e: e.activation(out=kst[:, 20:21], in_=kst[:, 19:20], func=AF.Sqrt), r=["ki3"], w=["ki4"])
        sch.op("dve", lambda e: e.reciprocal(out=kst[:, 21:22], in_=kst[:, 20:21]), r=["ki4"], w=["ki5"])
        for hf in range(2):
            sch.op("dve", lambda e, hf=hf: e.tensor_scalar(out=kicb[:, hf * 64:(hf + 1) * 64], in0=kic[:],
                                                           scalar1=kst[:, 21:22], scalar2=None, op0=ALU.mult),
                   r=["kic", "ki5"], w=["kicb%d" % hf])
        pT3 = V3(pTb[0], 8)

        def trk(e):
            e.transpose(pT3[:, 0, :], kn[:, 0:128], ident_b[:])
            e.transpose(pT3[:, 1, :], kn[:, 128:256], ident_b[:])
            return e.transpose(pT3[:, 2, :], kicb[:], ident_b[:])
        sch.op("pe", trk, r=["kn", "kicb0", "kicb1", "ident_b"], w=["pT0"])
        for pr in range(2):
            sch.op("act", lambda e, pr=pr, g=g: e.activation(out=kT[:, pr, g * 128:(g + 1) * 128], in_=pT3[:, pr, :],
                                                            func=AF.Identity, scale=colv[:, 0:1]),
                   r=["pT0", "colv"], w=["kT"])
        sch.op("act", lambda e, g=g: e.activation(out=kiT[:, g * 128:(g + 1) * 128], in_=pT3[:, 2, :], func=AF.Identity,
                                                  scale=colv[:, 1:2], bias=colv[:, 2:3]), r=["pT0", "colv_raw"], w=["kiT"])
    sch.barrier()
    if "stop1" in dbg:
        return nc, sch, es

    aoff[0] = P3BASE
    qzb = [aview(16 * 128, BF16) for _ in range(3)]
    for qq_ in qzb:
        sch.op("pool", lambda e, qq_=qq_: e.memset(qq_[:], 0.0), w=["qz_init"])
    sch.barrier()
    qiTb = [aview(8 * 128, BF16) for _ in range(3)]
    sgb_ = [aview(16, F32) for _ in range(3)]
    scoresb = [ring[0][:, 0:2 * S].bitcast(F32), ring[3][:, 0:2 * S].bitcast(F32)]
    m01 = ring[1][:, 0:S]
    sjunk = ring[1][:, S:2 * S]
    mT = V3(ring[2][:, 0:NBLK * 128], NBLK)
    Dmb = [V3(ring[2][:, 4096:4096 + 2048], 16), V3(ring[4][:, 0:2048], 16)]
    rbuf = [ring[2][:, 6144 + i * 512:6144 + (i + 1) * 512] for i in range(4)]
    amb = [aview(16, F32) for _ in range(2)]
    bst = aview(16, F32)
    pbuf = [aview(512, BF16) for _ in range(6)]
    rl = aview(512, F32)
    attn = [aview(16 * 128, BF16) for _ in range(2)]
    bT4 = bT[:].rearrange("p (r h t) -> p r h t", r=4, h=16)
    NQ = OWN if "p3n" not in dbg else 3
    pTm = V3(psb[7], 8)
    PQB = (3, 4, 7)

    def geom(j):
        nkb = 2 * j + 2
        n = nkb * 128
        nch = (n + 511) // 512
        return nkb, n, nch

    def gen_indexer(j):
        nkb, n, nch = geom(j)
        t3, t2 = j % 3, j % 2
        tg = "p3_%d" % t3
        qz3 = V3(qzb[t3][:], 16)
        qiT3 = V3(qiTb[t3][:], 8)
        sg = sgb_[t3]
        Dm = Dmb[t2]
        scores = scoresb[t2]
        am = amb[t2]
        DK, SK, AK = "Dm%d" % t2, "scores%d" % t2, "am%d" % t2
        qsrc = qT_s[j].rearrange("p (s t) -> p s t", s=8)
        for ci in range(2):
            sch.dma("sp", qz3[0:64, 8 * ci:8 * ci + 4, :], qsrc[0:64, 4 * ci:4 * ci + 4, :], w=[tg + "qT"])
            sch.dma("sp", qz3[64:128, 8 * ci + 4:8 * ci + 8, :], qsrc[64:128, 4 * ci:4 * ci + 4, :], w=[tg + "qT"])
        sch.dma("sp", qiTb[t3][:], qiT_s[j], w=[tg + "qiT"])
        sch.dma("sp", sg[:], sgn_s[j], w=[tg + "sg"])
        sch.op("dve", lambda e: e.tensor_tensor(out=Dm, in0=ident_b[:].unsqueeze(1).to_broadcast([128, 16, 128]),
                                                in1=sg[:].unsqueeze(2).to_broadcast([128, 16, 128]), op=ALU.mult),
               r=[tg + "sg"], w=[DK])
        yield
        for c in range(nch):
            w_ = min(512, n - c * 512)
            last = (c == nch - 1)

            def dots(h, c=c, w_=w_):
                half, slot = h % 2, h // 2
                pd = ps[h % 2]
                sch.op("pe", lambda e, pd=pd, half=half, slot=slot: e.matmul(
                    pd[:, 0:w_], qiT3[half * 64:(half + 1) * 64, slot, :],
                    kiT[half * 64:(half + 1) * 64, c * 512:c * 512 + w_], start=True, stop=True),
                    r=[tg + "qiT"], w=["ps%d" % (h % 2)])
                rb = rbuf[h % 4]
                if h % 2 == 0:
                    sch.op("act", lambda e, pd=pd, rb=rb: e.activation(out=rb[:, 0:w_], in_=pd[:, 0:w_], func=AF.Relu),
                           r=["ps%d" % (h % 2)], w=["rbuf%d" % (h % 4)])
                else:
                    sch.op("dve", lambda e, pd=pd, rb=rb: e.tensor_scalar(out=rb[:, 0:w_], in0=pd[:, 0:w_], scalar1=0.0,
                                                                          scalar2=None, op0=ALU.max),
                           r=["ps%d" % (h % 2)], w=["rbuf%d" % (h % 4)])

            dots(0)
            dots(1)
            for h in range(16):
                rb = rbuf[h % 4]
                sch.op("pe", lambda e, h=h, rb=rb, w_=w_: e.matmul(ps[2][:, 0:w_], Dm[:, h, :], rb[:, 0:w_],
                                                                  start=(h == 0), stop=(h == 15)),
                       r=["rbuf%d" % (h % 4), DK], w=["ps2"])
                if h + 2 < 16:
                    dots(h + 2)
                yield
            sch.op("dve", lambda e, c=c, w_=w_: e.tensor_reduce(out=am[:, c:c + 1], in_=ps[2][:, 0:w_], axis=AX.X, op=ALU.max,
                                                                apply_absolute_value=True), r=["ps2"], w=[AK])
            if last:
                if w_ > 256:
                    sch.op("dve", lambda e, c=c, w_=w_: e.tensor_copy(out=scores[:, c * 512:c * 512 + w_ - 256],
                                                                      in_=ps[2][:, 0:w_ - 256]),
                           r=["ps2"], w=[SK])
                sch.op("dve", lambda e, c=c, w_=w_: e.tensor_tensor(out=scores[:, c * 512 + w_ - 256:c * 512 + w_],
                                                                    in0=ps[2][:, w_ - 256:w_], in1=cmask[:], op=ALU.add),
                       r=["ps2"], w=[SK])
            else:
                sch.op("dve", lambda e, c=c: e.tensor_copy(out=scores[:, c * 512:(c + 1) * 512], in_=ps[2][:]),
                       r=["ps2"], w=[SK])
            yield

    def gen_bisect(j):
        nkb, n, nch = geom(j)
        t2 = j % 2
        scores = scoresb[t2]
        am = amb[t2]
        SK, AK = "scores%d" % t2, "am%d" % t2
        sch.op("dve", lambda e: e.tensor_reduce(out=bst[:, 0:1], in_=am[:, 0:nch], axis=AX.X, op=ALU.max),
               r=[AK], w=["b_am0"])
        sch.op("dve", lambda e: e.tensor_scalar(out=bst[:, 0:1], in0=bst[:, 0:1], scalar1=1.001, scalar2=1e-6, op0=ALU.mult,
                                                op1=ALU.add), r=["b_am0"], w=["b_am"])
        sch.op("dve", lambda e: e.tensor_scalar(out=bst[:, 1:2], in0=bst[:, 0:1], scalar1=-1.0, scalar2=None, op0=ALU.mult),
               r=["b_am"], w=["b_lo"])
        yield
        thr_cnt = 512.0 - n - 0.5
        for it in range(NBIS):
            sc_ = 2.0 ** (-it)
            sch.op("dve", lambda e, sc_=sc_: e.scalar_tensor_tensor(out=bst[:, 2:3], in0=bst[:, 0:1], scalar=-sc_,
                                                                    in1=bst[:, 1:2], op0=ALU.mult, op1=ALU.subtract),
                   r=["b_am", "b_lo"], w=["b_nm"])
            sch.op("act", lambda e: e.activation(out=sjunk[:, 0:n], in_=scores[:, 0:n], func=AF.Sign, bias=bst[:, 2:3],
                                                 scale=1.0, accum_out=bst[:, 3:4]),
                   r=[SK, "b_nm"], w=["b_cnt", "sjunk"])
            sch.op("dve", lambda e, sc_=sc_: e.tensor_scalar(out=bst[:, 4:5], in0=bst[:, 3:4], scalar1=thr_cnt, scalar2=sc_,
                                                             op0=ALU.is_ge, op1=ALU.mult), r=["b_cnt"], w=["b_c2"])
            sch.op("dve", lambda e: e.scalar_tensor_tensor(out=bst[:, 1:2], in0=bst[:, 4:5], scalar=bst[:, 0:1],
                                                           in1=bst[:, 1:2], op0=ALU.mult, op1=ALU.add),
                   r=["b_c2", "b_am", "b_lo"], w=["b_lo"])
            yield

    def emit_masks(j):
        nkb, n, nch = geom(j)
        scores = scoresb[j % 2]
        SK = "scores%d" % (j % 2)
        sch.op("dve", lambda e: e.tensor_scalar(out=m01[:, 0:n], in0=scores[:, 0:n], scalar1=bst[:, 1:2], scalar2=None,
                                                op0=ALU.is_ge), r=[SK, "b_lo"], w=["m01"])
        for b0 in range(0, nkb, 8):
            nb_ = min(8, nkb - b0)

            def trm(e, b0=b0, nb_=nb_):
                ins = None
                for i in range(nb_):
                    ins = e.transpose(pTm[:, i, :], m01[:, (b0 + i) * 128:(b0 + i + 1) * 128], ident_b[:])
                return ins
            sch.op("pe", trm, r=["m01"], w=["ps7"])
            sch.op("act", lambda e, b0=b0, nb_=nb_: e.activation(out=mT[:, b0:b0 + nb_, :], in_=pTm[:, 0:nb_, :],
                                                               func=AF.Copy), r=["ps7"], w=["mT"])

    def gen_main(j):
        nkb, n, nch = geom(j)
        t3, t2 = j % 3, j % 2
        tg = "p3_%d" % t3
        qz3 = V3(qzb[t3][:], 16)
        at = attn[t2]
        at3 = V3(at[:], 16)
        ATK = "attn%d" % t2
        for g in range(4):
            pr = g // 2
            qg = qz3[:, 4 * g:4 * g + 4, :]

            def qk(kb, g=g, qg=qg, pr=pr):
                pq = ps[PQB[kb % 3]]
                r_ = min(2 * j + 1 - kb, 3)

                def mmqk(e):
                    e.matmul(pq[:], kT[:, pr, kb * 128:(kb + 1) * 128], qg, start=True, stop=False)
                    return e.matmul(pq[:], ident_b[:], bT4[:, r_, 4 * g:4 * g + 4, :], start=False, stop=True)
                sch.op("pe", mmqk, r=[tg + "qT"], w=["ps%d" % PQB[kb % 3]])

            LAG = 3
            for k0 in range(min(LAG, nkb)):
                qk(k0)
            for kb in range(nkb):
                pq = ps[PQB[kb % 3]]
                pqk = "ps%d" % PQB[kb % 3]
                pbf = pbuf[kb % 6]
                pbk = "pbuf%d" % (kb % 6)
                sch.op("act", lambda e, pq=pq, pbf=pbf: e.activation(out=pbf[:], in_=pq[:], func=AF.Exp), r=[pqk], w=[pbk])
                if kb + LAG < nkb:
                    qk(kb + LAG)
                sch.op("dve", lambda e, pbf=pbf, kb=kb: e.tensor_tensor(
                    out=V3(pbf[:], 4), in0=V3(pbf[:], 4), in1=mT[:, kb, :].unsqueeze(1).to_broadcast([128, 4, 128]),
                    op=ALU.mult), r=[pbk, "mT"], w=[pbk])

                def mmpv(e, pbf=pbf, kb=kb, g=g):
                    e.matmul(ps[5][:], Vt[:, kb, g * 64:g * 64 + 128], pbf[:], start=(kb == 0), stop=(kb == nkb - 1))
                    return e.matmul(ps[6][:], ones_b[:], pbf[:], start=(kb == 0), stop=(kb == nkb - 1))
                sch.op("pe", mmpv, r=[pbk], w=["ps5", "ps6"])
                yield
            sch.op("dve", lambda e: e.reciprocal(out=rl[0:64, :], in_=ps[6][0:64, :]), r=["ps6"], w=["rl"])
            sch.op("dve", lambda e, g=g: e.tensor_tensor(out=at3[0:64, 4 * g:4 * g + 4, :], in0=V3(ps[5][0:64, :], 4),
                                                         in1=V3(rl[0:64, :], 4), op=ALU.mult),
                   r=["ps5", "rl"], w=[ATK])
            yield
        sch.dma("sp", attn_s[j], at[0:64, :], r=[ATK], w=["attn_s"])
        yield

    def run_all(g):
        for _ in g:
            pass

    def take(g, k):
        if g is None:
            return None
        for _ in range(k):
            try:
                next(g)
            except StopIteration:
                return None
        return g

    run_all(gen_indexer(0))
    gM = None
    for j in range(NQ):
        gB = gen_bisect(j)
        gI = gen_indexer(j + 1) if j + 1 < NQ else None
        lenI = (17 * geom(j + 1)[2] + 1) if j + 1 < NQ else 0
        lenM = (4 * geom(j - 1)[0] + 5) if j >= 1 else 0
        kI = (lenI + NBIS - 1) // NBIS
        kM = (lenM + NBIS - 1) // NBIS
        while gB is not None:
            gB = take(gB, 1)
            gI = take(gI, kI)
            gM = take(gM, kM)
        if gI is not None:
            run_all(gI)
        if gM is not None:
            run_all(gM)
        emit_masks(j)
        gM = gen_main(j)
    run_all(gM)
    sch.barrier()
    if "stop3" in dbg:
        return nc, sch, es

    areset()
    attnT = V3(aview(16 * 512, BF16), 16)
    mcT = aview(16 * 512, BF16)
    sgbT = aview(16 * 512, BF16)
    tmpb = aview(512, BF16)
    xt4 = aview(4 * D, F32)
    tmpf = aview(512, F32)
    g1_bc = aview(D, F32)
    sch.dma("sp", g1_bc[:], mod_flat[:, 32 * 128:48 * 128].partition_broadcast(128), w=["g1_bc"])
    for tl in range(NT):
        for blk in range(4):
            sch.dma("sp", attnT[0:64, :, blk * 128:(blk + 1) * 128],
                    attn_s[tl * 4 + blk].rearrange("p (h t) -> p h t", h=16), w=["attnT"])
        sch.dma("sp", mcT[:], mc_s[tl], w=["mcT"])
        sch.dma("sp", sgbT[:], sgb_s[tl], w=["sgbT"])
        sch.dma("sp", V3(xt4[:], 4), xo[tl * 512:(tl + 1) * 512, :].rearrange("(b p) n -> p b n", p=128), w=["xt4"])
        for c4 in range(4):
            i = rstate["i"]
            rstate["i"] += 1
            buf = ring[i % len(ring)]
            key = ("ring", i % len(ring))
            sch.dma("pool", V3(buf[0:64, :], 16), wao_d[:, c4 * 512:(c4 + 1) * 512].rearrange("(h p) n -> p h n", p=64), w=[key])
            b3 = V3(buf[:], 16)
            for q4 in range(4):
                cc = c4 * 4 + q4
                cs = slice(q4 * 128, (q4 + 1) * 128)
                pp = ps[cc % 2]
                pk = "ps%d" % (cc % 2)

                def mma(e, cs=cs, pp=pp, b3=b3):
                    ins = None
                    for h in range(16):
                        ins = e.matmul(pp[:], b3[0:64, h, cs], attnT[0:64, h, :], start=(h == 0), stop=(h == 15))
                    return ins
                sch.op("pe", mma, r=[key, "attnT"], w=[pk])
                sch.op("dve", lambda e, cc=cc, pp=pp: e.tensor_tensor(out=tmpb[:], in0=pp[:], in1=sgbT[:, cc * 512:(cc + 1) * 512],
                                                                     op=ALU.mult), r=[pk, "sgbT"], w=["tmpb"])
                sch.op("dve", lambda e, cc=cc: e.tensor_tensor(out=mcT[:, cc * 512:(cc + 1) * 512], in0=mcT[:, cc * 512:(cc + 1) * 512],
                                                               in1=tmpb[:], op=ALU.add), r=["tmpb", "mcT"], w=["mixT"])
        mx3 = V3(mcT[:], 16)
        for c4 in range(4):
            buf, key = wchunk(wo_d, c4 * 512)
            b3 = V3(buf[:], 16)
            for blk in range(4):
                pp = ps[2 + blk % 2]
                pk = "ps%d" % (2 + blk % 2)

                def mmo(e, blk=blk, pp=pp, b3=b3):
                    ins = None
                    for k in range(KC):
                        ins = e.matmul(pp[:], mx3[:, k, blk * 128:(blk + 1) * 128], b3[:, k, :], start=(k == 0), stop=(k == KC - 1))
                    return ins
                sch.op("pe", mmo, r=[key, "mixT", "mcT"], w=[pk])
                xs = xt4[:, blk * D + c4 * 512: blk * D + (c4 + 1) * 512]
                sch.op("dve", lambda e, pp=pp, c4=c4: e.tensor_tensor(out=tmpf[:], in0=pp[:], in1=g1_bc[:, c4 * 512:(c4 + 1) * 512],
                                                                     op=ALU.mult), r=[pk, "g1_bc"], w=["tmpf"])
                sch.op("dve", lambda e, xs=xs: e.tensor_tensor(out=xs, in0=xs, in1=tmpf[:], op=ALU.add), r=["tmpf", "xt4"], w=["x1t"])
        sch.dma("sp", x1_s[tl * 512:(tl + 1) * 512, :].rearrange("(b p) n -> p b n", p=128), V3(xt4[:], 4),
                r=["x1t", "xt4"], w=["x1_s"])
    sch.barrier()
    if "stop4" in dbg:
        return nc, sch, es

    areset()
    xb2 = [aview(D, F32) for _ in range(2)]
    acc = aview(4 * D, F32)
    h2T = V3(aview(16 * 512, BF16), 16)
    xnb = aview(D, BF16)
    stt = aview(8, F32)
    wr_f = V3(aview(16 * E, F32), 16)
    wr2 = V3(aview(16 * E, F32), 16)
    brow = aview(E, F32)
    rt = aview(8 * E, F32)
    comb = aview(4 * (E + 1) + 4, F32)
    g2_bc = aview(D, F32)
    XBASE = aoff[0]
    xnf = aview(D, F32)
    xnT = V3(aview(16 * 128, F32), 16)
    aoff[0] = XBASE
    sil = [aview(512, F32) for _ in range(2)]
    gT = [V3(aview(4 * 512, BF16), 4) for _ in range(2)]
    comb3 = V3(comb[:, 0:4 * (E + 1)], 4)
    sch.dma("sp", g2_bc[:], mod_flat[:, 80 * 128:96 * 128].partition_broadcast(128), w=["g2_bc"])
    sch.dma("sp", wr_f, wr_d.rearrange("(k p) n -> p k n", p=128), w=["wr_f"])
    sch.op("dve", lambda e: e.tensor_tensor(out=wr2, in0=wr_f, in1=a2[:].unsqueeze(2).to_broadcast([128, 16, E]), op=ALU.mult),
           r=["wr_f", "a2"], w=["wr2"])

    def mmb(e):
        ins = None
        for k in range(KC):
            ins = e.matmul(ps[7][0:1, 0:E], sh2[:, k:k + 1], wr_f[:, k, :], start=(k == 0), stop=(k == KC - 1))
        return ins
    sch.op("pe", mmb, r=["wr_f", "modT"], w=["ps7"])
    sch.op("dve", lambda e: e.tensor_copy(out=brow[0:1, :], in_=ps[7][0:1, 0:E]), r=["ps7"], w=["brow"])
    sch.op("pool", lambda e: e.memset(comb[:], 1.0), w=["comb"])
    sch.barrier()

    for tl in range(NT):
        tg = "p5_"
        for blk in range(4):
            xs_t = xb2[blk % 2]
            xs = xs_t[:]
            sch.dma("sp", xs, x1_s[(tl * 4 + blk) * 128:(tl * 4 + blk + 1) * 128, :], w=[tg + "x"])
            norm_T(tg, xs, 128, a2, sh2, h2T[:, :, blk * 128:(blk + 1) * 128], xnb, stt, pTb, xn_f32=xnf)
            for hf in range(2):
                def trf(e, hf=hf):
                    ins = None
                    for k in range(8):
                        kk = hf * 8 + k
                        ins = e.transpose(ps[hf * 2 + k // 4][:, (k % 4) * 128:(k % 4 + 1) * 128], xnf[:, kk * 128:(kk + 1) * 128],
                                          ident_f[:])
                    return ins
                sch.op("pe", trf, r=[tg + "xnf", "ident_f"], w=["ps%d" % (hf * 2), "ps%d" % (hf * 2 + 1)])
                for q in range(2):
                    pi = hf * 2 + q
                    sch.op("act", lambda e, pi=pi: e.activation(out=xnT[:, pi * 4:(pi + 1) * 4, :], in_=V3(ps[pi][:], 4), func=AF.Copy),
                           r=["ps%d" % pi], w=["xnT%d" % pi])

            def mmr(e):
                for k in range(KC):
                    e.matmul(ps[4][:, 0:E], xnT[:, k, :], wr2[:, k, :], start=(k == 0), stop=False)
                return e.matmul(ps[4][:, 0:E], ones_f[0:1, :], brow[0:1, :], start=False, stop=True)
            sch.op("pe", mmr, r=["xnT0", "xnT1", "xnT2", "xnT3", "wr2", "brow", "ones_f"], w=["ps4"])
            R = lambda i: rt[:, i * E:(i + 1) * E]
            sch.op("act", lambda e: e.activation(out=R(0), in_=ps[4][:, 0:E], func=AF.Sigmoid), r=["ps4"], w=["r0"])
            sch.op("dve", lambda e: e.tensor_tensor(out=R(1), in0=R(0), in1=rbias[:], op=ALU.add), r=["r0", "rbias"], w=["r1"])
            g3 = V3(R(1), 8)
            sch.op("dve", lambda e: e.tensor_reduce(out=R(7)[:, 0:8], in_=g3, axis=AX.X, op=ALU.max), r=["r1"], w=["m1"])
            sch.op("dve", lambda e: e.tensor_tensor(out=V3(R(2), 8), in0=g3, in1=R(7)[:, 0:8].unsqueeze(2).to_broadcast([128, 8, 8]),
                                                    op=ALU.is_equal), r=["r1", "m1"], w=["r2"])
            sch.op("dve", lambda e: e.scalar_tensor_tensor(out=R(2), in0=R(2), scalar=-BIG, in1=R(1), op0=ALU.mult, op1=ALU.add),
                   r=["r2", "r1"], w=["r2b"])
            sch.op("dve", lambda e: e.tensor_reduce(out=R(7)[:, 8:16], in_=V3(R(2), 8), axis=AX.X, op=ALU.max), r=["r2b"], w=["m2"])
            sch.op("dve", lambda e: e.tensor_tensor(out=R(7)[:, 16:24], in0=R(7)[:, 0:8], in1=R(7)[:, 8:16], op=ALU.add),
                   r=["m1", "m2"], w=["gs"])
            sch.op("dve", lambda e: e.max(out=R(7)[:, 24:32], in_=R(7)[:, 16:24]), r=["gs"], w=["gsort"])
            sch.op("dve", lambda e: e.tensor_scalar(out=R(7)[:, 32:40], in0=R(7)[:, 16:24], scalar1=R(7)[:, 27:28], scalar2=None,
                                                    op0=ALU.is_ge), r=["gs", "gsort"], w=["gmask"])
            sch.op("dve", lambda e: e.tensor_tensor(out=V3(R(3), 8), in0=g3, in1=R(7)[:, 32:40].unsqueeze(2).to_broadcast([128, 8, 8]),
                                                    op=ALU.mult), r=["r1", "gmask"], w=["r3"])
            sch.op("dve", lambda e: e.tensor_scalar(out=R(7)[:, 40:48], in0=R(7)[:, 32:40], scalar1=-1.0, scalar2=BIG,
                                                    op0=ALU.add, op1=ALU.mult), r=["gmask"], w=["gneg"])
            sch.op("dve", lambda e: e.tensor_tensor(out=V3(R(3), 8), in0=V3(R(3), 8),
                                                    in1=R(7)[:, 40:48].unsqueeze(2).to_broadcast([128, 8, 8]), op=ALU.add),
                   r=["r3", "gneg"], w=["r3b"])
            sch.op("dve", lambda e: e.max(out=R(7)[:, 48:56], in_=R(3)), r=["r3b"], w=["esort"])
            sch.op("dve", lambda e: e.tensor_scalar(out=R(4), in0=R(3), scalar1=R(7)[:, 55:56], scalar2=None, op0=ALU.is_ge),
                   r=["r3b", "esort"], w=["r4"])
            sch.op("dve", lambda e: e.tensor_tensor(out=R(5), in0=R(4), in1=R(0), op=ALU.mult), r=["r4", "r0"], w=["r5"])
            sch.op("dve", lambda e: e.tensor_reduce(out=R(7)[:, 56:57], in_=R(5), axis=AX.X, op=ALU.add), r=["r5"], w=["den"])
            sch.op("dve", lambda e: e.reciprocal(out=R(7)[:, 57:58], in_=R(7)[:, 56:57]), r=["den"], w=["rden"])
            sch.op("dve", lambda e, blk=blk: e.tensor_scalar(out=comb3[:, blk, 0:E], in0=R(5), scalar1=R(7)[:, 57:58], scalar2=2.5,
                                                             op0=ALU.mult, op1=ALU.mult), r=["r5", "rden", "comb"], w=["comb"])
        sch.barrier()
        acc3 = V3(acc[:], 4)
        def load_e(e_):
            if e_ < E:
                s1, s3, s2 = w1_d[e_], w3_d[e_], w2_d[e_]
            else:
                s1, s3, s2 = ws1_d, ws3_d, ws2_d
            b1, k1 = wchunk(s1, 0)
            b3_, k3 = wchunk(s3, 0)
            i = rstate["i"]
            rstate["i"] += 1
            b2 = ring[i % len(ring)]
            k2 = ("ring", i % len(ring))
            sch.dma("pool", V3(b2[:], 4), s2.rearrange("(k p) n -> p k n", p=128), w=[k2], max_dma_last_dim=8192)
            return dict(w1v=V3(b1[:], 16), w3v=V3(b3_[:], 16), w2v=V3(b2[:], 4), k1=k1, k3=k3, k2=k2)

        def emit_H(e_, W, fs):
            gt = gT[e_ % 2]
            gk = "gT%d" % (e_ % 2)
            w1v, w3v = W["w1v"], W["w3v"]
            for f in fs:
                pa, pb2 = ps[(f % 2) * 2], ps[(f % 2) * 2 + 1]
                ka, kb2 = "ps%d" % ((f % 2) * 2), "ps%d" % ((f % 2) * 2 + 1)

                def mmh(e, f=f, pa=pa, pb2=pb2):
                    ins = None
                    for k in range(KC):
                        ins = e.matmul(pa[:], w1v[:, k, f * 128:(f + 1) * 128], h2T[:, k, :], start=(k == 0), stop=(k == KC - 1))
                    for k in range(KC):
                        ins = e.matmul(pb2[:], w3v[:, k, f * 128:(f + 1) * 128], h2T[:, k, :], start=(k == 0), stop=(k == KC - 1))
                    return ins
                sch.op("pe", mmh, r=[W["k1"], W["k3"]], w=[ka, kb2])
                sl = sil[f % 2]
                sch.op("act", lambda e, pa=pa, sl=sl: e.activation(out=sl[:], in_=pa[:], func=AF.Silu), r=[ka], w=["sil%d" % (f % 2)])
                sch.op("dve", lambda e, pb2=pb2, sl=sl, f=f: e.tensor_tensor(out=gt[:, f, :], in0=pb2[:], in1=sl[:], op=ALU.mult),
                       r=[kb2, "sil%d" % (f % 2)], w=[gk + "_%d" % f])

        def emit_Y(e_, W):
            gt = gT[e_ % 2]
            gk = "gT%d" % (e_ % 2)
            w2v = W["w2v"]
            for blk in range(4):
                for c4 in range(4):
                    pi = 4 + (blk * 4 + c4) % 4
                    px, pk = ps[pi], "ps%d" % pi

                    def mmy(e, blk=blk, c4=c4, px=px):
                        ins = None
                        for f in range(4):
                            ins = e.matmul(px[:], gt[:, f, blk * 128:(blk + 1) * 128], w2v[:, f, c4 * 512:(c4 + 1) * 512],
                                           start=(f == 0), stop=(f == 3))
                        return ins
                    sch.op("pe", mmy, r=[gk + "_0", gk + "_1", gk + "_2", gk + "_3", W["k2"]], w=[pk])
                    av = acc3[:, blk, c4 * 512:(c4 + 1) * 512]
                    if e_ == 0:
                        sch.op("dve", lambda e, px=px, av=av, blk=blk: e.tensor_scalar(
                            out=av, in0=px[:], scalar1=comb3[:, blk, e_:e_ + 1], scalar2=None, op0=ALU.mult),
                            r=[pk], w=["acc"])
                    else:
                        sch.op("dve", lambda e, px=px, av=av, blk=blk: e.scalar_tensor_tensor(
                            out=av, in0=px[:], scalar=comb3[:, blk, e_:e_ + 1], in1=av, op0=ALU.mult, op1=ALU.add),
                            r=[pk, "acc"], w=["acc"])

        Wc = load_e(0)
        emit_H(0, Wc, range(4))
        for e_ in range(E + 1):
            Wn = None
            if e_ + 1 <= E:
                Wn = load_e(e_ + 1)
                emit_H(e_ + 1, Wn, [0])
            emit_Y(e_, Wc)
            if Wn is not None:
                emit_H(e_ + 1, Wn, [1, 2, 3])
            Wc = Wn
        for blk in range(4):
            xs_t = xb2[blk % 2]
            sch.dma("sp", xs_t[:], x1_s[(tl * 4 + blk) * 128:(tl * 4 + blk + 1) * 128, :], w=["xfin%d" % (blk % 2)])
            for c4 in range(4):
                av = acc3[:, blk, c4 * 512:(c4 + 1) * 512]
                xs = xs_t[:, c4 * 512:(c4 + 1) * 512]
                sch.op("dve", lambda e, av=av, c4=c4: e.tensor_tensor(out=av, in0=av, in1=g2_bc[:, c4 * 512:(c4 + 1) * 512], op=ALU.mult),
                       r=["acc"], w=["acc"])
                sch.op("dve", lambda e, av=av, xs=xs: e.tensor_tensor(out=av, in0=av, in1=xs, op=ALU.add),
                       r=["acc", "xfin%d" % (blk % 2)], w=["acc"])
        sch.dma("sp", out_d[tl * 512:(tl + 1) * 512, :].rearrange("(b p) n -> p b n", p=128), acc3, r=["acc"], w=["out"])
        sch.barrier()
    return nc, sch, es


def finish(nc, sch, es):
    from contextlib import ExitStack
    with es:
        sem_names = ["pe", "act", "dve", "pool"]
        sems = {}
        for n_ in sem_names:
            sems[n_] = es.enter_context(nc.semaphore("s_" + n_))
        dpool = {"sp": [es.enter_context(nc.semaphore("dsp%d" % i)) for i in range(12)],
                 "pool": [es.enter_context(nc.semaphore("dpl%d" % i)) for i in range(8)]}
        with nc.Block() as block:
            @block.tensor
            def _(e):
                sch_emit_one(nc, sch, "pe", e, sems, dpool)

            @block.scalar
            def _(e):
                sch_emit_one(nc, sch, "act", e, sems, dpool)

            @block.vector
            def _(e):
                sch_emit_one(nc, sch, "dve", e, sems, dpool)

            @block.gpsimd
            def _(e):
                sch_emit_one(nc, sch, "pool", e, sems, dpool)

            @block.sync
            def _(e):
                sch_emit_one(nc, sch, "sp", e, sems, dpool)
    return nc


_assigned = {}


def _assign_events(sch, sems, dpool):
    if id(sch) in _assigned:
        return
    _assigned[id(sch)] = True
    for e, lst in sch.ops.items():
        cnt = 0
        dcount = {}
        di = 0
        for o in lst:
            if o.is_dma:
                pool = dpool[e]
                s = pool[di % len(pool)]
                di += 1
                c = dcount.get(id(s), 0)
                o.presem = (s, c * 16)
                dcount[id(s)] = c + 1
                o.event = (s, (c + 1) * 16)
            elif o.fn is not None and o.needed:
                cnt += 1
                o.event = (sems[e], cnt)


def sch_emit_one(nc, sch, e, eng, sems, dpool):
    _assign_events(sch, sems, dpool)
    waited = {}
    finals = {}

    def wait(ev):
        s, v = ev
        if waited.get(id(s), 0) < v:
            eng.wait_ge(s, v)
            waited[id(s)] = v

    for o in sch.ops[e]:
        for d in o.deps:
            if d.event is None:
                continue
            if d.eng == e and e == "pe" and not d.is_dma:
                continue
            wait(d.event)
        if o.fn is None:
            continue
        if o.is_dma:
            if o.presem[1] > 0:
                wait(o.presem)
            ins = o.fn(eng)
            ins.then_inc(o.event[0], 16)
            finals[id(o.event[0])] = o.event
        else:
            ins = o.fn(eng)
            if o.event is not None:
                ins.then_inc(o.event[0], 1)
    for ev in finals.values():
        wait(ev)


def make_inputs(core, x, c, rel_bias, norm1_w, norm2_w, w_ada, b_ada, w_in, conv_w, w_conv_out, q_norm_w, k_norm_w,
                idx_k_norm_w, idx_k_norm_b, w_attn_out, w_o, w_router, router_bias, w1, w3, w2, ws1, ws3, ws2):
    b, p = core // 2, core % 2
    f = lambda a: np.ascontiguousarray(a, dtype=np.float32)
    xb = x[b]
    xo = xb.reshape(NBLK, 128, D)[p::2].reshape(OWN * 128, D)
    xh = np.zeros((32, D), np.float32)
    hmask = np.ones((128, 32), np.float32)
    for j in range(OWN):
        st = (2 * j + p) * 128
        if st == 0:
            hmask[:, 0:2] = 0.0
        else:
            xh[2 * j:2 * j + 2] = xb[st - 2:st]
    tri = np.where(np.arange(128)[None, :] <= np.arange(128)[:, None], 0.0, -BIG).astype(np.float32)
    cmask = np.zeros((128, 256), np.float32)
    if p == 0:
        cmask[:, 0:128] = tri
        cmask[:, 128:256] = -BIG
    else:
        cmask[:, 128:256] = tri
    sl = np.arange(128)[:, None]
    tl = np.arange(128)[None, :]
    biasT = np.zeros((128, 4, 16, 128), np.float32)
    for r in range(4):
        delta = p - 1 + r
        if delta < 0:
            continue
        dist = 128 * delta + tl - sl
        bk = t5_bucket_np(dist.astype(np.int32))
        biasT[:, r] = np.transpose(rel_bias[bk], (0, 2, 1))
    return {
        "xa": f(xb), "xo": f(xo), "xh": xh, "hmask": hmask, "cmask": cmask, "biasT": f(biasT.reshape(128, -1)),
        "c": f(c[b].reshape(16, 128)), "norm1_w": f(norm1_w[0].reshape(16, 128)), "norm2_w": f(norm2_w[0].reshape(16, 128)),
        "w_ada": f(w_ada[0]), "b_ada": f(b_ada[0].reshape(96, 128)), "w_in": f(w_in[0]),
        "conv_w": f(conv_w[0].reshape(24, 128)), "w_conv_out": f(w_conv_out[0]),
        "q_norm_w": f(q_norm_w[0].reshape(64, 1)), "k_norm_w": f(k_norm_w[0].reshape(64, 1)),
        "idx_k_norm_w": f(idx_k_norm_w[0].reshape(64, 1)), "idx_k_norm_b": f(idx_k_norm_b[0].reshape(64, 1)),
        "w_attn_out": f(w_attn_out[0]), "w_o": f(w_o[0]), "w_router": f(w_router[0]), "router_bias": f(router_bias[0].reshape(1, E)),
        "w1": f(w1[0]), "w3": f(w3[0]), "w2": f(w2[0]), "ws1": f(ws1[0]), "ws3": f(ws3[0]), "ws2": f(ws2[0]),
    }


def kernel(**inputs):
    inputs = {k: np.asarray(v) for k, v in inputs.items()}
    nc, sch, es = build_program()
    nc = finish(nc, sch, es)
    shared = None
    in_maps = []
    for core in range(8):
        m = make_inputs(core, **inputs)
        if shared is None:
            shared = m
        else:
            for k in ("w_ada", "w_in", "w_conv_out", "w_attn_out", "w_o", "w_router", "w1", "w3", "w2", "ws1", "ws3", "ws2"):
                m[k] = shared[k]
        in_maps.append(m)
    res = run_bass_kernel_spmd(nc, in_maps, core_ids=list(range(8)))
    out = np.zeros((4, S, D), np.float32)
    for core in range(8):
        b, p = core // 2, core % 2
        o = np.asarray(res.results[core]["out"]).reshape(OWN, 128, D)
        out[b].reshape(NBLK, 128, D)[p::2] = o
    return out
```
